# Optimizing a Trainium2 kernel written in Bass

```python
import math
import jax, jax.numpy as jnp
from jax import lax
import numpy as np

D_MODEL = 4096
BATCH = 4
SEQ = 2048
DEPTH = 1

CHUNK = 64
D_MIX = D_MODEL
NORM_EPS = 1e-6
D_ATTN = D_MIX // 2
N_HEADS = 16
HEAD_DIM = D_ATTN // N_HEADS
KV_LATENT = 512
N_IDX_HEADS = 16
IDX_DIM = 64
TOPK_MAX = 256
Q_BLOCK = 128
D_POOL = D_MIX - D_ATTN
POOL_WINDOWS = (2, 4, 8, 16)
N_POOL_GROUPS = 4
POOL_GROUP = D_POOL // N_POOL_GROUPS
PROJ_SIZES = (D_ATTN, KV_LATENT, N_IDX_HEADS * IDX_DIM, IDX_DIM, N_IDX_HEADS, D_ATTN, D_POOL, D_POOL)
D_IN = sum(PROJ_SIZES)

kernel_name = "hybrid_dsa_pool_parallel_block"


def _rmsnorm(x, g):
    xf = x.astype(jnp.float32)
    y = xf * lax.rsqrt(jnp.mean(xf * xf, axis=-1, keepdims=True) + NORM_EPS)
    return (y * g.astype(jnp.float32)).astype(x.dtype)


def _layernorm(x, g, b):
    xf = x.astype(jnp.float32)
    mu = jnp.mean(xf, axis=-1, keepdims=True)
    var = jnp.mean(jnp.square(xf - mu), axis=-1, keepdims=True)
    y = (xf - mu) * lax.rsqrt(var + NORM_EPS)
    return (y * g.astype(jnp.float32) + b.astype(jnp.float32)).astype(x.dtype)


def _split_cols(z):
    offs = []
    acc = 0
    for s in PROJ_SIZES[:-1]:
        acc += s
        offs.append(acc)
    return jnp.split(z, offs, axis=-1)


def _dsa_attention(q, c, qi, k_idx, w_idx, k_top):
    B, S = q.shape[0], q.shape[1]
    nb = S // Q_BLOCK

    def blocks(a):
        a = a.reshape((B, nb, Q_BLOCK) + a.shape[2:])
        return jnp.moveaxis(a, 1, 0)

    key_chunk = jnp.arange(S) // CHUNK
    idx_scale = IDX_DIM ** -0.5
    attn_scale = HEAD_DIM ** -0.5

    def one_block(args):
        qb, qib, wb, t0 = args
        q_chunk = (t0 + jnp.arange(Q_BLOCK)) // CHUNK
        admissible = key_chunk[None, :] <= q_chunk[:, None]
        dots = jnp.einsum('bqhd,bsd->bqhs', qib.astype(jnp.float32), k_idx.astype(jnp.float32)) * idx_scale
        iscore = jnp.einsum('bqh,bqhs->bqs', wb.astype(jnp.float32), jax.nn.relu(dots))
        iscore = jnp.where(admissible[None], iscore, -jnp.inf)
        _, sel = lax.top_k(iscore, k_top)
        valid = (sel // CHUNK) <= q_chunk[None, :, None]
        c_sel = jax.vmap(lambda cb, ib: cb[ib])(c, sel)
        q_lat = jnp.einsum('bqhd,hcd->bqhc', qb, w_uk_g)
        scores = jnp.einsum('bqhc,bqkc->bqhk', q_lat, c_sel).astype(jnp.float32) * attn_scale
        scores = jnp.where(valid[:, :, None, :], scores, -jnp.inf)
        p = jax.nn.softmax(scores, axis=-1).astype(c.dtype)
        o_lat = jnp.einsum('bqhk,bqkc->bqhc', p, c_sel)
        return jnp.einsum('bqhc,hcd->bqhd', o_lat, w_uv_g)

    w_uk_g, w_uv_g = _dsa_attention.w_uk, _dsa_attention.w_uv
    out = lax.map(one_block, (blocks(q), blocks(qi), blocks(w_idx), jnp.arange(nb) * Q_BLOCK))
    return jnp.moveaxis(out, 0, 1).reshape(q.shape)


def _sparse_attention(q, c, qi, k_idx, w_idx, w_uk, w_uv, k_top):
    B, S = q.shape[0], q.shape[1]
    nb = S // Q_BLOCK

    def blocks(a):
        a = a.reshape((B, nb, Q_BLOCK) + a.shape[2:])
        return jnp.moveaxis(a, 1, 0)

    key_chunk = jnp.arange(S) // CHUNK
    idx_scale = IDX_DIM ** -0.5
    attn_scale = HEAD_DIM ** -0.5
    kf = k_idx.astype(jnp.float32)

    def one_block(args):
        qb, qib, wb, t0 = args
        q_chunk = (t0 + jnp.arange(Q_BLOCK)) // CHUNK
        admissible = key_chunk[None, :] <= q_chunk[:, None]
        dots = jnp.einsum('bqhd,bsd->bqhs', qib.astype(jnp.float32), kf) * idx_scale
        iscore = jnp.einsum('bqh,bqhs->bqs', wb.astype(jnp.float32), jax.nn.relu(dots))
        iscore = jnp.where(admissible[None], iscore, -jnp.inf)
        _, sel = lax.top_k(iscore, k_top)
        valid = (sel // CHUNK) <= q_chunk[None, :, None]
        c_sel = jax.vmap(lambda cb, ib: cb[ib])(c, sel)
        q_lat = jnp.einsum('bqhd,hcd->bqhc', qb, w_uk)
        scores = jnp.einsum('bqhc,bqkc->bqhk', q_lat, c_sel).astype(jnp.float32) * attn_scale
        scores = jnp.where(valid[:, :, None, :], scores, -jnp.inf)
        p = jax.nn.softmax(scores, axis=-1).astype(c_sel.dtype)
        o_lat = jnp.einsum('bqhk,bqkc->bqhc', p, c_sel)
        return jnp.einsum('bqhc,hcd->bqhd', o_lat, w_uv)

    out = lax.map(one_block, (blocks(q), blocks(qi), blocks(w_idx), jnp.arange(nb) * Q_BLOCK))
    return jnp.moveaxis(out, 0, 1).reshape(q.shape)


def _multiscale_pool(u, w_pool, pool_scale):
    B, S = u.shape[0], u.shape[1]
    p = u.reshape(B, S, N_POOL_GROUPS, POOL_GROUP).astype(jnp.float32)
    cs = jnp.concatenate([jnp.zeros((B, 1, N_POOL_GROUPS, POOL_GROUP), jnp.float32), jnp.cumsum(p, axis=1)], axis=1)
    pos = jnp.arange(S)
    win = jnp.array(POOL_WINDOWS, dtype=jnp.int32)
    start = jnp.maximum(pos[:, None] + 1 - win[None, :], 0)
    count = (pos[:, None] + 1 - start).astype(jnp.float32)
    gidx = jnp.broadcast_to(jnp.arange(N_POOL_GROUPS)[None, :], (S, N_POOL_GROUPS))
    cs_start = cs[:, start, gidx]
    mean = (cs[:, 1:] - cs_start) / count[None, :, :, None]
    mixed = (mean - p).astype(u.dtype)
    y = jnp.einsum('bsgc,gcd->bsgd', mixed, w_pool) * pool_scale.reshape(N_POOL_GROUPS, POOL_GROUP)
    return y.reshape(B, S, D_POOL)


def setup_inputs(seed: int = 0) -> dict:
    key = jax.random.key(seed)
    ks = jax.random.split(key, 14)
    f = jnp.float32
    x = jax.random.normal(ks[0], (BATCH, SEQ, D_MODEL), f)
    pre_norm = 1.0 + 0.02 * jax.random.normal(ks[1], (DEPTH, D_MODEL), f)
    w_in = jax.random.normal(ks[2], (DEPTH, D_MODEL, D_IN), f) * D_MODEL ** -0.5
    kv_norm = 1.0 + 0.02 * jax.random.normal(ks[3], (DEPTH, KV_LATENT), f)
    w_uk = jax.random.normal(ks[4], (DEPTH, N_HEADS, KV_LATENT, HEAD_DIM), f) * KV_LATENT ** -0.5
    w_uv = jax.random.normal(ks[5], (DEPTH, N_HEADS, KV_LATENT, HEAD_DIM), f) * KV_LATENT ** -0.5
    idx_k_norm_g = 1.0 + 0.02 * jax.random.normal(ks[6], (DEPTH, IDX_DIM), f)
    idx_k_norm_b = 0.02 * jax.random.normal(ks[7], (DEPTH, IDX_DIM), f)
    w_pool = jax.random.normal(ks[8], (DEPTH, N_POOL_GROUPS, POOL_GROUP, POOL_GROUP), f) * POOL_GROUP ** -0.5
    pool_scale = 1.0 + 0.02 * jax.random.normal(ks[9], (DEPTH, D_POOL), f)
    w_out = jax.random.normal(ks[10], (DEPTH, D_MIX, D_MODEL), f) * D_MIX ** -0.5
    post_norm = 1.0 + 0.02 * jax.random.normal(ks[11], (DEPTH, D_MODEL), f)
    return {"x": x, "pre_norm": pre_norm, "w_in": w_in, "kv_norm": kv_norm, "w_uk": w_uk, "w_uv": w_uv,
            "idx_k_norm_g": idx_k_norm_g, "idx_k_norm_b": idx_k_norm_b, "w_pool": w_pool,
            "pool_scale": pool_scale, "w_out": w_out, "post_norm": post_norm}


def reference(x, pre_norm, w_in, kv_norm, w_uk, w_uv, idx_k_norm_g, idx_k_norm_b, w_pool, pool_scale, w_out, post_norm):
    B, S, _ = x.shape
    k_top = min(TOPK_MAX, S // 4)
    for l in range(DEPTH):
        h = _rmsnorm(x, pre_norm[l])
        z = jnp.einsum('bsd,de->bse', h, w_in[l])
        q, c, qi, kix, wix, gate_a, pool_in, gate_b = _split_cols(z)
        q = q.reshape(B, S, N_HEADS, HEAD_DIM)
        c = _rmsnorm(c, kv_norm[l])
        qi = qi.reshape(B, S, N_IDX_HEADS, IDX_DIM)
        kix = _layernorm(kix, idx_k_norm_g[l], idx_k_norm_b[l])
        wix = wix * (N_IDX_HEADS ** -0.5)
        ya = _sparse_attention(q, c, qi, kix, wix, w_uk[l], w_uv[l], k_top).reshape(B, S, D_ATTN)
        ya = ya * jax.nn.silu(gate_a)
        yb = _multiscale_pool(pool_in, w_pool[l], pool_scale[l]) * jax.nn.silu(gate_b)
        y = jnp.einsum('bse,ed->bsd', jnp.concatenate([ya, yb], axis=-1), w_out[l])
        x = x + _rmsnorm(y, post_norm[l])
    return x
```

```python
from contextlib import ExitStack
import numpy as np
import ml_dtypes
import concourse.bass as bass
import concourse.mybir as mybir
from concourse.bass_utils import run_bass_kernel_spmd

F32 = mybir.dt.float32
BF16 = mybir.dt.bfloat16
ALU = mybir.AluOpType
AF = mybir.ActivationFunctionType

ENGS = ("pe", "act", "dve", "pool", "sp")
N_DMA_SEMS = 22

D = 4096
DIN = 9808
SEQ = 2048
KC = 32
GT = 512
C_Q, C_C, C_QI, C_KX, C_WX, C_GA, C_PI, C_GB = 0, 2048, 2560, 3584, 3648, 3664, 5712, 7760
EPS = 1e-6
ATTN_SCALE = 128 ** -0.5
IDX_SCALE = 64 ** -0.5
WIX_SCALE = 16 ** -0.5
BIG = 1.0e30
BIS_R = 16.0
BIS_N = 21
TOPK = 256
MASK_OFF = 100.0


class Op:
    __slots__ = ("eng", "fn", "deps", "dma", "sig", "sem", "val", "idx")

    def __init__(self, eng, fn, dma):
        self.eng = eng
        self.fn = fn
        self.deps = set()
        self.dma = dma
        self.sig = False
        self.sem = None
        self.val = 0


class Sched:
    def __init__(self, nc):
        self.nc = nc
        self.ops = []
        self.lastw = {}
        self.readers = {}
        self.fence = set()
        self.last_eng = {}
        self.last_dma = {}
        self.frozen = False

    def add(self, eng, fn, reads=(), writes=(), dma=None):
        if self.frozen:
            return None
        op = Op(eng, fn, dma)
        idx = len(self.ops)
        op.idx = idx
        if eng != "pe":
            extra = [("pbx", r[1]) for r in reads if isinstance(r, tuple) and r[0] == "pb"]
            if extra:
                writes = list(writes) + extra
        for r in reads:
            w = self.lastw.get(r)
            if w is not None:
                op.deps.add(w)
        for w_ in writes:
            w = self.lastw.get(w_)
            if w is not None:
                op.deps.add(w)
            for rd in self.readers.get(w_, ()):
                op.deps.add(rd)
        for r in reads:
            self.readers.setdefault(r, []).append(idx)
        for w_ in writes:
            self.lastw[w_] = idx
            self.readers[w_] = []
        op.deps |= self.fence
        op.deps.discard(idx)
        self.ops.append(op)
        if dma is None:
            self.last_eng[eng] = idx
        else:
            self.last_dma[dma] = idx
        return idx

    def barrier(self):
        self.fence = set(self.last_eng.values()) | set(self.last_dma.values())

    def emit(self, stack):
        nc = self.nc
        ops = self.ops

        def skip(dop, op):
            return dop.eng == "pe" and op.eng == "pe" and dop.dma is None and op.dma is None

        for op in ops:
            for d in op.deps:
                if not skip(ops[d], op):
                    ops[d].sig = True
        for op in ops:
            if op.dma is not None:
                op.sig = True
        esem = {e: stack.enter_context(nc.semaphore("s_" + e)) for e in ENGS}
        dsem = [stack.enter_context(nc.semaphore("d_%d" % i)) for i in range(N_DMA_SEMS)]
        ecount = {e: 0 for e in ENGS}
        dcount = [0] * N_DMA_SEMS
        for op in ops:
            if not op.sig:
                continue
            if op.dma is not None:
                dcount[op.dma] += 16
                op.sem = dsem[op.dma]
                op.val = dcount[op.dma]
            else:
                ecount[op.eng] += 1
                op.sem = esem[op.eng]
                op.val = ecount[op.eng]
        per_eng = {e: [] for e in ENGS}
        for op in ops:
            per_eng[op.eng].append(op)
        dfinal = list(dcount)
        block = stack.enter_context(nc.Block())

        def make_body(e):
            def body(eng):
                waited = {}
                for op in per_eng[e]:
                    need = {}
                    for d in op.deps:
                        dop = ops[d]
                        if skip(dop, op):
                            continue
                        k = id(dop.sem)
                        if k not in need or need[k][1] < dop.val:
                            need[k] = (dop.sem, dop.val)
                    for k, (sem, val) in need.items():
                        if waited.get(k, 0) >= val:
                            continue
                        eng.wait_ge(sem, val)
                        waited[k] = val
                    ins = op.fn(eng)
                    if op.sig:
                        ins.then_inc(op.sem, 16 if op.dma is not None else 1)
                if e == "sp":
                    for i in range(N_DMA_SEMS):
                        if dfinal[i] > 0:
                            eng.wait_ge(dsem[i], dfinal[i])
            return body

        block.tensor(make_body("pe"))
        block.scalar(make_body("act"))
        block.vector(make_body("dve"))
        block.gpsimd(make_body("pool"))
        block.sync(make_body("sp"))


class _Stop(Exception):
    pass


STOP = None


def build_nc():
    nc = bass.Bass("TRN2", target_bir_lowering=False)

    def chk(label):
        if STOP == label:
            S.frozen = True

    def din(name, shape, dt=F32):
        return nc.dram_tensor(name, list(shape), dt, kind="ExternalInput").ap()

    xk = din("xk", [SEQ, D])
    xo = din("xo", [1024, D])
    xh = din("xh", [2, 128, D])
    w_in = din("w_in", [D, DIN])
    w_out = din("w_out", [D, D])
    w_uk = din("w_uk", [16, 512, 128])
    w_uv = din("w_uv", [16, 512, 128])
    w_pool = din("w_pool", [4, 512, 512])
    gpre_d = din("gpre_b", [128, D])
    kchunk_d = din("kchunk_b", [128, SEQ])
    cst_d = din("cst", [128, 512])
    identf_d = din("ident_f", [128, 128])
    blk64_d = din("blk64", [128, 128])
    identb_d = din("ident_bf", [128, 128], BF16)
    yout = nc.dram_tensor("y", [1024, D], F32, kind="ExternalOutput").ap()

    w_in_v = w_in.rearrange("(kc p) e -> p kc e", p=128)
    w_out_v = w_out.rearrange("(kc p) e -> p kc e", p=128)
    w_pool_v = w_pool.rearrange("g (cc p) d -> p (g cc) d", p=128)

    st = ExitStack()
    st.enter_context(nc.allow_low_precision("bf16 matmul operands, fp32 accumulation"))
    ACOLS = 49152
    arena = st.enter_context(nc.sbuf_tensor("arena", [128, ACOLS], F32))
    pbs = [st.enter_context(nc.psum_tensor("pb%d" % i, [128, 512], F32)) for i in range(8)]
    S = Sched(nc)

    def fv(off, n):
        assert off + n <= ACOLS
        return arena[:, off:off + n]

    def bvw(off, ncols):
        assert off + ncols <= ACOLS
        return arena[:, off:off + ncols].bitcast(BF16)

    def pbT(i):
        return pbs[i][:].bitcast(BF16).rearrange("p (a b) -> p a b", b=128)

    cTn = bvw(0, 4096).rearrange("p (a b) -> p a b", b=SEQ)
    kixT = bvw(4096, 1024)
    o = 5120
    cst = fv(o, 512); o += 512
    ident_f = fv(o, 128); o += 128
    blk64 = fv(o, 128); o += 128
    ones_f = fv(o, 128); o += 128
    ident_bf = bvw(o, 64); o += 64
    ones_bf = bvw(o, 64); o += 64
    assert o <= 6144
    gkv = cst[:, 0:4]
    kxg = cst[:, 4:5]
    kxb = cst[:, 5:6]
    epsT = cst[:, 6:7]
    negoff = cst[:, 7:8]
    pscale = cst[:, 8:24]
    gpost = cst[:, 24:56]
    qchunk = cst[:, 56:64]
    pfix = cst[:, 64:192].rearrange("p (g a b) -> p g a b", g=2, a=4)
    ss2 = cst[:, 192:194]
    sd2 = cst[:, 194:196]
    rstd2 = cst[:, 196:198]
    wix_sb = cst[:, 200:264].rearrange("p (j h) -> p j h", h=16)
    cand = cst[:, 264:268]
    cnt = cst[:, 268:272]
    tflag = cst[:, 272:276]
    sdo = cst[:, 276:280]
    rstdo = cst[:, 280:284]

    YIN0, QT0, QIT0, HT0, HTH0, R10 = 6144, 14336, 18432, 20480, 28672, 30720
    yin = bvw(YIN0, 8192).rearrange("p (a b) -> p a b", b=GT)
    qT = bvw(QT0, 4096).rearrange("p (a b) -> p a b", b=GT)
    qiT = bvw(QIT0, 2048).rearrange("p (a b) -> p a b", b=GT)
    hT = bvw(HT0, 8192).rearrange("p (a b) -> p a b", b=GT)
    hTh = bvw(HTH0, 2048).rearrange("p (a b) -> p a b", b=128)
    xt = [fv(R10 + i * 4096, 4096) for i in range(2)]
    xsb = [bvw(R10 + 8192 + i * 2048, 2048) for i in range(2)]
    gpre = fv(R10 + 12288, 4096)
    wb = [bvw(R10 + i * 2048, 2048).rearrange("p (a b) -> p a b", b=128) for i in range(3)]
    ptmp = [fv(R10 + 6144 + i * 528, 528) for i in range(3)]
    wbx = bvw(R10 + 7744, 256).rearrange("p (a b) -> p a b", b=16)
    mixT = bvw(R10 + 8192, 4096).rearrange("p (a b) -> p a b", b=GT)
    wck_c = bvw(6144, 8192).rearrange("p (a b) -> p a b", b=512)
    wck_k = bvw(14336, 2048).rearrange("p (a b) -> p a b", b=128)
    csb = fv(16384, 2048).rearrange("p (a b) -> p a b", b=512)
    sqc = fv(18432, 2048).rearrange("p (a b) -> p a b", b=512)
    P0M = R10 + 16384
    kxsb = fv(P0M, 512)
    xcb = fv(P0M + 512, 512)
    sq2b = fv(P0M + 1024, 512)
    rsb = fv(P0M + 1536, 512)
    sdb = sq2b
    knb = xcb
    wpool = bvw(HT0, 4096).rearrange("p (a b) -> p a b", b=512)
    acc = [fv(R10 + j * 2048, 2048) for j in range(4)]
    negb = [bvw(R10 + 8192 + j * 1024, 1024) for j in range(4)]
    rbuf = [bvw(R10 + 12288 + i * 256, 256) for i in range(4)]
    junk = bvw(R10 + 13312, 1024)
    maskbf = [bvw(R10 + 14336 + i * 1024, 1024) for i in range(2)]
    kixAB = [bvw(R10 + 16384 + i * 1024, 1024) for i in range(2)]
    kchunk = fv(HTH0, 2048)
    maskT = bvw(HT0, 4096).rearrange("p (k j t) -> p k j t", k=16, j=4)
    diag = [bvw(HT0 + 4096 + i * 1024, 1024).rearrange("p (a b) -> p a b", b=128) for i in range(2)]
    kTh = [bvw(R10 + i * 1024, 1024) for i in range(2)]
    vS4 = [bvw(R10 + 2048 + i * 4096, 4096).rearrange("p (a b) -> p a b", b=512) for i in range(2)]
    wuk = [bvw(R10 + 10240 + i * 256, 256).rearrange("p (a b) -> p a b", b=128) for i in range(2)]
    wuv4 = [bvw(R10 + 10752 + i * 1024, 1024).rearrange("p (a b) -> p a b", b=512) for i in range(2)]
    eT = [bvw(R10 + 12800 + i * 256, 256) for i in range(3)]
    pT = [bvw(R10 + 13568 + i * 256, 256) for i in range(3)]
    rcb = [fv(R10 + 14336 + i * 512, 512) for i in range(2)]
    otmp = [fv(R10 + 15360 + i * 512, 512) for i in range(2)]
    yg = fv(QT0, 16384).rearrange("p (a b) -> p a b", b=GT)
    wb6 = [bvw(R10 + 8192 + i * 2048, 2048).rearrange("p (a b) -> p a b", b=128) for i in range(3)]
    sqb = [bvw(R10 + 14336 + i * 256, 256) for i in range(2)]

    def MM(out, lhsT, rhs, start, stop, r, w):
        S.add("pe", lambda e: e.matmul(out, lhsT, rhs, start=start, stop=stop), r, w)

    def TR(out, in_, ident, r, w):
        S.add("pe", lambda e: e.transpose(out, in_, ident), r, w)

    def ACT(out, in_, func, r, w, scale=None, bias=None, accum=None):
        kw = {}
        if scale is not None:
            kw["scale"] = scale
        if bias is not None:
            kw["bias"] = bias
        if accum is not None:
            kw["accum_out"] = accum
        S.add("act", lambda e: e.activation(out, in_, func, **kw), r, w)

    def TS(eng, out, in0, s1, s2, op0, op1, r, w, accum=None):
        if op1 is None:
            if accum is None:
                S.add(eng, lambda e: e.tensor_scalar(out, in0, s1, None, op0), r, w)
            else:
                raise ValueError
        else:
            if accum is None:
                S.add(eng, lambda e: e.tensor_scalar(out, in0, s1, s2, op0, op1), r, w)
            else:
                S.add(eng, lambda e: e.tensor_scalar(out, in0, s1, s2, op0, op1, accum_out=accum), r, w)

    def TT(eng, out, in0, in1, op, r, w):
        S.add(eng, lambda e: e.tensor_tensor(out, in0, in1, op), r, w)

    def STT(out, in0, scalar, in1, op0, op1, r, w):
        S.add("dve", lambda e: e.scalar_tensor_tensor(out, in0, scalar, in1, op0, op1), r, w)

    def RCP(out, in_, r, w):
        S.add("dve", lambda e: e.reciprocal(out, in_), r, w)

    def CP(eng, out, in_, r, w):
        if eng == "act":
            S.add("act", lambda e: e.copy(out, in_), r, w)
        else:
            S.add(eng, lambda e: e.tensor_copy(out, in_), r, w)

    def DMA(eng, out, in_, r, w, key):
        S.add(eng, lambda e: e.dma_start(out=out, in_=in_), r, w, dma=key)

    K_XT = [0, 1]
    K_WB = [2, 3, 4]
    K_ST = [5, 6]
    K_UK = [7, 8]
    K_UV = [9, 10]
    K_MISC = 11
    K_MISC_SW = 12
    K_UV4 = [[13, 14, 15, 16], [17, 18, 19, 20]]

    DMA("sp", cst, cst_d, [], ["cst"], K_MISC)
    DMA("sp", ident_f, identf_d, [], ["identf"], K_MISC)
    DMA("sp", blk64, blk64_d, [], ["blk64"], K_MISC)
    DMA("sp", ident_bf, identb_d, [], ["identb"], K_MISC)
    S.add("dve", lambda e: e.memset(ones_f, 1.0), [], ["onesf"])
    S.add("dve", lambda e: e.memset(ones_bf, 1.0), [], ["onesb"])
    S.barrier(); chk("setup")

    tr_banks = [0, 1]
    tr_ctr = [0]
    xt_ctr = [0]

    def stage1(src, sl):
        xs = xsb[sl]
        xk_ = ("xs", sl)
        DMA("sp", xt[sl], src, [], [("xt", sl)], K_XT[sl])
        ACT(xs, xt[sl], AF.Square, [("xt", sl)], [xk_, ("ss", sl)], accum=ss2[:, sl:sl + 1])
        ACT(sd2[:, sl:sl + 1], ss2[:, sl:sl + 1], AF.Sqrt, [("ss", sl), "cst"], [("sd", sl)],
            scale=1.0 / D, bias=epsT)
        RCP(rstd2[:, sl:sl + 1], sd2[:, sl:sl + 1], [("sd", sl)], [("rstd", sl)])
        STT(xs, xt[sl], rstd2[:, sl:sl + 1], gpre, ALU.mult, ALU.mult,
            [("xt", sl), ("rstd", sl), "gpre"], [xk_])

    def stage2(sl, dst, dst_keys):
        xs = xsb[sl]
        xk_ = ("xs", sl)
        for kq in range(4):
            b = tr_banks[tr_ctr[0] % 2]
            tr_ctr[0] += 1
            pt = pbT(b)
            for i in range(8):
                kc = kq * 8 + i
                TR(pt[:, i, :], xs[:, kc * 128:(kc + 1) * 128], ident_bf, [xk_, "identb"], [("pb", b)])
            eng = "act" if kq % 2 == 0 else "dve"
            CP(eng, dst[:, kq * 8:(kq + 1) * 8, :], pt, [("pb", b)], [dst_keys[kq]])

    def run_tiles(tiles, after=None):
        n = len(tiles)
        sls = []
        for s_ in range(n + 1):
            if s_ < n:
                sl = xt_ctr[0] % 2
                xt_ctr[0] += 1
                sls.append(sl)
                stage1(tiles[s_][0], sl)
            if s_ >= 1:
                t = s_ - 1
                stage2(sls[t], tiles[t][1], tiles[t][2])
                if after is not None:
                    after(t)

    acc_banks = [2, 3, 4, 5]
    acc_ctr = [0]

    def next_bank():
        b = acc_banks[acc_ctr[0] % 4]
        acc_ctr[0] += 1
        return b

    DMA("sp", gpre, gpre_d, [], ["gpre"], K_MISC)
    DMA("pool", wck_c, w_in_v[:, :, C_C:C_C + 512], [], ["wckc"], K_WB[0])
    DMA("pool", wck_k[:, :, 0:64], w_in_v[:, :, C_KX:C_KX + 64], [], ["wckk0"], K_WB[1])
    DMA("pool", wck_k[:, :, 64:128], w_in_v[:, :, C_KX:C_KX + 64], [], ["wckk1"], K_WB[2])
    chk("p0a")
    hT_keys = [[("hT", kq, j) for kq in range(4)] for j in range(4)]
    hT_all = [k for ks in hT_keys for k in ks]
    HW = 256

    def hg_info(hg):
        hf = hg % 2
        hc = slice(hf * HW, (hf + 1) * HW)
        hkeys = hT_keys[hf * 2] + hT_keys[hf * 2 + 1]
        cols = slice(hg * HW, (hg + 1) * HW)
        return hc, hkeys, cols

    def mm_block(hg):
        hc, hkeys, cols = hg_info(hg)
        for blk in range(4):
            b = 2 + blk
            for kc in range(KC):
                MM(pbs[b][:, 0:HW], wck_c[:, kc, blk * 128:(blk + 1) * 128], hT[:, kc, hc], kc == 0, kc == KC - 1,
                   hkeys + ["wckc"], [("pb", b)])
        for kc in range(KC):
            MM(pbs[6][:, 0:HW], wck_k[:, kc, :], hT[:, kc, hc], kc == 0, kc == KC - 1,
               hkeys + ["wckk0", "wckk1"], [("pb", 6)])

    def norm_a(hg):
        for blk in range(4):
            b = 2 + blk
            CP("dve", csb[:, blk, 0:HW], pbs[b][:, 0:HW], [("pb", b)], [("csb", blk)])
            ACT(sqc[:, blk, 0:HW], pbs[b][:, 0:HW], AF.Square, [("pb", b)], [("sqc", blk)])
        CP("dve", kxsb[:, 0:HW], pbs[6][:, 0:HW], [("pb", 6)], ["kxsb"])
        for blk in range(4):
            MM(pbs[7][:, 0:HW], ones_f, sqc[:, blk, 0:HW], blk == 0, blk == 3, [("sqc", blk), "onesf"], [("pb", 7)])
        MM(pbs[6][:, 0:HW], blk64, kxsb[:, 0:HW], True, True, ["kxsb", "blk64"], [("pb", 6)])
        TT("dve", xcb[:, 0:HW], kxsb[:, 0:HW], pbs[6][:, 0:HW], ALU.subtract, ["kxsb", ("pb", 6)], ["xcb"])

    def norm_b(hg):
        hc, hkeys, cols = hg_info(hg)
        ACT(sdb[:, 0:HW], pbs[7][:, 0:HW], AF.Sqrt, [("pb", 7), "cst"], ["sq2b"], scale=1.0 / 512, bias=epsT)
        RCP(rsb[:, 0:HW], sdb[:, 0:HW], ["sq2b"], ["rsb"])
        for blk in range(4):
            STT(cTn[:, blk, cols], csb[:, blk, 0:HW], gkv[:, blk:blk + 1], rsb[:, 0:HW], ALU.mult, ALU.mult,
                [("csb", blk), "rsb", "cst"], [("cTn", hg // 2)])
        ACT(sq2b[:, 0:HW], xcb[:, 0:HW], AF.Square, ["xcb"], ["sq2b"])
        MM(pbs[7][:, 0:HW], blk64, sq2b[:, 0:HW], True, True, ["sq2b", "blk64"], [("pb", 7)])

    def norm_c(hg):
        hc, hkeys, cols = hg_info(hg)
        ACT(sdb[:, 0:HW], pbs[7][:, 0:HW], AF.Sqrt, [("pb", 7), "cst"], ["sq2b"], scale=1.0, bias=epsT)
        RCP(rsb[:, 0:HW], sdb[:, 0:HW], ["sq2b"], ["rsb"])
        TT("dve", knb[:, 0:HW], xcb[:, 0:HW], rsb[:, 0:HW], ALU.mult, ["xcb", "rsb"], ["xcb"])
        TS("dve", kixT[:, cols], knb[:, 0:HW], kxg, kxb, ALU.mult, ALU.add, ["xcb", "cst"], [("kixT", hg // 2)])

    def after0(t):
        if t % 2 == 1:
            hg = t // 2
            mm_block(hg)
            if hg >= 1:
                norm_b(hg - 1)
        elif t >= 2:
            hg = t // 2 - 1
            if hg >= 1:
                norm_c(hg - 1)
            norm_a(hg)

    ktiles = []
    for t in range(16):
        hf = (t // 2) % 2
        j = hf * 2 + (t % 2)
        ktiles.append((xk[t * 128:(t + 1) * 128, :], hT[:, :, j * 128:(j + 1) * 128], hT_keys[j]))
    run_tiles(ktiles, after0)
    norm_c(6)
    norm_a(7)
    norm_b(7)
    norm_c(7)
    S.barrier(); chk("p0")

    cTn_all = [("cTn", kg) for kg in range(4)]
    kix_all = [("kixT", kg) for kg in range(4)]

    for g in range(2):
        if g > 0:
            DMA("sp", gpre, gpre_d, [], ["gpre"], K_MISC)
        otiles = []
        for j in range(4):
            t0 = g * GT + j * 128
            otiles.append((xo[t0:t0 + 128, :], hT[:, :, j * 128:(j + 1) * 128], hT_keys[j]))
        otiles.append((xh[g], hTh, [("hTh", kq) for kq in range(4)]))
        run_tiles(otiles)
        hTh_all = [("hTh", kq) for kq in range(4)]
        S.barrier(); chk("g%d_p1" % g)

        blocks = []
        for b_ in range(8):
            blocks.append(("qi", b_, C_QI + b_ * 128))
        for h in range(16):
            blocks.append(("q", h, C_Q + h * 128))
        for h in range(16):
            blocks.append(("ga", h, C_GA + h * 128))
        for b_ in range(16):
            blocks.append(("pi", b_, C_PI + b_ * 128))
        for b_ in range(16):
            blocks.append(("gb", b_, C_GB + b_ * 128))
        DMA("pool", wbx, w_in_v[:, :, C_WX:C_WX + 16], [], ["wbx"], K_MISC_SW)
        for j in range(4):
            for kc in range(KC):
                MM(pbs[7][:, 0:16], hT[:, kc, j * 128:(j + 1) * 128], wbx[:, kc, :], kc == 0, kc == KC - 1,
                   hT_keys[j] + ["wbx"], [("pb", 7)])
            ACT(wix_sb[:, j, :], pbs[7][:, 0:16], AF.Copy, [("pb", 7)], [("wix", j)], scale=WIX_SCALE)
        for bi, (kind, idx, c0) in enumerate(blocks):
            sl = bi % 3
            DMA("pool", wb[sl], w_in_v[:, :, c0:c0 + 128], [], [("wb", sl)], K_WB[sl])
            b = next_bank()
            for kc in range(KC):
                MM(pbs[b][:], wb[sl][:, kc, :], hT[:, kc, :], kc == 0, kc == KC - 1,
                   hT_all + [("wb", sl)], [("pb", b)])
            if kind == "qi":
                ACT(qiT[:, idx, :], pbs[b][:], AF.Copy, [("pb", b)], [("qiT", idx)], scale=IDX_SCALE)
            elif kind == "q":
                ACT(qT[:, idx, :], pbs[b][:], AF.Copy, [("pb", b)], [("qT", idx)], scale=ATTN_SCALE)
            elif kind == "ga":
                ACT(yin[:, idx, :], pbs[b][:], AF.Silu, [("pb", b)], [("yin", idx)])
            elif kind == "gb":
                ACT(yin[:, 16 + idx, :], pbs[b][:], AF.Silu, [("pb", b)], [("yin", 16 + idx)])
            else:
                for kc in range(KC):
                    MM(pbs[6][:, 0:16], wb[sl][:, kc, :], hTh[:, kc, 112:128], kc == 0, kc == KC - 1,
                       hTh_all + [("wb", sl)], [("pb", 6)])
                ps = bi % 3
                p = ptmp[ps]
                ta = ptmp[(ps + 1) % 3]
                tb = ptmp[(ps + 2) % 3]
                pk = ("ptmp", ps)
                tak = ("ptmp", (ps + 1) % 3)
                tbk = ("ptmp", (ps + 2) % 3)
                CP("act", p[:, 0:16], pbs[6][:, 0:16], [("pb", 6)], [pk])
                CP("act", p[:, 16:528], pbs[b][:], [("pb", b)], [pk])
                gp = idx // 4
                nlev = gp + 1
                win = 2 ** nlev
                src, srck = p, pk
                dsts = [(ta, tak), (tb, tbk)]
                for lev in range(nlev):
                    sh = 2 ** lev
                    dst, dstk = dsts[lev % 2]
                    lo_ = 2 ** (lev + 1) - 1
                    TT("dve", dst[:, lo_:528], src[:, lo_:528], src[:, lo_ - sh:528 - sh], ALU.add,
                       [srck], [dstk])
                    src, srck = dst, dstk
                TS("dve", src[:, 16:528], src[:, 16:528], 1.0 / win, None, ALU.mult, None, [srck], [srck])
                TT("dve", src[:, 16:32], src[:, 16:32], pfix[:, g, gp, :], ALU.mult, [srck, "cst"], [srck])
                TT("dve", mixT[:, idx, :], src[:, 16:528], p[:, 16:528], ALU.subtract, [srck, pk],
                   [("mixT", idx)])
        S.barrier(); chk("g%d_p2" % g)

        DMA("pool", wpool, w_pool_v, [], ["wpool"], K_MISC_SW)
        for gp in range(4):
            for dj in range(4):
                b = next_bank()
                for cc in range(4):
                    MM(pbs[b][:], wpool[:, gp * 4 + cc, dj * 128:(dj + 1) * 128], mixT[:, gp * 4 + cc, :],
                       cc == 0, cc == 3, ["wpool", ("mixT", gp * 4 + cc)], [("pb", b)])
                blk = gp * 4 + dj
                STT(yin[:, 16 + blk, :], pbs[b][:], pscale[:, blk:blk + 1], yin[:, 16 + blk, :],
                    ALU.mult, ALU.mult, [("pb", b), ("yin", 16 + blk), "cst"], [("yin", 16 + blk)])
        S.barrier(); chk("g%d_p3" % g)

        DMA("sp", kchunk, kchunk_d, [], ["kchunk"], K_MISC)
        nks = [8 + 4 * g + j + 1 for j in range(4)]
        S.add("pool", lambda e: e.memset(kixAB[0][64:128, :], 0.0), [], [("kixAB", 0, 1)])
        S.add("pool", lambda e: e.memset(kixAB[1][0:64, :], 0.0), [], [("kixAB", 1, 0)])
        CP("pool", kixAB[0][0:64, :], kixT[0:64, :], kix_all, [("kixAB", 0, 0)])
        CP("pool", kixAB[1][64:128, :], kixT[64:128, :], kix_all, [("kixAB", 1, 1)])
        kixAB_keys = [("kixAB", 0, 0), ("kixAB", 0, 1), ("kixAB", 1, 0), ("kixAB", 1, 1)]
        for j in range(4):
            ncol = nks[j] * 128
            qi_ = 4 * g + j
            TS("dve", negb[j][:, 0:ncol], kchunk[:, 0:ncol], qchunk[:, qi_:qi_ + 1], -BIG, ALU.is_gt, ALU.mult,
               ["kchunk", "cst"], [("negb", j)])
        rctr = [0]
        abctr = [0]

        def emit_scores(j):
            ncol = nks[j] * 128
            dg = diag[j % 2]
            for h in range(16):
                TS("pool", dg[:, h, :], ident_bf, wix_sb[:, j, h:h + 1], 1.0, ALU.mult, ALU.mult,
                   ["identb", ("wix", j)], [("diag", j % 2, h)])
            for s0 in range(0, ncol, 512):
                wd = min(512, ncol - s0)
                ab = 6 + abctr[0] % 2
                abctr[0] += 1
                slots = {}

                def dots(h):
                    b = next_bank()
                    MM(pbs[b][:, 0:wd], qiT[:, h // 2, j * 128:(j + 1) * 128], kixAB[h % 2][:, s0:s0 + wd],
                       True, True, [("qiT", h // 2)] + kixAB_keys, [("pb", b)])
                    rs_ = rctr[0] % 4
                    rctr[0] += 1
                    slots[h] = rs_
                    if j < 2 and h % 2 == 1:
                        TS("dve", rbuf[rs_][:, 0:wd], pbs[b][:, 0:wd], 0.0, None, ALU.max, None,
                           [("pb", b)], [("rbuf", rs_)])
                    else:
                        ACT(rbuf[rs_][:, 0:wd], pbs[b][:, 0:wd], AF.Relu, [("pb", b)], [("rbuf", rs_)])

                dots(0)
                dots(1)
                for h in range(16):
                    if h + 2 < 16:
                        dots(h + 2)
                    rs_ = slots[h]
                    MM(pbs[ab][:, 0:wd], dg[:, h, :], rbuf[rs_][:, 0:wd], h == 0, False,
                       [("diag", j % 2, h), ("rbuf", rs_)], [("pb", ab)])
                MM(pbs[ab][:, 0:wd], ident_bf, negb[j][:, s0:s0 + wd], False, True,
                   ["identb", ("negb", j)], [("pb", ab)])
                CP("act", acc[j][:, s0:s0 + wd], pbs[ab][:, 0:wd], [("pb", ab)], [("acc", j)])

        def emit_bisect(js):
            for j in js:
                S.add("dve", lambda e, j=j: e.memset(cand[:, j:j + 1], 0.0), [], [("cand", j)])
            for it in range(BIS_N):
                step = BIS_R * (2.0 ** (-it))
                last = it == BIS_N - 1
                for j in js:
                    ncol = nks[j] * 128
                    TS("dve", junk[:, 0:ncol], acc[j][:, 0:ncol], cand[:, j:j + 1], 0.0, ALU.is_ge, ALU.add,
                       [("acc", j), ("cand", j)], ["junk", ("cnt", j)], accum=cnt[:, j:j + 1])
                    TS("dve", tflag[:, j:j + 1], cnt[:, j:j + 1], TOPK - 0.5, step, ALU.is_ge, ALU.mult,
                       [("cnt", j)], [("tflag", j)])
                    STT(cand[:, j:j + 1], tflag[:, j:j + 1], (-step if last else -0.5 * step), cand[:, j:j + 1],
                        ALU.add, ALU.add, [("tflag", j), ("cand", j)], [("cand", j)])

        def emit_masks(js):
            for j in js:
                ncol = nks[j] * 128
                ms = j % 2
                TS("dve", maskbf[ms][:, 0:ncol], acc[j][:, 0:ncol], cand[:, j:j + 1], MASK_OFF, ALU.is_ge, ALU.mult,
                   [("acc", j), ("cand", j)], [("maskbf", ms)])
                for k8 in range(0, nks[j], 8):
                    n8 = min(8, nks[j] - k8)
                    b = tr_banks[tr_ctr[0] % 2]
                    tr_ctr[0] += 1
                    pt = pbT(b)
                    for i in range(n8):
                        kt = k8 + i
                        TR(pt[:, i, :], maskbf[ms][:, kt * 128:(kt + 1) * 128], ident_bf,
                           [("maskbf", ms), "identb"], [("pb", b)])
                    CP("act", maskT[:, k8:k8 + n8, j, :], pt[:, 0:n8, :], [("pb", b)], [("maskT", j)])

        emit_scores(0)
        emit_scores(1)
        emit_bisect([0, 1])
        emit_scores(2)
        emit_scores(3)
        emit_masks([0, 1])
        emit_bisect([2, 3])
        emit_masks([2, 3])
        S.barrier(); chk("g%d_p4" % g)

        nkmax = 8 + 4 * g + 4
        mask_all = [("maskT", j) for j in range(4)]
        items = [(h, kt) for h in range(16) for kt in range(nkmax)]
        LA = 2
        SB = [0, 1, 2]
        OB = [3, 6]
        kvb = [0]

        def emit_k(h):
            sl = h % 2
            DMA("pool", wuk[sl], w_uk[h].rearrange("(cc p) d -> p cc d", p=128), [], [("wuk", sl)], K_UK[sl])
            for s0 in range(0, nkmax * 128, 512):
                b = 4 + kvb[0] % 2
                kvb[0] += 1
                for cc in range(4):
                    MM(pbs[b][:], wuk[sl][:, cc, :], cTn[:, cc, s0:s0 + 512], cc == 0, cc == 3,
                       [("wuk", sl)] + cTn_all, [("pb", b)])
                CP("act", kTh[sl][:, s0:s0 + 512], pbs[b][:], [("pb", b)], [("kTh", sl, s0 // 512)])

        def emit_v4(hgp):
            sl = hgp % 2
            for hh in range(4):
                DMA("pool", wuv4[sl][:, :, hh * 128:(hh + 1) * 128],
                    w_uv[4 * hgp + hh].rearrange("(cc p) d -> p cc d", p=128),
                    [], [("wuv4", sl, hh)], K_UV4[sl][hh])
            for kt in range(nkmax):
                b = 4 + kvb[0] % 2
                kvb[0] += 1
                for cc in range(4):
                    MM(pbs[b][:], cTn[:, cc, kt * 128:(kt + 1) * 128], wuv4[sl][:, cc, :],
                       cc == 0, cc == 3, [("wuv4", sl, hh_) for hh_ in range(4)] + cTn_all, [("pb", b)])
                eng = "dve" if kt % 2 == 0 else "act"
                CP(eng, vS4[sl][:, kt, :], pbs[b][:], [("pb", b)], [("vS4", sl, kt // 4)])

        def emit_qk(i):
            h, kt = items[i]
            sl = h % 2
            jmin = max(0, kt - 8 - 4 * g)
            c0 = jmin * 128
            sb_ = SB[i % 3]
            es = i % 3
            MM(pbs[sb_][:, c0:GT], kTh[sl][:, kt * 128:(kt + 1) * 128], qT[:, h, c0:GT], True, False,
               [("kTh", sl, kt // 4), ("qT", h)], [("pb", sb_)])
            MM(pbs[sb_][:, c0:GT], ident_bf, maskT[:, kt, jmin:4, :].rearrange("p j t -> p (j t)"), False, True,
               ["identb"] + mask_all, [("pb", sb_)])
            ACT(eT[es][:, c0:GT], pbs[sb_][:, c0:GT], AF.Exp, [("pb", sb_), "cst"], [("eT", es)], bias=negoff)

        def emit_pv(i):
            h, kt = items[i]
            sl = h % 2
            jmin = max(0, kt - 8 - 4 * g)
            c0 = jmin * 128
            es = i % 3
            ob = OB[h % 2]
            vsl = (h // 4) % 2
            MM(pbs[ob][:, c0:GT], vS4[vsl][:, kt, (h % 4) * 128:(h % 4 + 1) * 128], eT[es][:, c0:GT],
               kt == 0, kt == nkmax - 1, [("vS4", vsl, kt // 4), ("eT", es)], [("pb", ob)])
            MM(pbs[7][:, c0:GT], ones_bf, eT[es][:, c0:GT], kt == 0, kt == nkmax - 1,
               ["onesb", ("eT", es)], [("pb", 7)])
            if kt == nkmax - 1:
                os_ = h % 2
                RCP(rcb[os_], pbs[7][:], [("pb", 7)], [("rcb", os_)])
                TT("dve", otmp[os_], pbs[ob][:], rcb[os_], ALU.mult, [("pb", ob), ("rcb", os_)], [("otmp", os_)])
                TT("pool", yin[:, h, :], otmp[os_], yin[:, h, :], ALU.mult,
                   [("otmp", os_), ("yin", h)], [("yin", h)])

        emit_k(0)
        emit_v4(0)
        for i in range(LA):
            emit_qk(i)
        for i in range(len(items)):
            h, kt = items[i]
            if kt == 0 and h + 1 < 16:
                emit_k(h + 1)
            if kt == 0 and h % 4 == 1 and h // 4 + 1 < 4:
                emit_v4(h // 4 + 1)
            if i + LA < len(items):
                emit_qk(i + LA)
            emit_pv(i)
        S.barrier(); chk("g%d_p5" % g)

        yin_all = [("yin", i) for i in range(32)]
        def ssq_mm(db):
            qs = db % 2
            for j in range(4):
                MM(pbs[6][:, j:j + 1], sqb[qs][:, j * 128:(j + 1) * 128], ones_bf[:, 0:1],
                   db == 0 and j == 0, db == 31 and j == 3, [("sqb", qs), "onesb"], [("pb", 6)])

        for db in range(32):
            sl = db % 3
            DMA("pool", wb6[sl], w_out_v[:, :, db * 128:(db + 1) * 128], [], [("wb6", sl)], K_WB[sl])
            b = next_bank()
            for ec in range(KC):
                MM(pbs[b][:], wb6[sl][:, ec, :], yin[:, ec, :], ec == 0, ec == KC - 1,
                   yin_all + [("wb6", sl)], [("pb", b)])
            if db > 0:
                ssq_mm(db - 1)
            qs = db % 2
            ACT(sqb[qs], pbs[b][:], AF.Square, [("pb", b)], [("sqb", qs)])
            TS("dve", yg[:, db, :], pbs[b][:], gpost[:, db:db + 1], None, ALU.mult, None,
               [("pb", b), "cst"], [("yg", db)])
        ssq_mm(31)
        ACT(sdo, pbs[6][:, 0:4], AF.Sqrt, [("pb", 6), "cst"], ["sdo"], scale=1.0 / D, bias=epsT)
        RCP(rstdo, sdo, ["sdo"], ["rstdo"])
        yg_all = [("yg", db) for db in range(32)]
        for j in range(4):
            sl = xt_ctr[0] % 2
            xt_ctr[0] += 1
            t0 = g * GT + j * 128
            DMA("sp", xt[sl], xo[t0:t0 + 128, :], [], [("xt", sl)], K_XT[sl])
            for d4 in range(8):
                b = tr_banks[tr_ctr[0] % 2]
                tr_ctr[0] += 1
                for i in range(4):
                    db = d4 * 4 + i
                    TR(pbs[b][:, i * 128:(i + 1) * 128], yg[:, db, j * 128:(j + 1) * 128], ident_f,
                       [("yg", db), "identf"], [("pb", b)])
                STT(xt[sl][:, d4 * 512:(d4 + 1) * 512], pbs[b][:], rstdo[:, j:j + 1],
                    xt[sl][:, d4 * 512:(d4 + 1) * 512], ALU.mult, ALU.add,
                    [("pb", b), "rstdo", ("xt", sl)], [("xt", sl)])
            DMA("sp", yout[t0:t0 + 128, :], xt[sl], [("xt", sl)], [("yout", g, j)], K_ST[sl])
        S.barrier(); chk("g%d_p6" % g)

    S.emit(st)
    st.close()
    return nc


_NC_CACHE = {}
_ONLY_MAPS = False


def _get_nc():
    if "nc" not in _NC_CACHE:
        _NC_CACHE["nc"] = build_nc()
    return _NC_CACHE["nc"]


def kernel(x, pre_norm, w_in, kv_norm, w_uk, w_uv, idx_k_norm_g, idx_k_norm_b, w_pool, pool_scale, w_out,
           post_norm):
    f32 = np.float32
    x = np.asarray(x, f32)
    B = x.shape[0]
    w_in0 = np.ascontiguousarray(np.asarray(w_in, f32)[0])
    w_out0 = np.ascontiguousarray(np.asarray(w_out, f32)[0])
    w_uk0 = np.ascontiguousarray(np.asarray(w_uk, f32)[0])
    w_uv0 = np.ascontiguousarray(np.asarray(w_uv, f32)[0])
    w_pool0 = np.ascontiguousarray(np.asarray(w_pool, f32)[0])
    gpre_b = np.ascontiguousarray(np.broadcast_to(np.asarray(pre_norm, f32)[0][None, :], (128, D)))
    kchunk_b = np.ascontiguousarray(np.broadcast_to((np.arange(SEQ) // 64).astype(f32)[None, :], (128, SEQ)))
    ident_f = np.eye(128, dtype=f32)
    ident_bf = np.eye(128, dtype=f32).astype(ml_dtypes.bfloat16)
    blk64 = np.zeros((128, 128), f32)
    blk64[0:64, 0:64] = 1.0 / 64
    blk64[64:128, 64:128] = 1.0 / 64
    cst0 = np.zeros((128, 512), f32)
    cst0[:, 0:4] = np.asarray(kv_norm, f32)[0].reshape(4, 128).T
    kg = np.asarray(idx_k_norm_g, f32)[0]
    kb = np.asarray(idx_k_norm_b, f32)[0]
    cst0[:, 4] = np.concatenate([kg, kg])
    cst0[:, 5] = np.concatenate([kb, kb])
    cst0[:, 6] = EPS
    cst0[:, 7] = -MASK_OFF
    cst0[:, 8:24] = np.asarray(pool_scale, f32)[0].reshape(16, 128).T
    cst0[:, 24:56] = np.asarray(post_norm, f32)[0].reshape(32, 128).T
    wins = [2, 4, 8, 16]
    in_maps = []
    for b in range(B):
        for half in range(2):
            cst = cst0.copy()
            tok = half * 1024 + np.arange(1024)
            cst[:, 56:64] = (tok // 64).astype(f32).reshape(8, 128).T
            pf = np.ones((2, 4, 16), f32)
            if half == 0:
                for gi, wdw in enumerate(wins):
                    t = np.arange(16)
                    pf[0, gi, :] = wdw / np.minimum(t + 1, wdw)
            cst[:, 64:192] = pf.reshape(1, 128)
            xo = np.ascontiguousarray(x[b, half * 1024:(half + 1) * 1024])
            xh = np.zeros((2, 128, D), f32)
            if half == 1:
                xh[0] = x[b, 896:1024]
            xh[1] = xo[384:512]
            in_maps.append({
                "xk": np.ascontiguousarray(x[b]), "xo": xo, "xh": xh,
                "w_in": w_in0, "w_out": w_out0, "w_uk": w_uk0, "w_uv": w_uv0, "w_pool": w_pool0,
                "gpre_b": gpre_b, "kchunk_b": kchunk_b, "cst": cst, "ident_f": ident_f, "blk64": blk64,
                "ident_bf": ident_bf,
            })
    if _ONLY_MAPS:
        return in_maps
    nc = _get_nc()
    res = run_bass_kernel_spmd(nc, in_maps, core_ids=list(range(2 * B)))
    out = np.zeros((B, SEQ, D), f32)
    for b in range(B):
        for half in range(2):
            out[b, half * 1024:(half + 1) * 1024] = res.results[2 * b + half]["y"]
    return out
```

```python
from contextlib import ExitStack
import numpy as np
import ml_dtypes
import concourse.bass as bass
import concourse.mybir as mybir
from concourse.bass_utils import run_bass_kernel_spmd

F32 = mybir.dt.float32
BF16 = mybir.dt.bfloat16
ALU = mybir.AluOpType
AF = mybir.ActivationFunctionType

ENGS = ("pe", "act", "dve", "pool", "sp")
N_DMA_SEMS = 22

D = 4096
DIN = 9808
SEQ = 2048
KC = 32
GT = 512
C_Q, C_C, C_QI, C_KX, C_WX, C_GA, C_PI, C_GB = 0, 2048, 2560, 3584, 3648, 3664, 5712, 7760
EPS = 1e-6
ATTN_SCALE = 128 ** -0.5
IDX_SCALE = 64 ** -0.5
WIX_SCALE = 16 ** -0.5
BIG = 1.0e30
BIS_R = 16.0
BIS_N = 21
TOPK = 256
MASK_OFF = 100.0


class Op:
    __slots__ = ("eng", "fn", "deps", "dma", "sig", "sem", "val", "idx")

    def __init__(self, eng, fn, dma):
        self.eng = eng
        self.fn = fn
        self.deps = set()
        self.dma = dma
        self.sig = False
        self.sem = None
        self.val = 0


class Sched:
    def __init__(self, nc):
        self.nc = nc
        self.ops = []
        self.lastw = {}
        self.readers = {}
        self.fence = set()
        self.last_eng = {}
        self.last_dma = {}
        self.frozen = False

    def add(self, eng, fn, reads=(), writes=(), dma=None):
        if self.frozen:
            return None
        op = Op(eng, fn, dma)
        idx = len(self.ops)
        op.idx = idx
        if eng != "pe":
            extra = [("pbx", r[1]) for r in reads if isinstance(r, tuple) and r[0] == "pb"]
            if extra:
                writes = list(writes) + extra
        for r in reads:
            w = self.lastw.get(r)
            if w is not None:
                op.deps.add(w)
        for w_ in writes:
            w = self.lastw.get(w_)
            if w is not None:
                op.deps.add(w)
            for rd in self.readers.get(w_, ()):
                op.deps.add(rd)
        for r in reads:
            self.readers.setdefault(r, []).append(idx)
        for w_ in writes:
            self.lastw[w_] = idx
            self.readers[w_] = []
        op.deps |= self.fence
        op.deps.discard(idx)
        self.ops.append(op)
        if dma is None:
            self.last_eng[eng] = idx
        else:
            self.last_dma[dma] = idx
        return idx

    def barrier(self):
        self.fence = set(self.last_eng.values()) | set(self.last_dma.values())

    def emit(self, stack):
        nc = self.nc
        ops = self.ops

        def skip(dop, op):
            return dop.eng == "pe" and op.eng == "pe" and dop.dma is None and op.dma is None

        for op in ops:
            for d in op.deps:
                if not skip(ops[d], op):
                    ops[d].sig = True
        for op in ops:
            if op.dma is not None:
                op.sig = True
        esem = {e: stack.enter_context(nc.semaphore("s_" + e)) for e in ENGS}
        dsem = [stack.enter_context(nc.semaphore("d_%d" % i)) for i in range(N_DMA_SEMS)]
        ecount = {e: 0 for e in ENGS}
        dcount = [0] * N_DMA_SEMS
        for op in ops:
            if not op.sig:
                continue
            if op.dma is not None:
                dcount[op.dma] += 16
                op.sem = dsem[op.dma]
                op.val = dcount[op.dma]
            else:
                ecount[op.eng] += 1
                op.sem = esem[op.eng]
                op.val = ecount[op.eng]
        per_eng = {e: [] for e in ENGS}
        for op in ops:
            per_eng[op.eng].append(op)
        dfinal = list(dcount)
        block = stack.enter_context(nc.Block())

        def make_body(e):
            def body(eng):
                waited = {}
                for op in per_eng[e]:
                    need = {}
                    for d in op.deps:
                        dop = ops[d]
                        if skip(dop, op):
                            continue
                        k = id(dop.sem)
                        if k not in need or need[k][1] < dop.val:
                            need[k] = (dop.sem, dop.val)
                    for k, (sem, val) in need.items():
                        if waited.get(k, 0) >= val:
                            continue
                        eng.wait_ge(sem, val)
                        waited[k] = val
                    ins = op.fn(eng)
                    if op.sig:
                        ins.then_inc(op.sem, 16 if op.dma is not None else 1)
                if e == "sp":
                    for i in range(N_DMA_SEMS):
                        if dfinal[i] > 0:
                            eng.wait_ge(dsem[i], dfinal[i])
            return body

        block.tensor(make_body("pe"))
        block.scalar(make_body("act"))
        block.vector(make_body("dve"))
        block.gpsimd(make_body("pool"))
        block.sync(make_body("sp"))


class _Stop(Exception):
    pass


STOP = None


def build_nc():
    nc = bass.Bass("TRN2", target_bir_lowering=False)

    def chk(label):
        if STOP == label:
            S.frozen = True

    def din(name, shape, dt=F32):
        return nc.dram_tensor(name, list(shape), dt, kind="ExternalInput").ap()

    xk = din("xk", [SEQ, D])
    xo = din("xo", [1024, D])
    xh = din("xh", [2, 128, D])
    w_in = din("w_in", [D, DIN])
    w_out = din("w_out", [D, D])
    w_uk = din("w_uk", [16, 512, 128])
    w_uv = din("w_uv", [16, 512, 128])
    w_pool = din("w_pool", [4, 512, 512])
    gpre_d = din("gpre_b", [128, D])
    kchunk_d = din("kchunk_b", [128, SEQ])
    cst_d = din("cst", [128, 512])
    identf_d = din("ident_f", [128, 128])
    blk64_d = din("blk64", [128, 128])
    identb_d = din("ident_bf", [128, 128], BF16)
    yout = nc.dram_tensor("y", [1024, D], F32, kind="ExternalOutput").ap()

    w_in_v = w_in.rearrange("(kc p) e -> p kc e", p=128)
    w_out_v = w_out.rearrange("(kc p) e -> p kc e", p=128)
    w_pool_v = w_pool.rearrange("g (cc p) d -> p (g cc) d", p=128)

    st = ExitStack()
    st.enter_context(nc.allow_low_precision("bf16 matmul operands, fp32 accumulation"))
    ACOLS = 49152
    arena = st.enter_context(nc.sbuf_tensor("arena", [128, ACOLS], F32))
    pbs = [st.enter_context(nc.psum_tensor("pb%d" % i, [128, 512], F32)) for i in range(8)]
    S = Sched(nc)

    def fv(off, n):
        assert off + n <= ACOLS
        return arena[:, off:off + n]

    def bvw(off, ncols):
        assert off + ncols <= ACOLS
        return arena[:, off:off + ncols].bitcast(BF16)

    def pbT(i):
        return pbs[i][:].bitcast(BF16).rearrange("p (a b) -> p a b", b=128)

    cTn = bvw(0, 4096).rearrange("p (a b) -> p a b", b=SEQ)
    kixT = bvw(4096, 1024)
    o = 5120
    cst = fv(o, 512); o += 512
    ident_f = fv(o, 128); o += 128
    blk64 = fv(o, 128); o += 128
    ones_f = fv(o, 128); o += 128
    ident_bf = bvw(o, 64); o += 64
    ones_bf = bvw(o, 64); o += 64
    assert o <= 6144
    gkv = cst[:, 0:4]
    kxg = cst[:, 4:5]
    kxb = cst[:, 5:6]
    epsT = cst[:, 6:7]
    negoff = cst[:, 7:8]
    pscale = cst[:, 8:24]
    gpost = cst[:, 24:56]
    qchunk = cst[:, 56:64]
    pfix = cst[:, 64:192].rearrange("p (g a b) -> p g a b", g=2, a=4)
    ss2 = cst[:, 192:194]
    sd2 = cst[:, 194:196]
    rstd2 = cst[:, 196:198]
    wix_sb = cst[:, 200:264].rearrange("p (j h) -> p j h", h=16)
    cand = cst[:, 264:268]
    cnt = cst[:, 268:272]
    tflag = cst[:, 272:276]
    sdo = cst[:, 276:280]
    rstdo = cst[:, 280:284]

    YIN0, QT0, QIT0, HT0, HTH0, R10 = 6144, 14336, 18432, 20480, 28672, 30720
    yin = bvw(YIN0, 8192).rearrange("p (a b) -> p a b", b=GT)
    qT = bvw(QT0, 4096).rearrange("p (a b) -> p a b", b=GT)
    qiT = bvw(QIT0, 2048).rearrange("p (a b) -> p a b", b=GT)
    hT = bvw(HT0, 8192).rearrange("p (a b) -> p a b", b=GT)
    hTh = bvw(HTH0, 2048).rearrange("p (a b) -> p a b", b=128)
    xt = [fv(R10 + i * 4096, 4096) for i in range(2)]
    xsb = [bvw(R10 + 8192 + i * 2048, 2048) for i in range(2)]
    gpre = fv(R10 + 12288, 4096)
    wb = [bvw(R10 + i * 2048, 2048).rearrange("p (a b) -> p a b", b=128) for i in range(3)]
    ptmp = [fv(R10 + 6144 + i * 528, 528) for i in range(3)]
    wbx = bvw(R10 + 7744, 256).rearrange("p (a b) -> p a b", b=16)
    mixT = bvw(R10 + 8192, 4096).rearrange("p (a b) -> p a b", b=GT)
    wck_c = bvw(6144, 8192).rearrange("p (a b) -> p a b", b=512)
    wck_k = bvw(14336, 2048).rearrange("p (a b) -> p a b", b=128)
    csb = fv(16384, 2048).rearrange("p (a b) -> p a b", b=512)
    sqc = fv(18432, 2048).rearrange("p (a b) -> p a b", b=512)
    P0M = R10 + 16384
    kxsb = fv(P0M, 512)
    xcb = fv(P0M + 512, 512)
    sq2b = fv(P0M + 1024, 512)
    rsb = fv(P0M + 1536, 512)
    sdb = sq2b
    knb = xcb
    wpool = bvw(HT0, 4096).rearrange("p (a b) -> p a b", b=512)
    acc = [fv(R10 + j * 2048, 2048) for j in range(4)]
    negb = [bvw(R10 + 8192 + j * 1024, 1024) for j in range(4)]
    rbuf = [bvw(R10 + 12288 + i * 256, 256) for i in range(4)]
    junk = bvw(R10 + 13312, 1024)
    maskbf = [bvw(R10 + 14336 + i * 1024, 1024) for i in range(2)]
    kixAB = [bvw(R10 + 16384 + i * 1024, 1024) for i in range(2)]
    kchunk = fv(HTH0, 2048)
    maskT = bvw(HT0, 4096).rearrange("p (k j t) -> p k j t", k=16, j=4)
    diag = [bvw(HT0 + 4096 + i * 1024, 1024).rearrange("p (a b) -> p a b", b=128) for i in range(2)]
    kTh = [bvw(R10 + i * 1024, 1024) for i in range(2)]
    vS4 = [bvw(R10 + 2048 + i * 4096, 4096).rearrange("p (a b) -> p a b", b=512) for i in range(2)]
    wuk = [bvw(R10 + 10240 + i * 256, 256).rearrange("p (a b) -> p a b", b=128) for i in range(2)]
    wuv4 = [bvw(R10 + 10752 + i * 1024, 1024).rearrange("p (a b) -> p a b", b=512) for i in range(2)]
    eT = [bvw(R10 + 12800 + i * 256, 256) for i in range(3)]
    pT = [bvw(R10 + 13568 + i * 256, 256) for i in range(3)]
    rcb = [fv(R10 + 14336 + i * 512, 512) for i in range(2)]
    otmp = [fv(R10 + 15360 + i * 512, 512) for i in range(2)]
    yg = fv(QT0, 16384).rearrange("p (a b) -> p a b", b=GT)
    wb6 = [bvw(R10 + 8192 + i * 2048, 2048).rearrange("p (a b) -> p a b", b=128) for i in range(3)]
    sqb = [bvw(R10 + 14336 + i * 256, 256) for i in range(2)]

    def MM(out, lhsT, rhs, start, stop, r, w):
        S.add("pe", lambda e: e.matmul(out, lhsT, rhs, start=start, stop=stop), r, w)

    def TR(out, in_, ident, r, w):
        S.add("pe", lambda e: e.transpose(out, in_, ident), r, w)

    def ACT(out, in_, func, r, w, scale=None, bias=None, accum=None):
        kw = {}
        if scale is not None:
            kw["scale"] = scale
        if bias is not None:
            kw["bias"] = bias
        if accum is not None:
            kw["accum_out"] = accum
        S.add("act", lambda e: e.activation(out, in_, func, **kw), r, w)

    def TS(eng, out, in0, s1, s2, op0, op1, r, w, accum=None):
        if op1 is None:
            if accum is None:
                S.add(eng, lambda e: e.tensor_scalar(out, in0, s1, None, op0), r, w)
            else:
                raise ValueError
        else:
            if accum is None:
                S.add(eng, lambda e: e.tensor_scalar(out, in0, s1, s2, op0, op1), r, w)
            else:
                S.add(eng, lambda e: e.tensor_scalar(out, in0, s1, s2, op0, op1, accum_out=accum), r, w)

    def TT(eng, out, in0, in1, op, r, w):
        S.add(eng, lambda e: e.tensor_tensor(out, in0, in1, op), r, w)

    def STT(out, in0, scalar, in1, op0, op1, r, w):
        S.add("dve", lambda e: e.scalar_tensor_tensor(out, in0, scalar, in1, op0, op1), r, w)

    def RCP(out, in_, r, w):
        S.add("dve", lambda e: e.reciprocal(out, in_), r, w)

    def CP(eng, out, in_, r, w):
        if eng == "act":
            S.add("act", lambda e: e.copy(out, in_), r, w)
        else:
            S.add(eng, lambda e: e.tensor_copy(out, in_), r, w)

    def DMA(eng, out, in_, r, w, key):
        S.add(eng, lambda e: e.dma_start(out=out, in_=in_), r, w, dma=key)

    K_XT = [0, 1]
    K_WB = [2, 3, 4]
    K_ST = [5, 6]
    K_UK = [7, 8]
    K_UV = [9, 10]
    K_MISC = 11
    K_MISC_SW = 12
    K_UV4 = [[13, 14, 15, 16], [17, 18, 19, 20]]

    DMA("sp", cst, cst_d, [], ["cst"], K_MISC)
    DMA("sp", ident_f, identf_d, [], ["identf"], K_MISC)
    DMA("sp", blk64, blk64_d, [], ["blk64"], K_MISC)
    DMA("sp", ident_bf, identb_d, [], ["identb"], K_MISC)
    S.add("dve", lambda e: e.memset(ones_f, 1.0), [], ["onesf"])
    S.add("dve", lambda e: e.memset(ones_bf, 1.0), [], ["onesb"])
    S.barrier(); chk("setup")

    tr_banks = [0, 1]
    tr_ctr = [0]
    xt_ctr = [0]

    def stage1(src, sl):
        xs = xsb[sl]
        xk_ = ("xs", sl)
        DMA("sp", xt[sl], src, [], [("xt", sl)], K_XT[sl])
        ACT(xs, xt[sl], AF.Square, [("xt", sl)], [xk_, ("ss", sl)], accum=ss2[:, sl:sl + 1])
        ACT(sd2[:, sl:sl + 1], ss2[:, sl:sl + 1], AF.Sqrt, [("ss", sl), "cst"], [("sd", sl)],
            scale=1.0 / D, bias=epsT)
        RCP(rstd2[:, sl:sl + 1], sd2[:, sl:sl + 1], [("sd", sl)], [("rstd", sl)])
        STT(xs, xt[sl], rstd2[:, sl:sl + 1], gpre, ALU.mult, ALU.mult,
            [("xt", sl), ("rstd", sl), "gpre"], [xk_])

    def stage2(sl, dst, dst_keys):
        xs = xsb[sl]
        xk_ = ("xs", sl)
        for kq in range(4):
            b = tr_banks[tr_ctr[0] % 2]
            tr_ctr[0] += 1
            pt = pbT(b)
            for i in range(8):
                kc = kq * 8 + i
                TR(pt[:, i, :], xs[:, kc * 128:(kc + 1) * 128], ident_bf, [xk_, "identb"], [("pb", b)])
            eng = "act" if kq % 2 == 0 else "dve"
            CP(eng, dst[:, kq * 8:(kq + 1) * 8, :], pt, [("pb", b)], [dst_keys[kq]])

    def run_tiles(tiles, after=None):
        n = len(tiles)
        sls = []
        for s_ in range(n + 1):
            if s_ < n:
                sl = xt_ctr[0] % 2
                xt_ctr[0] += 1
                sls.append(sl)
                stage1(tiles[s_][0], sl)
            if s_ >= 1:
                t = s_ - 1
                stage2(sls[t], tiles[t][1], tiles[t][2])
                if after is not None:
                    after(t)

    acc_banks = [2, 3, 4, 5]
    acc_ctr = [0]

    def next_bank():
        b = acc_banks[acc_ctr[0] % 4]
        acc_ctr[0] += 1
        return b

    DMA("sp", gpre, gpre_d, [], ["gpre"], K_MISC)
    DMA("pool", wck_c, w_in_v[:, :, C_C:C_C + 512], [], ["wckc"], K_WB[0])
    DMA("pool", wck_k[:, :, 0:64], w_in_v[:, :, C_KX:C_KX + 64], [], ["wckk0"], K_WB[1])
    DMA("pool", wck_k[:, :, 64:128], w_in_v[:, :, C_KX:C_KX + 64], [], ["wckk1"], K_WB[2])
    chk("p0a")
    hT_keys = [[("hT", kq, j) for kq in range(4)] for j in range(4)]
    hT_all = [k for ks in hT_keys for k in ks]
    HW = 256

    def hg_info(hg):
        hf = hg % 2
        hc = slice(hf * HW, (hf + 1) * HW)
        hkeys = hT_keys[hf * 2] + hT_keys[hf * 2 + 1]
        cols = slice(hg * HW, (hg + 1) * HW)
        return hc, hkeys, cols

    def mm_block(hg):
        hc, hkeys, cols = hg_info(hg)
        for blk in range(4):
            b = 2 + blk
            for kc in range(KC):
                MM(pbs[b][:, 0:HW], wck_c[:, kc, blk * 128:(blk + 1) * 128], hT[:, kc, hc], kc == 0, kc == KC - 1,
                   hkeys + ["wckc"], [("pb", b)])
        for kc in range(KC):
            MM(pbs[6][:, 0:HW], wck_k[:, kc, :], hT[:, kc, hc], kc == 0, kc == KC - 1,
               hkeys + ["wckk0", "wckk1"], [("pb", 6)])

    def norm_a(hg):
        for blk in range(4):
            b = 2 + blk
            CP("dve", csb[:, blk, 0:HW], pbs[b][:, 0:HW], [("pb", b)], [("csb", blk)])
            ACT(sqc[:, blk, 0:HW], pbs[b][:, 0:HW], AF.Square, [("pb", b)], [("sqc", blk)])
        CP("dve", kxsb[:, 0:HW], pbs[6][:, 0:HW], [("pb", 6)], ["kxsb"])
        for blk in range(4):
            MM(pbs[7][:, 0:HW], ones_f, sqc[:, blk, 0:HW], blk == 0, blk == 3, [("sqc", blk), "onesf"], [("pb", 7)])
        MM(pbs[6][:, 0:HW], blk64, kxsb[:, 0:HW], True, True, ["kxsb", "blk64"], [("pb", 6)])
        TT("dve", xcb[:, 0:HW], kxsb[:, 0:HW], pbs[6][:, 0:HW], ALU.subtract, ["kxsb", ("pb", 6)], ["xcb"])

    def norm_b(hg):
        hc, hkeys, cols = hg_info(hg)
        ACT(sdb[:, 0:HW], pbs[7][:, 0:HW], AF.Sqrt, [("pb", 7), "cst"], ["sq2b"], scale=1.0 / 512, bias=epsT)
        RCP(rsb[:, 0:HW], sdb[:, 0:HW], ["sq2b"], ["rsb"])
        for blk in range(4):
            STT(cTn[:, blk, cols], csb[:, blk, 0:HW], gkv[:, blk:blk + 1], rsb[:, 0:HW], ALU.mult, ALU.mult,
                [("csb", blk), "rsb", "cst"], [("cTn", hg // 2)])
        ACT(sq2b[:, 0:HW], xcb[:, 0:HW], AF.Square, ["xcb"], ["sq2b"])
        MM(pbs[7][:, 0:HW], blk64, sq2b[:, 0:HW], True, True, ["sq2b", "blk64"], [("pb", 7)])

    def norm_c(hg):
        hc, hkeys, cols = hg_info(hg)
        ACT(sdb[:, 0:HW], pbs[7][:, 0:HW], AF.Sqrt, [("pb", 7), "cst"], ["sq2b"], scale=1.0, bias=epsT)
        RCP(rsb[:, 0:HW], sdb[:, 0:HW], ["sq2b"], ["rsb"])
        TT("dve", knb[:, 0:HW], xcb[:, 0:HW], rsb[:, 0:HW], ALU.mult, ["xcb", "rsb"], ["xcb"])
        TS("dve", kixT[:, cols], knb[:, 0:HW], kxg, kxb, ALU.mult, ALU.add, ["xcb", "cst"], [("kixT", hg // 2)])

    def after0(t):
        if t % 2 == 1:
            hg = t // 2
            mm_block(hg)
            if hg >= 1:
                norm_b(hg - 1)
        elif t >= 2:
            hg = t // 2 - 1
            if hg >= 1:
                norm_c(hg - 1)
            norm_a(hg)

    ktiles = []
    for t in range(16):
        hf = (t // 2) % 2
        j = hf * 2 + (t % 2)
        ktiles.append((xk[t * 128:(t + 1) * 128, :], hT[:, :, j * 128:(j + 1) * 128], hT_keys[j]))
    run_tiles(ktiles, after0)
    norm_c(6)
    norm_a(7)
    norm_b(7)
    norm_c(7)
    S.barrier(); chk("p0")

    cTn_all = [("cTn", kg) for kg in range(4)]
    kix_all = [("kixT", kg) for kg in range(4)]

    for g in range(2):
        if g > 0:
            DMA("sp", gpre, gpre_d, [], ["gpre"], K_MISC)
        otiles = []
        for j in range(4):
            t0 = g * GT + j * 128
            otiles.append((xo[t0:t0 + 128, :], hT[:, :, j * 128:(j + 1) * 128], hT_keys[j]))
        otiles.append((xh[g], hTh, [("hTh", kq) for kq in range(4)]))
        run_tiles(otiles)
        hTh_all = [("hTh", kq) for kq in range(4)]
        S.barrier(); chk("g%d_p1" % g)

        blocks = []
        for b_ in range(8):
            blocks.append(("qi", b_, C_QI + b_ * 128))
        for h in range(16):
            blocks.append(("q", h, C_Q + h * 128))
        for h in range(16):
            blocks.append(("ga", h, C_GA + h * 128))
        for b_ in range(16):
            blocks.append(("pi", b_, C_PI + b_ * 128))
        for b_ in range(16):
            blocks.append(("gb", b_, C_GB + b_ * 128))
        DMA("pool", wbx, w_in_v[:, :, C_WX:C_WX + 16], [], ["wbx"], K_MISC_SW)
        for j in range(4):
            for kc in range(KC):
                MM(pbs[7][:, 0:16], hT[:, kc, j * 128:(j + 1) * 128], wbx[:, kc, :], kc == 0, kc == KC - 1,
                   hT_keys[j] + ["wbx"], [("pb", 7)])
            ACT(wix_sb[:, j, :], pbs[7][:, 0:16], AF.Copy, [("pb", 7)], [("wix", j)], scale=WIX_SCALE)
        for bi, (kind, idx, c0) in enumerate(blocks):
            sl = bi % 3
            DMA("pool", wb[sl], w_in_v[:, :, c0:c0 + 128], [], [("wb", sl)], K_WB[sl])
            b = next_bank()
            for kc in range(KC):
                MM(pbs[b][:], wb[sl][:, kc, :], hT[:, kc, :], kc == 0, kc == KC - 1,
                   hT_all + [("wb", sl)], [("pb", b)])
            if kind == "qi":
                ACT(qiT[:, idx, :], pbs[b][:], AF.Copy, [("pb", b)], [("qiT", idx)], scale=IDX_SCALE)
            elif kind == "q":
                ACT(qT[:, idx, :], pbs[b][:], AF.Copy, [("pb", b)], [("qT", idx)], scale=ATTN_SCALE)
            elif kind == "ga":
                ACT(yin[:, idx, :], pbs[b][:], AF.Silu, [("pb", b)], [("yin", idx)])
            elif kind == "gb":
                ACT(yin[:, 16 + idx, :], pbs[b][:], AF.Silu, [("pb", b)], [("yin", 16 + idx)])
            else:
                for kc in range(KC):
                    MM(pbs[6][:, 0:16], wb[sl][:, kc, :], hTh[:, kc, 112:128], kc == 0, kc == KC - 1,
                       hTh_all + [("wb", sl)], [("pb", 6)])
                ps = bi % 3
                p = ptmp[ps]
                ta = ptmp[(ps + 1) % 3]
                tb = ptmp[(ps + 2) % 3]
                pk = ("ptmp", ps)
                tak = ("ptmp", (ps + 1) % 3)
                tbk = ("ptmp", (ps + 2) % 3)
                CP("act", p[:, 0:16], pbs[6][:, 0:16], [("pb", 6)], [pk])
                CP("act", p[:, 16:528], pbs[b][:], [("pb", b)], [pk])
                gp = idx // 4
                nlev = gp + 1
                win = 2 ** nlev
                src, srck = p, pk
                dsts = [(ta, tak), (tb, tbk)]
                for lev in range(nlev):
                    sh = 2 ** lev
                    dst, dstk = dsts[lev % 2]
                    lo_ = 2 ** (lev + 1) - 1
                    TT("dve", dst[:, lo_:528], src[:, lo_:528], src[:, lo_ - sh:528 - sh], ALU.add,
                       [srck], [dstk])
                    src, srck = dst, dstk
                TS("dve", src[:, 16:528], src[:, 16:528], 1.0 / win, None, ALU.mult, None, [srck], [srck])
                TT("dve", src[:, 16:32], src[:, 16:32], pfix[:, g, gp, :], ALU.mult, [srck, "cst"], [srck])
                TT("dve", mixT[:, idx, :], src[:, 16:528], p[:, 16:528], ALU.subtract, [srck, pk],
                   [("mixT", idx)])
        S.barrier(); chk("g%d_p2" % g)

        DMA("pool", wpool, w_pool_v, [], ["wpool"], K_MISC_SW)
        for gp in range(4):
            for dj in range(4):
                b = next_bank()
                for cc in range(4):
                    MM(pbs[b][:], wpool[:, gp * 4 + cc, dj * 128:(dj + 1) * 128], mixT[:, gp * 4 + cc, :],
                       cc == 0, cc == 3, ["wpool", ("mixT", gp * 4 + cc)], [("pb", b)])
                blk = gp * 4 + dj
                STT(yin[:, 16 + blk, :], pbs[b][:], pscale[:, blk:blk + 1], yin[:, 16 + blk, :],
                    ALU.mult, ALU.mult, [("pb", b), ("yin", 16 + blk), "cst"], [("yin", 16 + blk)])
        S.barrier(); chk("g%d_p3" % g)

        DMA("sp", kchunk, kchunk_d, [], ["kchunk"], K_MISC)
        nks = [8 + 4 * g + j + 1 for j in range(4)]
        S.add("pool", lambda e: e.memset(kixAB[0][64:128, :], 0.0), [], [("kixAB", 0, 1)])
        S.add("pool", lambda e: e.memset(kixAB[1][0:64, :], 0.0), [], [("kixAB", 1, 0)])
        CP("pool", kixAB[0][0:64, :], kixT[0:64, :], kix_all, [("kixAB", 0, 0)])
        CP("pool", kixAB[1][64:128, :], kixT[64:128, :], kix_all, [("kixAB", 1, 1)])
        kixAB_keys = [("kixAB", 0, 0), ("kixAB", 0, 1), ("kixAB", 1, 0), ("kixAB", 1, 1)]
        for j in range(4):
            ncol = nks[j] * 128
            qi_ = 4 * g + j
            TS("dve", negb[j][:, 0:ncol], kchunk[:, 0:ncol], qchunk[:, qi_:qi_ + 1], -BIG, ALU.is_gt, ALU.mult,
               ["kchunk", "cst"], [("negb", j)])
        rctr = [0]
        abctr = [0]

        def emit_scores(j):
            ncol = nks[j] * 128
            dg = diag[j % 2]
            for h in range(16):
                TS("pool", dg[:, h, :], ident_bf, wix_sb[:, j, h:h + 1], 1.0, ALU.mult, ALU.mult,
                   ["identb", ("wix", j)], [("diag", j % 2, h)])
            for s0 in range(0, ncol, 512):
                wd = min(512, ncol - s0)
                ab = 6 + abctr[0] % 2
                abctr[0] += 1
                slots = {}

                def dots(h):
                    b = next_bank()
                    MM(pbs[b][:, 0:wd], qiT[:, h // 2, j * 128:(j + 1) * 128], kixAB[h % 2][:, s0:s0 + wd],
                       True, True, [("qiT", h // 2)] + kixAB_keys, [("pb", b)])
                    rs_ = rctr[0] % 4
                    rctr[0] += 1
                    slots[h] = rs_
                    if j < 2 and h % 2 == 1:
                        TS("dve", rbuf[rs_][:, 0:wd], pbs[b][:, 0:wd], 0.0, None, ALU.max, None,
                           [("pb", b)], [("rbuf", rs_)])
                    else:
                        ACT(rbuf[rs_][:, 0:wd], pbs[b][:, 0:wd], AF.Relu, [("pb", b)], [("rbuf", rs_)])

                dots(0)
                dots(1)
                for h in range(16):
                    if h + 2 < 16:
                        dots(h + 2)
                    rs_ = slots[h]
                    MM(pbs[ab][:, 0:wd], dg[:, h, :], rbuf[rs_][:, 0:wd], h == 0, False,
                       [("diag", j % 2, h), ("rbuf", rs_)], [("pb", ab)])
                MM(pbs[ab][:, 0:wd], ident_bf, negb[j][:, s0:s0 + wd], False, True,
                   ["identb", ("negb", j)], [("pb", ab)])
                CP("act", acc[j][:, s0:s0 + wd], pbs[ab][:, 0:wd], [("pb", ab)], [("acc", j)])

        def emit_bisect(js):
            for j in js:
                S.add("dve", lambda e, j=j: e.memset(cand[:, j:j + 1], 0.0), [], [("cand", j)])
            for it in range(BIS_N):
                step = BIS_R * (2.0 ** (-it))
                last = it == BIS_N - 1
                for j in js:
                    ncol = nks[j] * 128
                    TS("dve", junk[:, 0:ncol], acc[j][:, 0:ncol], cand[:, j:j + 1], 0.0, ALU.is_ge, ALU.add,
                       [("acc", j), ("cand", j)], ["junk", ("cnt", j)], accum=cnt[:, j:j + 1])
                    TS("dve", tflag[:, j:j + 1], cnt[:, j:j + 1], TOPK - 0.5, step, ALU.is_ge, ALU.mult,
                       [("cnt", j)], [("tflag", j)])
                    STT(cand[:, j:j + 1], tflag[:, j:j + 1], (-step if last else -0.5 * step), cand[:, j:j + 1],
                        ALU.add, ALU.add, [("tflag", j), ("cand", j)], [("cand", j)])

        def emit_masks(js):
            for j in js:
                ncol = nks[j] * 128
                ms = j % 2
                TS("dve", maskbf[ms][:, 0:ncol], acc[j][:, 0:ncol], cand[:, j:j + 1], MASK_OFF, ALU.is_ge, ALU.mult,
                   [("acc", j), ("cand", j)], [("maskbf", ms)])
                for k8 in range(0, nks[j], 8):
                    n8 = min(8, nks[j] - k8)
                    b = tr_banks[tr_ctr[0] % 2]
                    tr_ctr[0] += 1
                    pt = pbT(b)
                    for i in range(n8):
                        kt = k8 + i
                        TR(pt[:, i, :], maskbf[ms][:, kt * 128:(kt + 1) * 128], ident_bf,
                           [("maskbf", ms), "identb"], [("pb", b)])
                    CP("act", maskT[:, k8:k8 + n8, j, :], pt[:, 0:n8, :], [("pb", b)], [("maskT", j)])

        emit_scores(0)
        emit_scores(1)
        emit_bisect([0, 1])
        emit_scores(2)
        emit_scores(3)
        emit_masks([0, 1])
        emit_bisect([2, 3])
        emit_masks([2, 3])
        S.barrier(); chk("g%d_p4" % g)

        nkmax = 8 + 4 * g + 4
        mask_all = [("maskT", j) for j in range(4)]
        items = [(h, kt) for h in range(16) for kt in range(nkmax)]
        LA = 2
        SB = [0, 1, 2]
        OB = [3, 6]
        kvb = [0]

        def dma_k(h):
            sl = h % 2
            DMA("pool", wuk[sl], w_uk[h].rearrange("(cc p) d -> p cc d", p=128), [], [("wuk", sl)], K_UK[sl])

        def mm_k(h):
            sl = h % 2
            for s0 in range(0, nkmax * 128, 512):
                b = 4 + kvb[0] % 2
                kvb[0] += 1
                for cc in range(4):
                    MM(pbs[b][:], wuk[sl][:, cc, :], cTn[:, cc, s0:s0 + 512], cc == 0, cc == 3,
                       [("wuk", sl)] + cTn_all, [("pb", b)])
                CP("act", kTh[sl][:, s0:s0 + 512], pbs[b][:], [("pb", b)], [("kTh", sl, s0 // 512)])

        def dma_v4(hgp):
            sl = hgp % 2
            for hh in range(4):
                DMA("pool", wuv4[sl][:, :, hh * 128:(hh + 1) * 128],
                    w_uv[4 * hgp + hh].rearrange("(cc p) d -> p cc d", p=128),
                    [], [("wuv4", sl, hh)], K_UV4[sl][hh])

        def mm_v4(hgp):
            sl = hgp % 2
            for kt in range(nkmax):
                b = 4 + kvb[0] % 2
                kvb[0] += 1
                for cc in range(4):
                    MM(pbs[b][:], cTn[:, cc, kt * 128:(kt + 1) * 128], wuv4[sl][:, cc, :],
                       cc == 0, cc == 3, [("wuv4", sl, hh_) for hh_ in range(4)] + cTn_all, [("pb", b)])
                eng = "dve" if kt % 2 == 0 else "act"
                CP(eng, vS4[sl][:, kt, :], pbs[b][:], [("pb", b)], [("vS4", sl, kt // 4)])

        def emit_qk(i):
            h, kt = items[i]
            sl = h % 2
            jmin = max(0, kt - 8 - 4 * g)
            c0 = jmin * 128
            sb_ = SB[i % 3]
            es = i % 3
            MM(pbs[sb_][:, c0:GT], kTh[sl][:, kt * 128:(kt + 1) * 128], qT[:, h, c0:GT], True, False,
               [("kTh", sl, kt // 4), ("qT", h)], [("pb", sb_)])
            MM(pbs[sb_][:, c0:GT], ident_bf, maskT[:, kt, jmin:4, :].rearrange("p j t -> p (j t)"), False, True,
               ["identb"] + mask_all, [("pb", sb_)])
            ACT(eT[es][:, c0:GT], pbs[sb_][:, c0:GT], AF.Exp, [("pb", sb_), "cst"], [("eT", es)], bias=negoff)

        def emit_pv(i):
            h, kt = items[i]
            sl = h % 2
            jmin = max(0, kt - 8 - 4 * g)
            c0 = jmin * 128
            es = i % 3
            ob = OB[h % 2]
            vsl = (h // 4) % 2
            MM(pbs[ob][:, c0:GT], vS4[vsl][:, kt, (h % 4) * 128:(h % 4 + 1) * 128], eT[es][:, c0:GT],
               kt == 0, kt == nkmax - 1, [("vS4", vsl, kt // 4), ("eT", es)], [("pb", ob)])
            MM(pbs[7][:, c0:GT], ones_bf, eT[es][:, c0:GT], kt == 0, kt == nkmax - 1,
               ["onesb", ("eT", es)], [("pb", 7)])
            if kt == nkmax - 1:
                os_ = h % 2
                RCP(rcb[os_], pbs[7][:], [("pb", 7)], [("rcb", os_)])
                TT("dve", otmp[os_], pbs[ob][:], rcb[os_], ALU.mult, [("pb", ob), ("rcb", os_)], [("otmp", os_)])
                TT("pool", yin[:, h, :], otmp[os_], yin[:, h, :], ALU.mult,
                   [("otmp", os_), ("yin", h)], [("yin", h)])

        dma_k(0)
        dma_k(1)
        dma_v4(0)
        mm_k(0)
        mm_v4(0)
        for i in range(LA):
            emit_qk(i)
        for i in range(len(items)):
            h, kt = items[i]
            if kt == 0:
                if h + 2 < 16:
                    dma_k(h + 2)
                if h % 4 == 0 and h // 4 + 1 < 4:
                    dma_v4(h // 4 + 1)
                if h + 1 < 16:
                    mm_k(h + 1)
                if h % 4 == 1 and h // 4 + 1 < 4:
                    mm_v4(h // 4 + 1)
            if i + LA < len(items):
                emit_qk(i + LA)
            emit_pv(i)
        S.barrier(); chk("g%d_p5" % g)

        yin_all = [("yin", i) for i in range(32)]
        def ssq_mm(db):
            qs = db % 2
            for j in range(4):
                MM(pbs[6][:, j:j + 1], sqb[qs][:, j * 128:(j + 1) * 128], ones_bf[:, 0:1],
                   db == 0 and j == 0, db == 31 and j == 3, [("sqb", qs), "onesb"], [("pb", 6)])

        for db in range(32):
            sl = db % 3
            DMA("pool", wb6[sl], w_out_v[:, :, db * 128:(db + 1) * 128], [], [("wb6", sl)], K_WB[sl])
            b = next_bank()
            for ec in range(KC):
                MM(pbs[b][:], wb6[sl][:, ec, :], yin[:, ec, :], ec == 0, ec == KC - 1,
                   yin_all + [("wb6", sl)], [("pb", b)])
            if db > 0:
                ssq_mm(db - 1)
            qs = db % 2
            ACT(sqb[qs], pbs[b][:], AF.Square, [("pb", b)], [("sqb", qs)])
            TS("dve", yg[:, db, :], pbs[b][:], gpost[:, db:db + 1], None, ALU.mult, None,
               [("pb", b), "cst"], [("yg", db)])
        ssq_mm(31)
        ACT(sdo, pbs[6][:, 0:4], AF.Sqrt, [("pb", 6), "cst"], ["sdo"], scale=1.0 / D, bias=epsT)
        RCP(rstdo, sdo, ["sdo"], ["rstdo"])
        yg_all = [("yg", db) for db in range(32)]
        for j in range(4):
            sl = xt_ctr[0] % 2
            xt_ctr[0] += 1
            t0 = g * GT + j * 128
            DMA("sp", xt[sl], xo[t0:t0 + 128, :], [], [("xt", sl)], K_XT[sl])
            for d4 in range(8):
                b = tr_banks[tr_ctr[0] % 2]
                tr_ctr[0] += 1
                for i in range(4):
                    db = d4 * 4 + i
                    TR(pbs[b][:, i * 128:(i + 1) * 128], yg[:, db, j * 128:(j + 1) * 128], ident_f,
                       [("yg", db), "identf"], [("pb", b)])
                STT(xt[sl][:, d4 * 512:(d4 + 1) * 512], pbs[b][:], rstdo[:, j:j + 1],
                    xt[sl][:, d4 * 512:(d4 + 1) * 512], ALU.mult, ALU.add,
                    [("pb", b), "rstdo", ("xt", sl)], [("xt", sl)])
            DMA("sp", yout[t0:t0 + 128, :], xt[sl], [("xt", sl)], [("yout", g, j)], K_ST[sl])
        S.barrier(); chk("g%d_p6" % g)

    S.emit(st)
    st.close()
    return nc


_NC_CACHE = {}
_ONLY_MAPS = False


def _get_nc():
    if "nc" not in _NC_CACHE:
        _NC_CACHE["nc"] = build_nc()
    return _NC_CACHE["nc"]


def kernel(x, pre_norm, w_in, kv_norm, w_uk, w_uv, idx_k_norm_g, idx_k_norm_b, w_pool, pool_scale, w_out,
           post_norm):
    f32 = np.float32
    x = np.asarray(x, f32)
    B = x.shape[0]
    w_in0 = np.ascontiguousarray(np.asarray(w_in, f32)[0])
    w_out0 = np.ascontiguousarray(np.asarray(w_out, f32)[0])
    w_uk0 = np.ascontiguousarray(np.asarray(w_uk, f32)[0])
    w_uv0 = np.ascontiguousarray(np.asarray(w_uv, f32)[0])
    w_pool0 = np.ascontiguousarray(np.asarray(w_pool, f32)[0])
    gpre_b = np.ascontiguousarray(np.broadcast_to(np.asarray(pre_norm, f32)[0][None, :], (128, D)))
    kchunk_b = np.ascontiguousarray(np.broadcast_to((np.arange(SEQ) // 64).astype(f32)[None, :], (128, SEQ)))
    ident_f = np.eye(128, dtype=f32)
    ident_bf = np.eye(128, dtype=f32).astype(ml_dtypes.bfloat16)
    blk64 = np.zeros((128, 128), f32)
    blk64[0:64, 0:64] = 1.0 / 64
    blk64[64:128, 64:128] = 1.0 / 64
    cst0 = np.zeros((128, 512), f32)
    cst0[:, 0:4] = np.asarray(kv_norm, f32)[0].reshape(4, 128).T
    kg = np.asarray(idx_k_norm_g, f32)[0]
    kb = np.asarray(idx_k_norm_b, f32)[0]
    cst0[:, 4] = np.concatenate([kg, kg])
    cst0[:, 5] = np.concatenate([kb, kb])
    cst0[:, 6] = EPS
    cst0[:, 7] = -MASK_OFF
    cst0[:, 8:24] = np.asarray(pool_scale, f32)[0].reshape(16, 128).T
    cst0[:, 24:56] = np.asarray(post_norm, f32)[0].reshape(32, 128).T
    wins = [2, 4, 8, 16]
    in_maps = []
    for b in range(B):
        for half in range(2):
            cst = cst0.copy()
            tok = half * 1024 + np.arange(1024)
            cst[:, 56:64] = (tok // 64).astype(f32).reshape(8, 128).T
            pf = np.ones((2, 4, 16), f32)
            if half == 0:
                for gi, wdw in enumerate(wins):
                    t = np.arange(16)
                    pf[0, gi, :] = wdw / np.minimum(t + 1, wdw)
            cst[:, 64:192] = pf.reshape(1, 128)
            xo = np.ascontiguousarray(x[b, half * 1024:(half + 1) * 1024])
            xh = np.zeros((2, 128, D), f32)
            if half == 1:
                xh[0] = x[b, 896:1024]
            xh[1] = xo[384:512]
            in_maps.append({
                "xk": np.ascontiguousarray(x[b]), "xo": xo, "xh": xh,
                "w_in": w_in0, "w_out": w_out0, "w_uk": w_uk0, "w_uv": w_uv0, "w_pool": w_pool0,
                "gpre_b": gpre_b, "kchunk_b": kchunk_b, "cst": cst, "ident_f": ident_f, "blk64": blk64,
                "ident_bf": ident_bf,
            })
    if _ONLY_MAPS:
        return in_maps
    nc = _get_nc()
    res = run_bass_kernel_spmd(nc, in_maps, core_ids=list(range(2 * B)))
    out = np.zeros((B, SEQ, D), f32)
    for b in range(B):
        for half in range(2):
            out[b, half * 1024:(half + 1) * 1024] = res.results[2 * b + half]["y"]
    return out
```

```python
from contextlib import ExitStack
import numpy as np
import ml_dtypes
import concourse.bass as bass
import concourse.mybir as mybir
from concourse.bass_utils import run_bass_kernel_spmd

F32 = mybir.dt.float32
BF16 = mybir.dt.bfloat16
ALU = mybir.AluOpType
AF = mybir.ActivationFunctionType

ENGS = ("pe", "act", "dve", "pool", "sp")
N_DMA_SEMS = 22

D = 4096
DIN = 9808
SEQ = 2048
KC = 32
GT = 512
C_Q, C_C, C_QI, C_KX, C_WX, C_GA, C_PI, C_GB = 0, 2048, 2560, 3584, 3648, 3664, 5712, 7760
EPS = 1e-6
ATTN_SCALE = 128 ** -0.5
IDX_SCALE = 64 ** -0.5
WIX_SCALE = 16 ** -0.5
BIG = 1.0e30
BIS_R = 16.0
BIS_N = 21
TOPK = 256
MASK_OFF = 100.0


class Op:
    __slots__ = ("eng", "fn", "deps", "dma", "sig", "sem", "val", "idx")

    def __init__(self, eng, fn, dma):
        self.eng = eng
        self.fn = fn
        self.deps = set()
        self.dma = dma
        self.sig = False
        self.sem = None
        self.val = 0


class Sched:
    def __init__(self, nc):
        self.nc = nc
        self.ops = []
        self.lastw = {}
        self.readers = {}
        self.fence = set()
        self.last_eng = {}
        self.last_dma = {}
        self.frozen = False

    def add(self, eng, fn, reads=(), writes=(), dma=None):
        if self.frozen:
            return None
        op = Op(eng, fn, dma)
        idx = len(self.ops)
        op.idx = idx
        if eng != "pe":
            extra = [("pbx", r[1]) for r in reads if isinstance(r, tuple) and r[0] == "pb"]
            if extra:
                writes = list(writes) + extra
        for r in reads:
            w = self.lastw.get(r)
            if w is not None:
                op.deps.add(w)
        for w_ in writes:
            w = self.lastw.get(w_)
            if w is not None:
                op.deps.add(w)
            for rd in self.readers.get(w_, ()):
                op.deps.add(rd)
        for r in reads:
            self.readers.setdefault(r, []).append(idx)
        for w_ in writes:
            self.lastw[w_] = idx
            self.readers[w_] = []
        op.deps |= self.fence
        op.deps.discard(idx)
        self.ops.append(op)
        if dma is None:
            self.last_eng[eng] = idx
        else:
            self.last_dma[dma] = idx
        return idx

    def barrier(self):
        self.fence = set(self.last_eng.values()) | set(self.last_dma.values())

    def emit(self, stack):
        nc = self.nc
        ops = self.ops

        def skip(dop, op):
            return dop.eng == "pe" and op.eng == "pe" and dop.dma is None and op.dma is None

        for op in ops:
            for d in op.deps:
                if not skip(ops[d], op):
                    ops[d].sig = True
        for op in ops:
            if op.dma is not None:
                op.sig = True
        esem = {e: stack.enter_context(nc.semaphore("s_" + e)) for e in ENGS}
        dsem = [stack.enter_context(nc.semaphore("d_%d" % i)) for i in range(N_DMA_SEMS)]
        ecount = {e: 0 for e in ENGS}
        dcount = [0] * N_DMA_SEMS
        for op in ops:
            if not op.sig:
                continue
            if op.dma is not None:
                dcount[op.dma] += 16
                op.sem = dsem[op.dma]
                op.val = dcount[op.dma]
            else:
                ecount[op.eng] += 1
                op.sem = esem[op.eng]
                op.val = ecount[op.eng]
        per_eng = {e: [] for e in ENGS}
        for op in ops:
            per_eng[op.eng].append(op)
        dfinal = list(dcount)
        block = stack.enter_context(nc.Block())

        def make_body(e):
            def body(eng):
                waited = {}
                for op in per_eng[e]:
                    need = {}
                    for d in op.deps:
                        dop = ops[d]
                        if skip(dop, op):
                            continue
                        k = id(dop.sem)
                        if k not in need or need[k][1] < dop.val:
                            need[k] = (dop.sem, dop.val)
                    for k, (sem, val) in need.items():
                        if waited.get(k, 0) >= val:
                            continue
                        eng.wait_ge(sem, val)
                        waited[k] = val
                    ins = op.fn(eng)
                    if op.sig:
                        ins.then_inc(op.sem, 16 if op.dma is not None else 1)
                if e == "sp":
                    for i in range(N_DMA_SEMS):
                        if dfinal[i] > 0:
                            eng.wait_ge(dsem[i], dfinal[i])
            return body

        block.tensor(make_body("pe"))
        block.scalar(make_body("act"))
        block.vector(make_body("dve"))
        block.gpsimd(make_body("pool"))
        block.sync(make_body("sp"))


class _Stop(Exception):
    pass


STOP = None


def build_nc():
    nc = bass.Bass("TRN2", target_bir_lowering=False)

    def chk(label):
        if STOP == label:
            S.frozen = True

    def din(name, shape, dt=F32):
        return nc.dram_tensor(name, list(shape), dt, kind="ExternalInput").ap()

    xk = din("xk", [SEQ, D])
    xo = din("xo", [1024, D])
    xh = din("xh", [2, 128, D])
    w_in = din("w_in", [D, DIN])
    w_out = din("w_out", [D, D])
    w_uk = din("w_uk", [16, 512, 128])
    w_uv = din("w_uv", [16, 512, 128])
    w_pool = din("w_pool", [4, 512, 512])
    gpre_d = din("gpre_b", [128, D])
    kchunk_d = din("kchunk_b", [128, SEQ])
    cst_d = din("cst", [128, 512])
    identf_d = din("ident_f", [128, 128])
    blk64_d = din("blk64", [128, 128])
    identb_d = din("ident_bf", [128, 128], BF16)
    yout = nc.dram_tensor("y", [1024, D], F32, kind="ExternalOutput").ap()

    w_in_v = w_in.rearrange("(kc p) e -> p kc e", p=128)
    w_out_v = w_out.rearrange("(kc p) e -> p kc e", p=128)
    w_pool_v = w_pool.rearrange("g (cc p) d -> p (g cc) d", p=128)

    st = ExitStack()
    st.enter_context(nc.allow_low_precision("bf16 matmul operands, fp32 accumulation"))
    ACOLS = 49152
    arena = st.enter_context(nc.sbuf_tensor("arena", [128, ACOLS], F32))
    pbs = [st.enter_context(nc.psum_tensor("pb%d" % i, [128, 512], F32)) for i in range(8)]
    S = Sched(nc)

    def fv(off, n):
        assert off + n <= ACOLS
        return arena[:, off:off + n]

    def bvw(off, ncols):
        assert off + ncols <= ACOLS
        return arena[:, off:off + ncols].bitcast(BF16)

    def pbT(i):
        return pbs[i][:].bitcast(BF16).rearrange("p (a b) -> p a b", b=128)

    cTn = bvw(0, 4096).rearrange("p (a b) -> p a b", b=SEQ)
    kixT = bvw(4096, 1024)
    o = 5120
    cst = fv(o, 512); o += 512
    ident_f = fv(o, 128); o += 128
    blk64 = fv(o, 128); o += 128
    ones_f = fv(o, 128); o += 128
    ident_bf = bvw(o, 64); o += 64
    ones_bf = bvw(o, 64); o += 64
    assert o <= 6144
    gkv = cst[:, 0:4]
    kxg = cst[:, 4:5]
    kxb = cst[:, 5:6]
    epsT = cst[:, 6:7]
    negoff = cst[:, 7:8]
    pscale = cst[:, 8:24]
    gpost = cst[:, 24:56]
    qchunk = cst[:, 56:64]
    pfix = cst[:, 64:192].rearrange("p (g a b) -> p g a b", g=2, a=4)
    ss2 = cst[:, 192:194]
    sd2 = cst[:, 194:196]
    rstd2 = cst[:, 196:198]
    wix_sb = cst[:, 200:264].rearrange("p (j h) -> p j h", h=16)
    cand = cst[:, 264:268]
    cnt = cst[:, 268:272]
    tflag = cst[:, 272:276]
    sdo = cst[:, 276:280]
    rstdo = cst[:, 280:284]

    YIN0, QT0, QIT0, HT0, HTH0, R10 = 6144, 14336, 18432, 20480, 28672, 30720
    yin = bvw(YIN0, 8192).rearrange("p (a b) -> p a b", b=GT)
    qT = bvw(QT0, 4096).rearrange("p (a b) -> p a b", b=GT)
    qiT = bvw(QIT0, 2048).rearrange("p (a b) -> p a b", b=GT)
    hT = bvw(HT0, 8192).rearrange("p (a b) -> p a b", b=GT)
    hTh = bvw(HTH0, 2048).rearrange("p (a b) -> p a b", b=128)
    xt = [fv(R10 + i * 4096, 4096) for i in range(2)]
    xsb = [bvw(R10 + 8192 + i * 2048, 2048) for i in range(2)]
    gpre = fv(R10 + 12288, 4096)
    wb = [bvw(R10 + i * 2048, 2048).rearrange("p (a b) -> p a b", b=128) for i in range(3)]
    ptmp = [fv(R10 + 6144 + i * 528, 528) for i in range(3)]
    wbx = bvw(R10 + 7744, 256).rearrange("p (a b) -> p a b", b=16)
    mixT = bvw(R10 + 8192, 4096).rearrange("p (a b) -> p a b", b=GT)
    wck_c = bvw(6144, 8192).rearrange("p (a b) -> p a b", b=512)
    wck_k = bvw(14336, 2048).rearrange("p (a b) -> p a b", b=128)
    csb = fv(16384, 2048).rearrange("p (a b) -> p a b", b=512)
    sqc = fv(18432, 2048).rearrange("p (a b) -> p a b", b=512)
    P0M = R10 + 16384
    kxsb = fv(P0M, 512)
    xcb = fv(P0M + 512, 512)
    sq2b = fv(P0M + 1024, 512)
    rsb = fv(P0M + 1536, 512)
    sdb = sq2b
    knb = xcb
    wpool = bvw(R10 + 12288, 4096).rearrange("p (a b) -> p a b", b=512)
    acc = [fv(R10 + j * 2048, 2048) for j in range(4)]
    negb = [bvw(R10 + 8192 + j * 1024, 1024) for j in range(4)]
    rbuf = [bvw(R10 + 12288 + i * 256, 256) for i in range(4)]
    junk = bvw(R10 + 13312, 1024)
    maskbf = [bvw(R10 + 14336 + i * 1024, 1024) for i in range(2)]
    kixAB = [bvw(HTH0 + i * 1024, 1024) for i in range(2)]
    kchunk = fv(R10 + 16384, 2048)
    maskT = bvw(HT0, 4096).rearrange("p (k j t) -> p k j t", k=16, j=4)
    diag = [bvw(HT0 + 4096 + i * 1024, 1024).rearrange("p (a b) -> p a b", b=128) for i in range(2)]
    kTh = [bvw(R10 + i * 1024, 1024) for i in range(2)]
    vS4 = [bvw(R10 + 2048 + i * 4096, 4096).rearrange("p (a b) -> p a b", b=512) for i in range(2)]
    wuk = [bvw(R10 + 10240 + i * 256, 256).rearrange("p (a b) -> p a b", b=128) for i in range(2)]
    wuv4 = [bvw(R10 + 10752 + i * 1024, 1024).rearrange("p (a b) -> p a b", b=512) for i in range(2)]
    eT = [bvw(R10 + 12800 + i * 256, 256) for i in range(3)]
    pT = [bvw(R10 + 13568 + i * 256, 256) for i in range(3)]
    rcb = [fv(R10 + 14336 + i * 512, 512) for i in range(2)]
    otmp = [fv(R10 + 15360 + i * 512, 512) for i in range(2)]
    yg = fv(QT0, 16384).rearrange("p (a b) -> p a b", b=GT)
    wb6 = [bvw(R10 + 8192 + i * 2048, 2048).rearrange("p (a b) -> p a b", b=128) for i in range(3)]
    sqb = [bvw(R10 + 14336 + i * 256, 256) for i in range(2)]

    def MM(out, lhsT, rhs, start, stop, r, w):
        S.add("pe", lambda e: e.matmul(out, lhsT, rhs, start=start, stop=stop), r, w)

    def TR(out, in_, ident, r, w):
        S.add("pe", lambda e: e.transpose(out, in_, ident), r, w)

    def ACT(out, in_, func, r, w, scale=None, bias=None, accum=None):
        kw = {}
        if scale is not None:
            kw["scale"] = scale
        if bias is not None:
            kw["bias"] = bias
        if accum is not None:
            kw["accum_out"] = accum
        S.add("act", lambda e: e.activation(out, in_, func, **kw), r, w)

    def TS(eng, out, in0, s1, s2, op0, op1, r, w, accum=None):
        if op1 is None:
            if accum is None:
                S.add(eng, lambda e: e.tensor_scalar(out, in0, s1, None, op0), r, w)
            else:
                raise ValueError
        else:
            if accum is None:
                S.add(eng, lambda e: e.tensor_scalar(out, in0, s1, s2, op0, op1), r, w)
            else:
                S.add(eng, lambda e: e.tensor_scalar(out, in0, s1, s2, op0, op1, accum_out=accum), r, w)

    def TT(eng, out, in0, in1, op, r, w):
        S.add(eng, lambda e: e.tensor_tensor(out, in0, in1, op), r, w)

    def STT(out, in0, scalar, in1, op0, op1, r, w):
        S.add("dve", lambda e: e.scalar_tensor_tensor(out, in0, scalar, in1, op0, op1), r, w)

    def RCP(out, in_, r, w):
        S.add("dve", lambda e: e.reciprocal(out, in_), r, w)

    def CP(eng, out, in_, r, w):
        if eng == "act":
            S.add("act", lambda e: e.copy(out, in_), r, w)
        else:
            S.add(eng, lambda e: e.tensor_copy(out, in_), r, w)

    def DMA(eng, out, in_, r, w, key):
        S.add(eng, lambda e: e.dma_start(out=out, in_=in_), r, w, dma=key)

    K_XT = [0, 1]
    K_WB = [2, 3, 4]
    K_ST = [5, 6]
    K_UK = [7, 8]
    K_UV = [9, 10]
    K_MISC = 11
    K_MISC_SW = 12
    K_UV4 = [[13, 14, 15, 16], [17, 18, 19, 20]]

    DMA("sp", cst, cst_d, [], ["cst"], K_MISC)
    DMA("sp", ident_f, identf_d, [], ["identf"], K_MISC)
    DMA("sp", blk64, blk64_d, [], ["blk64"], K_MISC)
    DMA("sp", ident_bf, identb_d, [], ["identb"], K_MISC)
    S.add("dve", lambda e: e.memset(ones_f, 1.0), [], ["onesf"])
    S.add("dve", lambda e: e.memset(ones_bf, 1.0), [], ["onesb"])
    S.barrier(); chk("setup")

    tr_banks = [0, 1]
    tr_ctr = [0]
    xt_ctr = [0]

    def stage1(src, sl):
        xs = xsb[sl]
        xk_ = ("xs", sl)
        DMA("sp", xt[sl], src, [], [("xt", sl)], K_XT[sl])
        ACT(xs, xt[sl], AF.Square, [("xt", sl)], [xk_, ("ss", sl)], accum=ss2[:, sl:sl + 1])
        ACT(sd2[:, sl:sl + 1], ss2[:, sl:sl + 1], AF.Sqrt, [("ss", sl), "cst"], [("sd", sl)],
            scale=1.0 / D, bias=epsT)
        RCP(rstd2[:, sl:sl + 1], sd2[:, sl:sl + 1], [("sd", sl)], [("rstd", sl)])
        STT(xs, xt[sl], rstd2[:, sl:sl + 1], gpre, ALU.mult, ALU.mult,
            [("xt", sl), ("rstd", sl), "gpre"], [xk_])

    def stage2(sl, dst, dst_keys):
        xs = xsb[sl]
        xk_ = ("xs", sl)
        for kq in range(4):
            b = tr_banks[tr_ctr[0] % 2]
            tr_ctr[0] += 1
            pt = pbT(b)
            for i in range(8):
                kc = kq * 8 + i
                TR(pt[:, i, :], xs[:, kc * 128:(kc + 1) * 128], ident_bf, [xk_, "identb"], [("pb", b)])
            eng = "act" if kq % 2 == 0 else "dve"
            CP(eng, dst[:, kq * 8:(kq + 1) * 8, :], pt, [("pb", b)], [dst_keys[kq]])

    def run_tiles(tiles, after=None):
        n = len(tiles)
        sls = []
        for s_ in range(n + 1):
            if s_ < n:
                sl = xt_ctr[0] % 2
                xt_ctr[0] += 1
                sls.append(sl)
                stage1(tiles[s_][0], sl)
            if s_ >= 1:
                t = s_ - 1
                stage2(sls[t], tiles[t][1], tiles[t][2])
                if after is not None:
                    after(t)

    acc_banks = [2, 3, 4, 5]
    acc_ctr = [0]

    def next_bank():
        b = acc_banks[acc_ctr[0] % 4]
        acc_ctr[0] += 1
        return b

    DMA("sp", gpre, gpre_d, [], ["gpre"], K_MISC)
    DMA("pool", wck_c, w_in_v[:, :, C_C:C_C + 512], [], ["wckc"], K_WB[0])
    DMA("pool", wck_k[:, :, 0:64], w_in_v[:, :, C_KX:C_KX + 64], [], ["wckk0"], K_WB[1])
    DMA("pool", wck_k[:, :, 64:128], w_in_v[:, :, C_KX:C_KX + 64], [], ["wckk1"], K_WB[2])
    chk("p0a")
    hT_keys = [[("hT", kq, j) for kq in range(4)] for j in range(4)]
    hT_all = [k for ks in hT_keys for k in ks]
    HW = 256

    def hg_info(hg):
        hf = hg % 2
        hc = slice(hf * HW, (hf + 1) * HW)
        hkeys = hT_keys[hf * 2] + hT_keys[hf * 2 + 1]
        cols = slice(hg * HW, (hg + 1) * HW)
        return hc, hkeys, cols

    def mm_block(hg):
        hc, hkeys, cols = hg_info(hg)
        for blk in range(4):
            b = 2 + blk
            for kc in range(KC):
                MM(pbs[b][:, 0:HW], wck_c[:, kc, blk * 128:(blk + 1) * 128], hT[:, kc, hc], kc == 0, kc == KC - 1,
                   hkeys + ["wckc"], [("pb", b)])
        for kc in range(KC):
            MM(pbs[6][:, 0:HW], wck_k[:, kc, :], hT[:, kc, hc], kc == 0, kc == KC - 1,
               hkeys + ["wckk0", "wckk1"], [("pb", 6)])

    def norm_a(hg):
        for blk in range(4):
            b = 2 + blk
            CP("dve", csb[:, blk, 0:HW], pbs[b][:, 0:HW], [("pb", b)], [("csb", blk)])
            ACT(sqc[:, blk, 0:HW], pbs[b][:, 0:HW], AF.Square, [("pb", b)], [("sqc", blk)])
        CP("dve", kxsb[:, 0:HW], pbs[6][:, 0:HW], [("pb", 6)], ["kxsb"])
        for blk in range(4):
            MM(pbs[7][:, 0:HW], ones_f, sqc[:, blk, 0:HW], blk == 0, blk == 3, [("sqc", blk), "onesf"], [("pb", 7)])
        MM(pbs[6][:, 0:HW], blk64, kxsb[:, 0:HW], True, True, ["kxsb", "blk64"], [("pb", 6)])
        TT("dve", xcb[:, 0:HW], kxsb[:, 0:HW], pbs[6][:, 0:HW], ALU.subtract, ["kxsb", ("pb", 6)], ["xcb"])

    def norm_b(hg):
        hc, hkeys, cols = hg_info(hg)
        ACT(sdb[:, 0:HW], pbs[7][:, 0:HW], AF.Sqrt, [("pb", 7), "cst"], ["sq2b"], scale=1.0 / 512, bias=epsT)
        RCP(rsb[:, 0:HW], sdb[:, 0:HW], ["sq2b"], ["rsb"])
        for blk in range(4):
            STT(cTn[:, blk, cols], csb[:, blk, 0:HW], gkv[:, blk:blk + 1], rsb[:, 0:HW], ALU.mult, ALU.mult,
                [("csb", blk), "rsb", "cst"], [("cTn", hg // 2)])
        ACT(sq2b[:, 0:HW], xcb[:, 0:HW], AF.Square, ["xcb"], ["sq2b"])
        MM(pbs[7][:, 0:HW], blk64, sq2b[:, 0:HW], True, True, ["sq2b", "blk64"], [("pb", 7)])

    def norm_c(hg):
        hc, hkeys, cols = hg_info(hg)
        ACT(sdb[:, 0:HW], pbs[7][:, 0:HW], AF.Sqrt, [("pb", 7), "cst"], ["sq2b"], scale=1.0, bias=epsT)
        RCP(rsb[:, 0:HW], sdb[:, 0:HW], ["sq2b"], ["rsb"])
        TT("dve", knb[:, 0:HW], xcb[:, 0:HW], rsb[:, 0:HW], ALU.mult, ["xcb", "rsb"], ["xcb"])
        TS("dve", kixT[:, cols], knb[:, 0:HW], kxg, kxb, ALU.mult, ALU.add, ["xcb", "cst"], [("kixT", hg // 2)])

    def after0(t):
        if t % 2 == 1:
            hg = t // 2
            mm_block(hg)
            if hg >= 1:
                norm_b(hg - 1)
        elif t >= 2:
            hg = t // 2 - 1
            if hg >= 1:
                norm_c(hg - 1)
            norm_a(hg)

    ktiles = []
    for t in range(16):
        hf = (t // 2) % 2
        j = hf * 2 + (t % 2)
        ktiles.append((xk[t * 128:(t + 1) * 128, :], hT[:, :, j * 128:(j + 1) * 128], hT_keys[j]))
    run_tiles(ktiles, after0)
    norm_c(6)
    norm_a(7)
    norm_b(7)
    norm_c(7)
    S.barrier(); chk("p0")

    cTn_all = [("cTn", kg) for kg in range(4)]
    kix_all = [("kixT", kg) for kg in range(4)]

    for g in range(2):
        if g > 0:
            DMA("sp", gpre, gpre_d, [], ["gpre"], K_MISC)
        otiles = []
        for j in range(4):
            t0 = g * GT + j * 128
            otiles.append((xo[t0:t0 + 128, :], hT[:, :, j * 128:(j + 1) * 128], hT_keys[j]))
        otiles.append((xh[g], hTh, [("hTh", kq) for kq in range(4)]))
        run_tiles(otiles)
        hTh_all = [("hTh", kq) for kq in range(4)]
        S.barrier(); chk("g%d_p1" % g)

        blocks = []
        for b_ in range(8):
            blocks.append(("qi", b_, C_QI + b_ * 128))
        for h in range(16):
            blocks.append(("q", h, C_Q + h * 128))
        for h in range(16):
            blocks.append(("ga", h, C_GA + h * 128))
        for b_ in range(16):
            blocks.append(("pi", b_, C_PI + b_ * 128))
        for b_ in range(16):
            blocks.append(("gb", b_, C_GB + b_ * 128))
        DMA("pool", wbx, w_in_v[:, :, C_WX:C_WX + 16], [], ["wbx"], K_MISC_SW)
        DMA("pool", wpool, w_pool_v, ["wbx"], ["wpool"], K_MISC_SW)
        DMA("sp", kchunk, kchunk_d, [], ["kchunk"], K_MISC)
        for j in range(4):
            for kc in range(KC):
                MM(pbs[7][:, 0:16], hT[:, kc, j * 128:(j + 1) * 128], wbx[:, kc, :], kc == 0, kc == KC - 1,
                   hT_keys[j] + ["wbx"], [("pb", 7)])
            ACT(wix_sb[:, j, :], pbs[7][:, 0:16], AF.Copy, [("pb", 7)], [("wix", j)], scale=WIX_SCALE)
        for bi, (kind, idx, c0) in enumerate(blocks):
            sl = bi % 3
            DMA("pool", wb[sl], w_in_v[:, :, c0:c0 + 128], [], [("wb", sl)], K_WB[sl])
            b = next_bank()
            for kc in range(KC):
                MM(pbs[b][:], wb[sl][:, kc, :], hT[:, kc, :], kc == 0, kc == KC - 1,
                   hT_all + [("wb", sl)], [("pb", b)])
            if kind == "qi":
                ACT(qiT[:, idx, :], pbs[b][:], AF.Copy, [("pb", b)], [("qiT", idx)], scale=IDX_SCALE)
            elif kind == "q":
                ACT(qT[:, idx, :], pbs[b][:], AF.Copy, [("pb", b)], [("qT", idx)], scale=ATTN_SCALE)
            elif kind == "ga":
                ACT(yin[:, idx, :], pbs[b][:], AF.Silu, [("pb", b)], [("yin", idx)])
            elif kind == "gb":
                ACT(yin[:, 16 + idx, :], pbs[b][:], AF.Silu, [("pb", b)], [("yin", 16 + idx)])
            else:
                for kc in range(KC):
                    MM(pbs[6][:, 0:16], wb[sl][:, kc, :], hTh[:, kc, 112:128], kc == 0, kc == KC - 1,
                       hTh_all + [("wb", sl)], [("pb", 6)])
                ps = bi % 3
                p = ptmp[ps]
                ta = ptmp[(ps + 1) % 3]
                tb = ptmp[(ps + 2) % 3]
                pk = ("ptmp", ps)
                tak = ("ptmp", (ps + 1) % 3)
                tbk = ("ptmp", (ps + 2) % 3)
                CP("act", p[:, 0:16], pbs[6][:, 0:16], [("pb", 6)], [pk])
                CP("act", p[:, 16:528], pbs[b][:], [("pb", b)], [pk])
                gp = idx // 4
                nlev = gp + 1
                win = 2 ** nlev
                src, srck = p, pk
                dsts = [(ta, tak), (tb, tbk)]
                for lev in range(nlev):
                    sh = 2 ** lev
                    dst, dstk = dsts[lev % 2]
                    lo_ = 2 ** (lev + 1) - 1
                    TT("dve", dst[:, lo_:528], src[:, lo_:528], src[:, lo_ - sh:528 - sh], ALU.add,
                       [srck], [dstk])
                    src, srck = dst, dstk
                TS("dve", src[:, 16:528], src[:, 16:528], 1.0 / win, None, ALU.mult, None, [srck], [srck])
                TT("dve", src[:, 16:32], src[:, 16:32], pfix[:, g, gp, :], ALU.mult, [srck, "cst"], [srck])
                TT("dve", mixT[:, idx, :], src[:, 16:528], p[:, 16:528], ALU.subtract, [srck, pk],
                   [("mixT", idx)])
        S.barrier(); chk("g%d_p2" % g)

        for gp in range(4):
            for dj in range(4):
                b = next_bank()
                for cc in range(4):
                    MM(pbs[b][:], wpool[:, gp * 4 + cc, dj * 128:(dj + 1) * 128], mixT[:, gp * 4 + cc, :],
                       cc == 0, cc == 3, ["wpool", ("mixT", gp * 4 + cc)], [("pb", b)])
                blk = gp * 4 + dj
                STT(yin[:, 16 + blk, :], pbs[b][:], pscale[:, blk:blk + 1], yin[:, 16 + blk, :],
                    ALU.mult, ALU.mult, [("pb", b), ("yin", 16 + blk), "cst"], [("yin", 16 + blk)])
        S.barrier(); chk("g%d_p3" % g)

        nks = [8 + 4 * g + j + 1 for j in range(4)]
        S.add("pool", lambda e: e.memset(kixAB[0][64:128, :], 0.0), [], [("kixAB", 0, 1)])
        S.add("pool", lambda e: e.memset(kixAB[1][0:64, :], 0.0), [], [("kixAB", 1, 0)])
        CP("pool", kixAB[0][0:64, :], kixT[0:64, :], kix_all, [("kixAB", 0, 0)])
        CP("pool", kixAB[1][64:128, :], kixT[64:128, :], kix_all, [("kixAB", 1, 1)])
        kixAB_keys = [("kixAB", 0, 0), ("kixAB", 0, 1), ("kixAB", 1, 0), ("kixAB", 1, 1)]
        for j in range(4):
            ncol = nks[j] * 128
            qi_ = 4 * g + j
            TS("dve", negb[j][:, 0:ncol], kchunk[:, 0:ncol], qchunk[:, qi_:qi_ + 1], -BIG, ALU.is_gt, ALU.mult,
               ["kchunk", "cst"], [("negb", j)])
        rctr = [0]
        abctr = [0]

        def emit_scores(j):
            ncol = nks[j] * 128
            dg = diag[j % 2]
            for h in range(16):
                TS("pool", dg[:, h, :], ident_bf, wix_sb[:, j, h:h + 1], 1.0, ALU.mult, ALU.mult,
                   ["identb", ("wix", j)], [("diag", j % 2, h)])
            for s0 in range(0, ncol, 512):
                wd = min(512, ncol - s0)
                ab = 6 + abctr[0] % 2
                abctr[0] += 1
                slots = {}

                def dots(h):
                    b = next_bank()
                    MM(pbs[b][:, 0:wd], qiT[:, h // 2, j * 128:(j + 1) * 128], kixAB[h % 2][:, s0:s0 + wd],
                       True, True, [("qiT", h // 2)] + kixAB_keys, [("pb", b)])
                    rs_ = rctr[0] % 4
                    rctr[0] += 1
                    slots[h] = rs_
                    if j < 2 and h % 2 == 1:
                        TS("dve", rbuf[rs_][:, 0:wd], pbs[b][:, 0:wd], 0.0, None, ALU.max, None,
                           [("pb", b)], [("rbuf", rs_)])
                    else:
                        ACT(rbuf[rs_][:, 0:wd], pbs[b][:, 0:wd], AF.Relu, [("pb", b)], [("rbuf", rs_)])

                dots(0)
                dots(1)
                for h in range(16):
                    if h + 2 < 16:
                        dots(h + 2)
                    rs_ = slots[h]
                    MM(pbs[ab][:, 0:wd], dg[:, h, :], rbuf[rs_][:, 0:wd], h == 0, False,
                       [("diag", j % 2, h), ("rbuf", rs_)], [("pb", ab)])
                MM(pbs[ab][:, 0:wd], ident_bf, negb[j][:, s0:s0 + wd], False, True,
                   ["identb", ("negb", j)], [("pb", ab)])
                CP("act", acc[j][:, s0:s0 + wd], pbs[ab][:, 0:wd], [("pb", ab)], [("acc", j)])

        def emit_bisect(js):
            for j in js:
                S.add("dve", lambda e, j=j: e.memset(cand[:, j:j + 1], 0.0), [], [("cand", j)])
            for it in range(BIS_N):
                step = BIS_R * (2.0 ** (-it))
                last = it == BIS_N - 1
                for j in js:
                    ncol = nks[j] * 128
                    TS("dve", junk[:, 0:ncol], acc[j][:, 0:ncol], cand[:, j:j + 1], 0.0, ALU.is_ge, ALU.add,
                       [("acc", j), ("cand", j)], ["junk", ("cnt", j)], accum=cnt[:, j:j + 1])
                    TS("dve", tflag[:, j:j + 1], cnt[:, j:j + 1], TOPK - 0.5, step, ALU.is_ge, ALU.mult,
                       [("cnt", j)], [("tflag", j)])
                    STT(cand[:, j:j + 1], tflag[:, j:j + 1], (-step if last else -0.5 * step), cand[:, j:j + 1],
                        ALU.add, ALU.add, [("tflag", j), ("cand", j)], [("cand", j)])

        def emit_masks(js):
            for j in js:
                ncol = nks[j] * 128
                ms = j % 2
                TS("dve", maskbf[ms][:, 0:ncol], acc[j][:, 0:ncol], cand[:, j:j + 1], MASK_OFF, ALU.is_ge, ALU.mult,
                   [("acc", j), ("cand", j)], [("maskbf", ms)])
                for k8 in range(0, nks[j], 8):
                    n8 = min(8, nks[j] - k8)
                    b = tr_banks[tr_ctr[0] % 2]
                    tr_ctr[0] += 1
                    pt = pbT(b)
                    for i in range(n8):
                        kt = k8 + i
                        TR(pt[:, i, :], maskbf[ms][:, kt * 128:(kt + 1) * 128], ident_bf,
                           [("maskbf", ms), "identb"], [("pb", b)])
                    CP("act", maskT[:, k8:k8 + n8, j, :], pt[:, 0:n8, :], [("pb", b)], [("maskT", j)])

        emit_scores(0)
        emit_scores(1)
        emit_bisect([0, 1])
        emit_scores(2)
        emit_scores(3)
        emit_masks([0, 1])
        emit_bisect([2, 3])
        emit_masks([2, 3])
        S.barrier(); chk("g%d_p4" % g)

        nkmax = 8 + 4 * g + 4
        mask_all = [("maskT", j) for j in range(4)]
        items = [(h, kt) for h in range(16) for kt in range(nkmax)]
        LA = 2
        SB = [0, 1, 2]
        OB = [3, 6]
        kvb = [0]

        def dma_k(h):
            sl = h % 2
            DMA("pool", wuk[sl], w_uk[h].rearrange("(cc p) d -> p cc d", p=128), [], [("wuk", sl)], K_UK[sl])

        def mm_k(h):
            sl = h % 2
            for s0 in range(0, nkmax * 128, 512):
                b = 4 + kvb[0] % 2
                kvb[0] += 1
                for cc in range(4):
                    MM(pbs[b][:], wuk[sl][:, cc, :], cTn[:, cc, s0:s0 + 512], cc == 0, cc == 3,
                       [("wuk", sl)] + cTn_all, [("pb", b)])
                CP("act", kTh[sl][:, s0:s0 + 512], pbs[b][:], [("pb", b)], [("kTh", sl, s0 // 512)])

        def dma_v4(hgp):
            sl = hgp % 2
            for hh in range(4):
                DMA("pool", wuv4[sl][:, :, hh * 128:(hh + 1) * 128],
                    w_uv[4 * hgp + hh].rearrange("(cc p) d -> p cc d", p=128),
                    [], [("wuv4", sl, hh)], K_UV4[sl][hh])

        def mm_v4(hgp):
            sl = hgp % 2
            for kt in range(nkmax):
                b = 4 + kvb[0] % 2
                kvb[0] += 1
                for cc in range(4):
                    MM(pbs[b][:], cTn[:, cc, kt * 128:(kt + 1) * 128], wuv4[sl][:, cc, :],
                       cc == 0, cc == 3, [("wuv4", sl, hh_) for hh_ in range(4)] + cTn_all, [("pb", b)])
                eng = "dve" if kt % 2 == 0 else "act"
                CP(eng, vS4[sl][:, kt, :], pbs[b][:], [("pb", b)], [("vS4", sl, kt // 4)])

        def emit_qk(i):
            h, kt = items[i]
            sl = h % 2
            jmin = max(0, kt - 8 - 4 * g)
            c0 = jmin * 128
            sb_ = SB[i % 3]
            es = i % 3
            MM(pbs[sb_][:, c0:GT], kTh[sl][:, kt * 128:(kt + 1) * 128], qT[:, h, c0:GT], True, False,
               [("kTh", sl, kt // 4), ("qT", h)], [("pb", sb_)])
            MM(pbs[sb_][:, c0:GT], ident_bf, maskT[:, kt, jmin:4, :].rearrange("p j t -> p (j t)"), False, True,
               ["identb"] + mask_all, [("pb", sb_)])
            ACT(eT[es][:, c0:GT], pbs[sb_][:, c0:GT], AF.Exp, [("pb", sb_), "cst"], [("eT", es)], bias=negoff)

        def emit_pv(i):
            h, kt = items[i]
            sl = h % 2
            jmin = max(0, kt - 8 - 4 * g)
            c0 = jmin * 128
            es = i % 3
            ob = OB[h % 2]
            vsl = (h // 4) % 2
            MM(pbs[ob][:, c0:GT], vS4[vsl][:, kt, (h % 4) * 128:(h % 4 + 1) * 128], eT[es][:, c0:GT],
               kt == 0, kt == nkmax - 1, [("vS4", vsl, kt // 4), ("eT", es)], [("pb", ob)])
            MM(pbs[7][:, c0:GT], ones_bf, eT[es][:, c0:GT], kt == 0, kt == nkmax - 1,
               ["onesb", ("eT", es)], [("pb", 7)])
            if kt == nkmax - 1:
                os_ = h % 2
                RCP(rcb[os_], pbs[7][:], [("pb", 7)], [("rcb", os_)])
                TT("dve", otmp[os_], pbs[ob][:], rcb[os_], ALU.mult, [("pb", ob), ("rcb", os_)], [("otmp", os_)])
                TT("pool", yin[:, h, :], otmp[os_], yin[:, h, :], ALU.mult,
                   [("otmp", os_), ("yin", h)], [("yin", h)])

        dma_k(0)
        dma_k(1)
        dma_v4(0)
        mm_k(0)
        mm_v4(0)
        for i in range(LA):
            emit_qk(i)
        for i in range(len(items)):
            h, kt = items[i]
            if kt == 0:
                if h + 2 < 16:
                    dma_k(h + 2)
                if h % 4 == 0 and h // 4 + 1 < 4:
                    dma_v4(h // 4 + 1)
                if h + 1 < 16:
                    mm_k(h + 1)
                if h % 4 == 1 and h // 4 + 1 < 4:
                    mm_v4(h // 4 + 1)
            if i + LA < len(items):
                emit_qk(i + LA)
            emit_pv(i)
        S.barrier(); chk("g%d_p5" % g)

        yin_all = [("yin", i) for i in range(32)]
        def ssq_mm(db):
            qs = db % 2
            for j in range(4):
                MM(pbs[6][:, j:j + 1], sqb[qs][:, j * 128:(j + 1) * 128], ones_bf[:, 0:1],
                   db == 0 and j == 0, db == 31 and j == 3, [("sqb", qs), "onesb"], [("pb", 6)])

        for db in range(32):
            sl = db % 3
            DMA("pool", wb6[sl], w_out_v[:, :, db * 128:(db + 1) * 128], [], [("wb6", sl)], K_WB[sl])
            b = next_bank()
            for ec in range(KC):
                MM(pbs[b][:], wb6[sl][:, ec, :], yin[:, ec, :], ec == 0, ec == KC - 1,
                   yin_all + [("wb6", sl)], [("pb", b)])
            if db > 0:
                ssq_mm(db - 1)
            qs = db % 2
            ACT(sqb[qs], pbs[b][:], AF.Square, [("pb", b)], [("sqb", qs)])
            TS("dve", yg[:, db, :], pbs[b][:], gpost[:, db:db + 1], None, ALU.mult, None,
               [("pb", b), "cst"], [("yg", db)])
        ssq_mm(31)
        ACT(sdo, pbs[6][:, 0:4], AF.Sqrt, [("pb", 6), "cst"], ["sdo"], scale=1.0 / D, bias=epsT)
        RCP(rstdo, sdo, ["sdo"], ["rstdo"])
        yg_all = [("yg", db) for db in range(32)]
        for j in range(4):
            sl = xt_ctr[0] % 2
            xt_ctr[0] += 1
            t0 = g * GT + j * 128
            DMA("sp", xt[sl], xo[t0:t0 + 128, :], [], [("xt", sl)], K_XT[sl])
            for d4 in range(8):
                b = tr_banks[tr_ctr[0] % 2]
                tr_ctr[0] += 1
                for i in range(4):
                    db = d4 * 4 + i
                    TR(pbs[b][:, i * 128:(i + 1) * 128], yg[:, db, j * 128:(j + 1) * 128], ident_f,
                       [("yg", db), "identf"], [("pb", b)])
                STT(xt[sl][:, d4 * 512:(d4 + 1) * 512], pbs[b][:], rstdo[:, j:j + 1],
                    xt[sl][:, d4 * 512:(d4 + 1) * 512], ALU.mult, ALU.add,
                    [("pb", b), "rstdo", ("xt", sl)], [("xt", sl)])
            DMA("sp", yout[t0:t0 + 128, :], xt[sl], [("xt", sl)], [("yout", g, j)], K_ST[sl])
        S.barrier(); chk("g%d_p6" % g)

    S.emit(st)
    st.close()
    return nc


_NC_CACHE = {}
_ONLY_MAPS = False


def _get_nc():
    if "nc" not in _NC_CACHE:
        _NC_CACHE["nc"] = build_nc()
    return _NC_CACHE["nc"]


def kernel(x, pre_norm, w_in, kv_norm, w_uk, w_uv, idx_k_norm_g, idx_k_norm_b, w_pool, pool_scale, w_out,
           post_norm):
    f32 = np.float32
    x = np.asarray(x, f32)
    B = x.shape[0]
    w_in0 = np.ascontiguousarray(np.asarray(w_in, f32)[0])
    w_out0 = np.ascontiguousarray(np.asarray(w_out, f32)[0])
    w_uk0 = np.ascontiguousarray(np.asarray(w_uk, f32)[0])
    w_uv0 = np.ascontiguousarray(np.asarray(w_uv, f32)[0])
    w_pool0 = np.ascontiguousarray(np.asarray(w_pool, f32)[0])
    gpre_b = np.ascontiguousarray(np.broadcast_to(np.asarray(pre_norm, f32)[0][None, :], (128, D)))
    kchunk_b = np.ascontiguousarray(np.broadcast_to((np.arange(SEQ) // 64).astype(f32)[None, :], (128, SEQ)))
    ident_f = np.eye(128, dtype=f32)
    ident_bf = np.eye(128, dtype=f32).astype(ml_dtypes.bfloat16)
    blk64 = np.zeros((128, 128), f32)
    blk64[0:64, 0:64] = 1.0 / 64
    blk64[64:128, 64:128] = 1.0 / 64
    cst0 = np.zeros((128, 512), f32)
    cst0[:, 0:4] = np.asarray(kv_norm, f32)[0].reshape(4, 128).T
    kg = np.asarray(idx_k_norm_g, f32)[0]
    kb = np.asarray(idx_k_norm_b, f32)[0]
    cst0[:, 4] = np.concatenate([kg, kg])
    cst0[:, 5] = np.concatenate([kb, kb])
    cst0[:, 6] = EPS
    cst0[:, 7] = -MASK_OFF
    cst0[:, 8:24] = np.asarray(pool_scale, f32)[0].reshape(16, 128).T
    cst0[:, 24:56] = np.asarray(post_norm, f32)[0].reshape(32, 128).T
    wins = [2, 4, 8, 16]
    in_maps = []
    for b in range(B):
        for half in range(2):
            cst = cst0.copy()
            tok = half * 1024 + np.arange(1024)
            cst[:, 56:64] = (tok // 64).astype(f32).reshape(8, 128).T
            pf = np.ones((2, 4, 16), f32)
            if half == 0:
                for gi, wdw in enumerate(wins):
                    t = np.arange(16)
                    pf[0, gi, :] = wdw / np.minimum(t + 1, wdw)
            cst[:, 64:192] = pf.reshape(1, 128)
            xo = np.ascontiguousarray(x[b, half * 1024:(half + 1) * 1024])
            xh = np.zeros((2, 128, D), f32)
            if half == 1:
                xh[0] = x[b, 896:1024]
            xh[1] = xo[384:512]
            in_maps.append({
                "xk": np.ascontiguousarray(x[b]), "xo": xo, "xh": xh,
                "w_in": w_in0, "w_out": w_out0, "w_uk": w_uk0, "w_uv": w_uv0, "w_pool": w_pool0,
                "gpre_b": gpre_b, "kchunk_b": kchunk_b, "cst": cst, "ident_f": ident_f, "blk64": blk64,
                "ident_bf": ident_bf,
            })
    if _ONLY_MAPS:
        return in_maps
    nc = _get_nc()
    res = run_bass_kernel_spmd(nc, in_maps, core_ids=list(range(2 * B)))
    out = np.zeros((B, SEQ, D), f32)
    for b in range(B):
        for half in range(2):
            out[b, half * 1024:(half + 1) * 1024] = res.results[2 * b + half]["y"]
    return out
```

```python
from contextlib import ExitStack
import numpy as np
import ml_dtypes
import concourse.bass as bass
import concourse.mybir as mybir
from concourse.bass_utils import run_bass_kernel_spmd

F32 = mybir.dt.float32
BF16 = mybir.dt.bfloat16
ALU = mybir.AluOpType
AF = mybir.ActivationFunctionType

ENGS = ("pe", "act", "dve", "pool", "sp")
N_DMA_SEMS = 22

D = 4096
DIN = 9808
SEQ = 2048
KC = 32
GT = 512
C_Q, C_C, C_QI, C_KX, C_WX, C_GA, C_PI, C_GB = 0, 2048, 2560, 3584, 3648, 3664, 5712, 7760
EPS = 1e-6
ATTN_SCALE = 128 ** -0.5
IDX_SCALE = 64 ** -0.5
WIX_SCALE = 16 ** -0.5
BIG = 1.0e30
BIS_R = 16.0
BIS_N = 21
TOPK = 256
MASK_OFF = 100.0


class Op:
    __slots__ = ("eng", "fn", "deps", "dma", "sig", "sem", "val", "idx")

    def __init__(self, eng, fn, dma):
        self.eng = eng
        self.fn = fn
        self.deps = set()
        self.dma = dma
        self.sig = False
        self.sem = None
        self.val = 0


class Sched:
    def __init__(self, nc):
        self.nc = nc
        self.ops = []
        self.lastw = {}
        self.readers = {}
        self.fence = set()
        self.last_eng = {}
        self.last_dma = {}
        self.frozen = False

    def add(self, eng, fn, reads=(), writes=(), dma=None):
        if self.frozen:
            return None
        op = Op(eng, fn, dma)
        idx = len(self.ops)
        op.idx = idx
        if eng != "pe":
            extra = [("pbx", r[1]) for r in reads if isinstance(r, tuple) and r[0] == "pb"]
            if extra:
                writes = list(writes) + extra
        for r in reads:
            w = self.lastw.get(r)
            if w is not None:
                op.deps.add(w)
        for w_ in writes:
            w = self.lastw.get(w_)
            if w is not None:
                op.deps.add(w)
            for rd in self.readers.get(w_, ()):
                op.deps.add(rd)
        for r in reads:
            self.readers.setdefault(r, []).append(idx)
        for w_ in writes:
            self.lastw[w_] = idx
            self.readers[w_] = []
        op.deps |= self.fence
        op.deps.discard(idx)
        self.ops.append(op)
        if dma is None:
            self.last_eng[eng] = idx
        else:
            self.last_dma[dma] = idx
        return idx

    def barrier(self):
        self.fence = set(self.last_eng.values()) | set(self.last_dma.values())

    def emit(self, stack):
        nc = self.nc
        ops = self.ops

        def skip(dop, op):
            return dop.eng == "pe" and op.eng == "pe" and dop.dma is None and op.dma is None

        for op in ops:
            for d in op.deps:
                if not skip(ops[d], op):
                    ops[d].sig = True
        for op in ops:
            if op.dma is not None:
                op.sig = True
        esem = {e: stack.enter_context(nc.semaphore("s_" + e)) for e in ENGS}
        dsem = [stack.enter_context(nc.semaphore("d_%d" % i)) for i in range(N_DMA_SEMS)]
        ecount = {e: 0 for e in ENGS}
        dcount = [0] * N_DMA_SEMS
        for op in ops:
            if not op.sig:
                continue
            if op.dma is not None:
                dcount[op.dma] += 16
                op.sem = dsem[op.dma]
                op.val = dcount[op.dma]
            else:
                ecount[op.eng] += 1
                op.sem = esem[op.eng]
                op.val = ecount[op.eng]
        per_eng = {e: [] for e in ENGS}
        for op in ops:
            per_eng[op.eng].append(op)
        dfinal = list(dcount)
        block = stack.enter_context(nc.Block())

        def make_body(e):
            def body(eng):
                waited = {}
                for op in per_eng[e]:
                    need = {}
                    for d in op.deps:
                        dop = ops[d]
                        if skip(dop, op):
                            continue
                        k = id(dop.sem)
                        if k not in need or need[k][1] < dop.val:
                            need[k] = (dop.sem, dop.val)
                    for k, (sem, val) in need.items():
                        if waited.get(k, 0) >= val:
                            continue
                        eng.wait_ge(sem, val)
                        waited[k] = val
                    ins = op.fn(eng)
                    if op.sig:
                        ins.then_inc(op.sem, 16 if op.dma is not None else 1)
                if e == "sp":
                    for i in range(N_DMA_SEMS):
                        if dfinal[i] > 0:
                            eng.wait_ge(dsem[i], dfinal[i])
            return body

        block.tensor(make_body("pe"))
        block.scalar(make_body("act"))
        block.vector(make_body("dve"))
        block.gpsimd(make_body("pool"))
        block.sync(make_body("sp"))


class _Stop(Exception):
    pass


STOP = None


def build_nc():
    nc = bass.Bass("TRN2", target_bir_lowering=False)

    def chk(label):
        if STOP == label:
            S.frozen = True

    def din(name, shape, dt=F32):
        return nc.dram_tensor(name, list(shape), dt, kind="ExternalInput").ap()

    xk = din("xk", [SEQ, D])
    xo = din("xo", [1024, D])
    xh = din("xh", [2, 128, D])
    w_in = din("w_in", [D, DIN])
    w_out = din("w_out", [D, D])
    w_uk = din("w_uk", [16, 512, 128])
    w_uv = din("w_uv", [16, 512, 128])
    w_pool = din("w_pool", [4, 512, 512])
    gpre_d = din("gpre_b", [128, D])
    kchunk_d = din("kchunk_b", [128, SEQ])
    cst_d = din("cst", [128, 512])
    identf_d = din("ident_f", [128, 128])
    blk64_d = din("blk64", [128, 128])
    identb_d = din("ident_bf", [128, 128], BF16)
    yout = nc.dram_tensor("y", [1024, D], F32, kind="ExternalOutput").ap()

    w_in_v = w_in.rearrange("(kc p) e -> p kc e", p=128)
    w_out_v = w_out.rearrange("(kc p) e -> p kc e", p=128)
    w_pool_v = w_pool.rearrange("g (cc p) d -> p (g cc) d", p=128)

    st = ExitStack()
    st.enter_context(nc.allow_low_precision("bf16 matmul operands, fp32 accumulation"))
    ACOLS = 49152
    arena = st.enter_context(nc.sbuf_tensor("arena", [128, ACOLS], F32))
    pbs = [st.enter_context(nc.psum_tensor("pb%d" % i, [128, 512], F32)) for i in range(8)]
    S = Sched(nc)

    def fv(off, n):
        assert off + n <= ACOLS
        return arena[:, off:off + n]

    def bvw(off, ncols):
        assert off + ncols <= ACOLS
        return arena[:, off:off + ncols].bitcast(BF16)

    def pbT(i):
        return pbs[i][:].bitcast(BF16).rearrange("p (a b) -> p a b", b=128)

    cTn = bvw(0, 4096).rearrange("p (a b) -> p a b", b=SEQ)
    kixT = bvw(4096, 1024)
    o = 5120
    cst = fv(o, 512); o += 512
    ident_f = fv(o, 128); o += 128
    blk64 = fv(o, 128); o += 128
    ones_f = fv(o, 128); o += 128
    ident_bf = bvw(o, 64); o += 64
    ones_bf = bvw(o, 64); o += 64
    assert o <= 6144
    gkv = cst[:, 0:4]
    kxg = cst[:, 4:5]
    kxb = cst[:, 5:6]
    epsT = cst[:, 6:7]
    negoff = cst[:, 7:8]
    pscale = cst[:, 8:24]
    gpost = cst[:, 24:56]
    qchunk = cst[:, 56:64]
    pfix = cst[:, 64:192].rearrange("p (g a b) -> p g a b", g=2, a=4)
    ss2 = cst[:, 192:194]
    sd2 = cst[:, 194:196]
    rstd2 = cst[:, 196:198]
    wix_sb = cst[:, 200:264].rearrange("p (j h) -> p j h", h=16)
    cand = cst[:, 264:268]
    cnt = cst[:, 268:272]
    tflag = cst[:, 272:276]
    sdo = cst[:, 276:280]
    rstdo = cst[:, 280:284]

    YIN0, QT0, QIT0, HT0, HTH0, R10 = 6144, 14336, 18432, 20480, 28672, 30720
    yin = bvw(YIN0, 8192).rearrange("p (a b) -> p a b", b=GT)
    qT = bvw(QT0, 4096).rearrange("p (a b) -> p a b", b=GT)
    qiT = bvw(QIT0, 2048).rearrange("p (a b) -> p a b", b=GT)
    hT = bvw(HT0, 8192).rearrange("p (a b) -> p a b", b=GT)
    hTh = bvw(HTH0, 2048).rearrange("p (a b) -> p a b", b=128)
    xt = [fv(R10 + i * 4096, 4096) for i in range(2)]
    xsb = [bvw(R10 + 8192 + i * 2048, 2048) for i in range(2)]
    gpre = fv(R10 + 12288, 4096)
    wb = [bvw(R10 + i * 2048, 2048).rearrange("p (a b) -> p a b", b=128) for i in range(3)]
    ptmp = [fv(R10 + 6144 + i * 528, 528) for i in range(3)]
    wbx = bvw(R10 + 7744, 256).rearrange("p (a b) -> p a b", b=16)
    mixT = bvw(R10 + 8192, 4096).rearrange("p (a b) -> p a b", b=GT)
    wck_c = bvw(6144, 8192).rearrange("p (a b) -> p a b", b=512)
    wck_k = bvw(14336, 2048).rearrange("p (a b) -> p a b", b=128)
    csb = fv(16384, 2048).rearrange("p (a b) -> p a b", b=512)
    sqc = fv(18432, 2048).rearrange("p (a b) -> p a b", b=512)
    P0M = R10 + 16384
    kxsb = fv(P0M, 512)
    xcb = fv(P0M + 512, 512)
    sq2b = fv(P0M + 1024, 512)
    rsb = fv(P0M + 1536, 512)
    sdb = sq2b
    knb = xcb
    wpool = bvw(R10 + 12288, 4096).rearrange("p (a b) -> p a b", b=512)
    acc = [fv(R10 + j * 2048, 2048) for j in range(4)]
    negb = [bvw(R10 + 8192 + j * 1024, 1024) for j in range(4)]
    rbuf = [bvw(R10 + 12288 + i * 256, 256) for i in range(4)]
    junk = bvw(R10 + 13312, 1024)
    maskbf = [bvw(R10 + 14336 + i * 1024, 1024) for i in range(2)]
    kixAB = [bvw(HTH0 + i * 1024, 1024) for i in range(2)]
    kchunk = fv(R10 + 16384, 2048)
    maskT = bvw(HT0, 4096).rearrange("p (k j t) -> p k j t", k=16, j=4)
    diag = [bvw(HT0 + 4096 + i * 1024, 1024).rearrange("p (a b) -> p a b", b=128) for i in range(2)]
    kTh = [bvw(R10 + i * 1024, 1024) for i in range(2)]
    vS4 = [bvw(R10 + 2048 + i * 4096, 4096).rearrange("p (a b) -> p a b", b=512) for i in range(2)]
    wuk = [bvw(R10 + 10240 + i * 256, 256).rearrange("p (a b) -> p a b", b=128) for i in range(2)]
    wuv4 = [bvw(R10 + 10752 + i * 1024, 1024).rearrange("p (a b) -> p a b", b=512) for i in range(2)]
    eT = [bvw(R10 + 12800 + i * 256, 256) for i in range(3)]
    pT = [bvw(R10 + 13568 + i * 256, 256) for i in range(3)]
    rcb = [fv(R10 + 14336 + i * 512, 512) for i in range(2)]
    otmp = [fv(R10 + 15360 + i * 512, 512) for i in range(2)]
    yg = fv(QT0, 16384).rearrange("p (a b) -> p a b", b=GT)
    wb6 = [bvw(R10 + 8192 + i * 2048, 2048).rearrange("p (a b) -> p a b", b=128) for i in range(3)]
    sqb = [bvw(R10 + 14336 + i * 256, 256) for i in range(2)]

    def MM(out, lhsT, rhs, start, stop, r, w):
        S.add("pe", lambda e: e.matmul(out, lhsT, rhs, start=start, stop=stop), r, w)

    def TR(out, in_, ident, r, w):
        S.add("pe", lambda e: e.transpose(out, in_, ident), r, w)

    def ACT(out, in_, func, r, w, scale=None, bias=None, accum=None):
        kw = {}
        if scale is not None:
            kw["scale"] = scale
        if bias is not None:
            kw["bias"] = bias
        if accum is not None:
            kw["accum_out"] = accum
        S.add("act", lambda e: e.activation(out, in_, func, **kw), r, w)

    def TS(eng, out, in0, s1, s2, op0, op1, r, w, accum=None):
        if op1 is None:
            if accum is None:
                S.add(eng, lambda e: e.tensor_scalar(out, in0, s1, None, op0), r, w)
            else:
                raise ValueError
        else:
            if accum is None:
                S.add(eng, lambda e: e.tensor_scalar(out, in0, s1, s2, op0, op1), r, w)
            else:
                S.add(eng, lambda e: e.tensor_scalar(out, in0, s1, s2, op0, op1, accum_out=accum), r, w)

    def TT(eng, out, in0, in1, op, r, w):
        S.add(eng, lambda e: e.tensor_tensor(out, in0, in1, op), r, w)

    def STT(out, in0, scalar, in1, op0, op1, r, w):
        S.add("dve", lambda e: e.scalar_tensor_tensor(out, in0, scalar, in1, op0, op1), r, w)

    def RCP(out, in_, r, w):
        S.add("dve", lambda e: e.reciprocal(out, in_), r, w)

    def CP(eng, out, in_, r, w):
        if eng == "act":
            S.add("act", lambda e: e.copy(out, in_), r, w)
        else:
            S.add(eng, lambda e: e.tensor_copy(out, in_), r, w)

    def DMA(eng, out, in_, r, w, key):
        S.add(eng, lambda e: e.dma_start(out=out, in_=in_), r, w, dma=key)

    K_XT = [0, 1]
    K_WB = [2, 3, 4]
    K_ST = [5, 6]
    K_UK = [7, 8]
    K_UV = [9, 10]
    K_MISC = 11
    K_MISC_SW = 12
    K_UV4 = [[13, 14, 15, 16], [17, 18, 19, 20]]

    DMA("sp", cst, cst_d, [], ["cst"], K_MISC)
    DMA("sp", ident_f, identf_d, [], ["identf"], K_MISC)
    DMA("sp", blk64, blk64_d, [], ["blk64"], K_MISC)
    DMA("sp", ident_bf, identb_d, [], ["identb"], K_MISC)
    S.add("dve", lambda e: e.memset(ones_f, 1.0), [], ["onesf"])
    S.add("dve", lambda e: e.memset(ones_bf, 1.0), [], ["onesb"])
    S.barrier(); chk("setup")

    tr_banks = [0, 1]
    tr_ctr = [0]
    xt_ctr = [0]

    def stage1(src, sl):
        xs = xsb[sl]
        xk_ = ("xs", sl)
        DMA("sp", xt[sl], src, [], [("xt", sl)], K_XT[sl])
        ACT(xs, xt[sl], AF.Square, [("xt", sl)], [xk_, ("ss", sl)], accum=ss2[:, sl:sl + 1])
        ACT(sd2[:, sl:sl + 1], ss2[:, sl:sl + 1], AF.Sqrt, [("ss", sl), "cst"], [("sd", sl)],
            scale=1.0 / D, bias=epsT)
        RCP(rstd2[:, sl:sl + 1], sd2[:, sl:sl + 1], [("sd", sl)], [("rstd", sl)])
        STT(xs, xt[sl], rstd2[:, sl:sl + 1], gpre, ALU.mult, ALU.mult,
            [("xt", sl), ("rstd", sl), "gpre"], [xk_])

    def stage2(sl, dst, dst_keys):
        xs = xsb[sl]
        xk_ = ("xs", sl)
        for kq in range(4):
            b = tr_banks[tr_ctr[0] % 2]
            tr_ctr[0] += 1
            pt = pbT(b)
            for i in range(8):
                kc = kq * 8 + i
                TR(pt[:, i, :], xs[:, kc * 128:(kc + 1) * 128], ident_bf, [xk_, "identb"], [("pb", b)])
            eng = "act" if kq % 2 == 0 else "dve"
            CP(eng, dst[:, kq * 8:(kq + 1) * 8, :], pt, [("pb", b)], [dst_keys[kq]])

    def run_tiles(tiles, after=None):
        n = len(tiles)
        sls = []
        for s_ in range(n + 1):
            if s_ < n:
                sl = xt_ctr[0] % 2
                xt_ctr[0] += 1
                sls.append(sl)
                stage1(tiles[s_][0], sl)
            if s_ >= 1:
                t = s_ - 1
                stage2(sls[t], tiles[t][1], tiles[t][2])
                if after is not None:
                    after(t)

    acc_banks = [2, 3, 4, 5]
    acc_ctr = [0]

    def next_bank():
        b = acc_banks[acc_ctr[0] % 4]
        acc_ctr[0] += 1
        return b

    DMA("sp", gpre, gpre_d, [], ["gpre"], K_MISC)
    DMA("pool", wck_c, w_in_v[:, :, C_C:C_C + 512], [], ["wckc"], K_WB[0])
    DMA("pool", wck_k[:, :, 0:64], w_in_v[:, :, C_KX:C_KX + 64], [], ["wckk0"], K_WB[1])
    DMA("pool", wck_k[:, :, 64:128], w_in_v[:, :, C_KX:C_KX + 64], [], ["wckk1"], K_WB[2])
    chk("p0a")
    hT_keys = [[("hT", kq, j) for kq in range(4)] for j in range(4)]
    hT_all = [k for ks in hT_keys for k in ks]
    HW = 256

    def hg_info(hg):
        hf = hg % 2
        hc = slice(hf * HW, (hf + 1) * HW)
        hkeys = hT_keys[hf * 2] + hT_keys[hf * 2 + 1]
        cols = slice(hg * HW, (hg + 1) * HW)
        return hc, hkeys, cols

    def mm_block(hg):
        hc, hkeys, cols = hg_info(hg)
        for blk in range(4):
            b = 2 + blk
            for kc in range(KC):
                MM(pbs[b][:, 0:HW], wck_c[:, kc, blk * 128:(blk + 1) * 128], hT[:, kc, hc], kc == 0, kc == KC - 1,
                   hkeys + ["wckc"], [("pb", b)])
        for kc in range(KC):
            MM(pbs[6][:, 0:HW], wck_k[:, kc, :], hT[:, kc, hc], kc == 0, kc == KC - 1,
               hkeys + ["wckk0", "wckk1"], [("pb", 6)])

    def norm_a(hg):
        for blk in range(4):
            b = 2 + blk
            CP("dve", csb[:, blk, 0:HW], pbs[b][:, 0:HW], [("pb", b)], [("csb", blk)])
            ACT(sqc[:, blk, 0:HW], pbs[b][:, 0:HW], AF.Square, [("pb", b)], [("sqc", blk)])
        CP("dve", kxsb[:, 0:HW], pbs[6][:, 0:HW], [("pb", 6)], ["kxsb"])
        for blk in range(4):
            MM(pbs[7][:, 0:HW], ones_f, sqc[:, blk, 0:HW], blk == 0, blk == 3, [("sqc", blk), "onesf"], [("pb", 7)])
        MM(pbs[6][:, 0:HW], blk64, kxsb[:, 0:HW], True, True, ["kxsb", "blk64"], [("pb", 6)])
        TT("dve", xcb[:, 0:HW], kxsb[:, 0:HW], pbs[6][:, 0:HW], ALU.subtract, ["kxsb", ("pb", 6)], ["xcb"])

    def norm_b(hg):
        hc, hkeys, cols = hg_info(hg)
        ACT(sdb[:, 0:HW], pbs[7][:, 0:HW], AF.Sqrt, [("pb", 7), "cst"], ["sq2b"], scale=1.0 / 512, bias=epsT)
        RCP(rsb[:, 0:HW], sdb[:, 0:HW], ["sq2b"], ["rsb"])
        for blk in range(4):
            STT(cTn[:, blk, cols], csb[:, blk, 0:HW], gkv[:, blk:blk + 1], rsb[:, 0:HW], ALU.mult, ALU.mult,
                [("csb", blk), "rsb", "cst"], [("cTn", hg // 2)])
        ACT(sq2b[:, 0:HW], xcb[:, 0:HW], AF.Square, ["xcb"], ["sq2b"])
        MM(pbs[7][:, 0:HW], blk64, sq2b[:, 0:HW], True, True, ["sq2b", "blk64"], [("pb", 7)])

    def norm_c(hg):
        hc, hkeys, cols = hg_info(hg)
        ACT(sdb[:, 0:HW], pbs[7][:, 0:HW], AF.Sqrt, [("pb", 7), "cst"], ["sq2b"], scale=1.0, bias=epsT)
        RCP(rsb[:, 0:HW], sdb[:, 0:HW], ["sq2b"], ["rsb"])
        TT("dve", knb[:, 0:HW], xcb[:, 0:HW], rsb[:, 0:HW], ALU.mult, ["xcb", "rsb"], ["xcb"])
        TS("dve", kixT[:, cols], knb[:, 0:HW], kxg, kxb, ALU.mult, ALU.add, ["xcb", "cst"], [("kixT", hg // 2)])

    def after0(t):
        if t % 2 == 1:
            hg = t // 2
            mm_block(hg)
            if hg >= 1:
                norm_b(hg - 1)
        elif t >= 2:
            hg = t // 2 - 1
            if hg >= 1:
                norm_c(hg - 1)
            norm_a(hg)

    ktiles = []
    for t in range(16):
        hf = (t // 2) % 2
        j = hf * 2 + (t % 2)
        ktiles.append((xk[t * 128:(t + 1) * 128, :], hT[:, :, j * 128:(j + 1) * 128], hT_keys[j]))
    run_tiles(ktiles, after0)
    norm_c(6)
    norm_a(7)
    norm_b(7)
    norm_c(7)
    S.barrier(); chk("p0")

    cTn_all = [("cTn", kg) for kg in range(4)]
    kix_all = [("kixT", kg) for kg in range(4)]

    for g in range(2):
        if g > 0:
            DMA("sp", gpre, gpre_d, [], ["gpre"], K_MISC)
        otiles = []
        for j in range(4):
            t0 = g * GT + j * 128
            otiles.append((xo[t0:t0 + 128, :], hT[:, :, j * 128:(j + 1) * 128], hT_keys[j]))
        otiles.append((xh[g], hTh, [("hTh", kq) for kq in range(4)]))
        run_tiles(otiles)
        hTh_all = [("hTh", kq) for kq in range(4)]
        S.barrier(); chk("g%d_p1" % g)

        blocks = []
        for b_ in range(8):
            blocks.append(("qi", b_, C_QI + b_ * 128))
        for h in range(16):
            blocks.append(("q", h, C_Q + h * 128))
        for h in range(16):
            blocks.append(("ga", h, C_GA + h * 128))
        for b_ in range(16):
            blocks.append(("pi", b_, C_PI + b_ * 128))
        for b_ in range(16):
            blocks.append(("gb", b_, C_GB + b_ * 128))
        DMA("pool", wbx, w_in_v[:, :, C_WX:C_WX + 16], [], ["wbx"], K_MISC_SW)
        DMA("pool", wpool, w_pool_v, ["wbx"], ["wpool"], K_MISC_SW)
        DMA("sp", kchunk, kchunk_d, [], ["kchunk"], K_MISC)
        for j in range(4):
            for kc in range(KC):
                MM(pbs[7][:, 0:16], hT[:, kc, j * 128:(j + 1) * 128], wbx[:, kc, :], kc == 0, kc == KC - 1,
                   hT_keys[j] + ["wbx"], [("pb", 7)])
            ACT(wix_sb[:, j, :], pbs[7][:, 0:16], AF.Copy, [("pb", 7)], [("wix", j)], scale=WIX_SCALE)
        for bi, (kind, idx, c0) in enumerate(blocks):
            sl = bi % 3
            DMA("pool", wb[sl], w_in_v[:, :, c0:c0 + 128], [], [("wb", sl)], K_WB[sl])
            b = next_bank()
            for kc in range(KC):
                MM(pbs[b][:], wb[sl][:, kc, :], hT[:, kc, :], kc == 0, kc == KC - 1,
                   hT_all + [("wb", sl)], [("pb", b)])
            if kind == "qi":
                ACT(qiT[:, idx, :], pbs[b][:], AF.Copy, [("pb", b)], [("qiT", idx)], scale=IDX_SCALE)
            elif kind == "q":
                ACT(qT[:, idx, :], pbs[b][:], AF.Copy, [("pb", b)], [("qT", idx)], scale=ATTN_SCALE)
            elif kind == "ga":
                ACT(yin[:, idx, :], pbs[b][:], AF.Silu, [("pb", b)], [("yin", idx)])
            elif kind == "gb":
                ACT(yin[:, 16 + idx, :], pbs[b][:], AF.Silu, [("pb", b)], [("yin", 16 + idx)])
            else:
                for kc in range(KC):
                    MM(pbs[6][:, 0:16], wb[sl][:, kc, :], hTh[:, kc, 112:128], kc == 0, kc == KC - 1,
                       hTh_all + [("wb", sl)], [("pb", 6)])
                ps = bi % 3
                p = ptmp[ps]
                ta = ptmp[(ps + 1) % 3]
                tb = ptmp[(ps + 2) % 3]
                pk = ("ptmp", ps)
                tak = ("ptmp", (ps + 1) % 3)
                tbk = ("ptmp", (ps + 2) % 3)
                CP("act", p[:, 0:16], pbs[6][:, 0:16], [("pb", 6)], [pk])
                CP("act", p[:, 16:528], pbs[b][:], [("pb", b)], [pk])
                gp = idx // 4
                nlev = gp + 1
                win = 2 ** nlev
                src, srck = p, pk
                dsts = [(ta, tak), (tb, tbk)]
                for lev in range(nlev):
                    sh = 2 ** lev
                    dst, dstk = dsts[lev % 2]
                    lo_ = 2 ** (lev + 1) - 1
                    TT("dve", dst[:, lo_:528], src[:, lo_:528], src[:, lo_ - sh:528 - sh], ALU.add,
                       [srck], [dstk])
                    src, srck = dst, dstk
                TS("dve", src[:, 16:528], src[:, 16:528], 1.0 / win, None, ALU.mult, None, [srck], [srck])
                TT("dve", src[:, 16:32], src[:, 16:32], pfix[:, g, gp, :], ALU.mult, [srck, "cst"], [srck])
                TT("dve", mixT[:, idx, :], src[:, 16:528], p[:, 16:528], ALU.subtract, [srck, pk],
                   [("mixT", idx)])
        S.barrier(); chk("g%d_p2" % g)

        for gp in range(4):
            for dj in range(4):
                b = next_bank()
                for cc in range(4):
                    MM(pbs[b][:], wpool[:, gp * 4 + cc, dj * 128:(dj + 1) * 128], mixT[:, gp * 4 + cc, :],
                       cc == 0, cc == 3, ["wpool", ("mixT", gp * 4 + cc)], [("pb", b)])
                blk = gp * 4 + dj
                STT(yin[:, 16 + blk, :], pbs[b][:], pscale[:, blk:blk + 1], yin[:, 16 + blk, :],
                    ALU.mult, ALU.mult, [("pb", b), ("yin", 16 + blk), "cst"], [("yin", 16 + blk)])
        S.barrier(); chk("g%d_p3" % g)

        nks = [8 + 4 * g + j + 1 for j in range(4)]
        S.add("pool", lambda e: e.memset(kixAB[0][64:128, :], 0.0), [], [("kixAB", 0, 1)])
        S.add("pool", lambda e: e.memset(kixAB[1][0:64, :], 0.0), [], [("kixAB", 1, 0)])
        CP("pool", kixAB[0][0:64, :], kixT[0:64, :], kix_all, [("kixAB", 0, 0)])
        CP("pool", kixAB[1][64:128, :], kixT[64:128, :], kix_all, [("kixAB", 1, 1)])
        kixAB_keys = [("kixAB", 0, 0), ("kixAB", 0, 1), ("kixAB", 1, 0), ("kixAB", 1, 1)]
        for j in range(4):
            ncol = nks[j] * 128
            qi_ = 4 * g + j
            TS("dve", negb[j][:, 0:ncol], kchunk[:, 0:ncol], qchunk[:, qi_:qi_ + 1], -BIG, ALU.is_gt, ALU.mult,
               ["kchunk", "cst"], [("negb", j)])
        rctr = [0]
        abctr = [0]

        def emit_scores(j):
            ncol = nks[j] * 128
            dg = diag[j % 2]
            for h in range(16):
                TS("pool", dg[:, h, :], ident_bf, wix_sb[:, j, h:h + 1], 1.0, ALU.mult, ALU.mult,
                   ["identb", ("wix", j)], [("diag", j % 2, h)])
            for s0 in range(0, ncol, 512):
                wd = min(512, ncol - s0)
                ab = 6 + abctr[0] % 2
                abctr[0] += 1
                slots = {}

                def dots(h):
                    b = next_bank()
                    MM(pbs[b][:, 0:wd], qiT[:, h // 2, j * 128:(j + 1) * 128], kixAB[h % 2][:, s0:s0 + wd],
                       True, True, [("qiT", h // 2)] + kixAB_keys, [("pb", b)])
                    rs_ = rctr[0] % 4
                    rctr[0] += 1
                    slots[h] = rs_
                    if j >= 2 and h % 2 == 1:
                        TS("dve", rbuf[rs_][:, 0:wd], pbs[b][:, 0:wd], 0.0, None, ALU.max, None,
                           [("pb", b)], [("rbuf", rs_)])
                    else:
                        ACT(rbuf[rs_][:, 0:wd], pbs[b][:, 0:wd], AF.Relu, [("pb", b)], [("rbuf", rs_)])

                dots(0)
                dots(1)
                for h in range(16):
                    if h + 2 < 16:
                        dots(h + 2)
                    rs_ = slots[h]
                    MM(pbs[ab][:, 0:wd], dg[:, h, :], rbuf[rs_][:, 0:wd], h == 0, False,
                       [("diag", j % 2, h), ("rbuf", rs_)], [("pb", ab)])
                MM(pbs[ab][:, 0:wd], ident_bf, negb[j][:, s0:s0 + wd], False, True,
                   ["identb", ("negb", j)], [("pb", ab)])
                CP("act", acc[j][:, s0:s0 + wd], pbs[ab][:, 0:wd], [("pb", ab)], [("acc", j)])

        def emit_bisect(js):
            for j in js:
                S.add("dve", lambda e, j=j: e.memset(cand[:, j:j + 1], 0.0), [], [("cand", j)])
            for it in range(BIS_N):
                step = BIS_R * (2.0 ** (-it))
                last = it == BIS_N - 1
                for j in js:
                    ncol = nks[j] * 128
                    TS("dve", junk[:, 0:ncol], acc[j][:, 0:ncol], cand[:, j:j + 1], 0.0, ALU.is_ge, ALU.add,
                       [("acc", j), ("cand", j)], ["junk", ("cnt", j)], accum=cnt[:, j:j + 1])
                    TS("dve", tflag[:, j:j + 1], cnt[:, j:j + 1], TOPK - 0.5, step, ALU.is_ge, ALU.mult,
                       [("cnt", j)], [("tflag", j)])
                    STT(cand[:, j:j + 1], tflag[:, j:j + 1], (-step if last else -0.5 * step), cand[:, j:j + 1],
                        ALU.add, ALU.add, [("tflag", j), ("cand", j)], [("cand", j)])

        def emit_masks(js):
            for j in js:
                ncol = nks[j] * 128
                ms = j % 2
                TS("dve", maskbf[ms][:, 0:ncol], acc[j][:, 0:ncol], cand[:, j:j + 1], MASK_OFF, ALU.is_ge, ALU.mult,
                   [("acc", j), ("cand", j)], [("maskbf", ms)])
                for k8 in range(0, nks[j], 8):
                    n8 = min(8, nks[j] - k8)
                    b = tr_banks[tr_ctr[0] % 2]
                    tr_ctr[0] += 1
                    pt = pbT(b)
                    for i in range(n8):
                        kt = k8 + i
                        TR(pt[:, i, :], maskbf[ms][:, kt * 128:(kt + 1) * 128], ident_bf,
                           [("maskbf", ms), "identb"], [("pb", b)])
                    CP("act", maskT[:, k8:k8 + n8, j, :], pt[:, 0:n8, :], [("pb", b)], [("maskT", j)])

        emit_scores(3)
        emit_scores(2)
        emit_bisect([3, 2])
        emit_scores(1)
        emit_scores(0)
        emit_masks([3, 2])
        emit_bisect([1, 0])
        emit_masks([1, 0])
        S.barrier(); chk("g%d_p4" % g)

        nkmax = 8 + 4 * g + 4
        mask_all = [("maskT", j) for j in range(4)]
        items = [(h, kt) for h in range(16) for kt in range(nkmax)]
        LA = 2
        SB = [0, 1, 2]
        OB = [3, 6]
        kvb = [0]

        def dma_k(h):
            sl = h % 2
            DMA("pool", wuk[sl], w_uk[h].rearrange("(cc p) d -> p cc d", p=128), [], [("wuk", sl)], K_UK[sl])

        def mm_k(h):
            sl = h % 2
            for s0 in range(0, nkmax * 128, 512):
                b = 4 + kvb[0] % 2
                kvb[0] += 1
                for cc in range(4):
                    MM(pbs[b][:], wuk[sl][:, cc, :], cTn[:, cc, s0:s0 + 512], cc == 0, cc == 3,
                       [("wuk", sl)] + cTn_all, [("pb", b)])
                CP("act", kTh[sl][:, s0:s0 + 512], pbs[b][:], [("pb", b)], [("kTh", sl, s0 // 512)])

        def dma_v4(hgp):
            sl = hgp % 2
            for hh in range(4):
                DMA("pool", wuv4[sl][:, :, hh * 128:(hh + 1) * 128],
                    w_uv[4 * hgp + hh].rearrange("(cc p) d -> p cc d", p=128),
                    [], [("wuv4", sl, hh)], K_UV4[sl][hh])

        def mm_v4(hgp):
            sl = hgp % 2
            for kt in range(nkmax):
                b = 4 + kvb[0] % 2
                kvb[0] += 1
                for cc in range(4):
                    MM(pbs[b][:], cTn[:, cc, kt * 128:(kt + 1) * 128], wuv4[sl][:, cc, :],
                       cc == 0, cc == 3, [("wuv4", sl, hh_) for hh_ in range(4)] + cTn_all, [("pb", b)])
                eng = "dve" if kt % 2 == 0 else "act"
                CP(eng, vS4[sl][:, kt, :], pbs[b][:], [("pb", b)], [("vS4", sl, kt // 4)])

        def emit_qk(i):
            h, kt = items[i]
            sl = h % 2
            jmin = max(0, kt - 8 - 4 * g)
            c0 = jmin * 128
            sb_ = SB[i % 3]
            es = i % 3
            MM(pbs[sb_][:, c0:GT], kTh[sl][:, kt * 128:(kt + 1) * 128], qT[:, h, c0:GT], True, False,
               [("kTh", sl, kt // 4), ("qT", h)], [("pb", sb_)])
            MM(pbs[sb_][:, c0:GT], ident_bf, maskT[:, kt, jmin:4, :].rearrange("p j t -> p (j t)"), False, True,
               ["identb"] + mask_all, [("pb", sb_)])
            ACT(eT[es][:, c0:GT], pbs[sb_][:, c0:GT], AF.Exp, [("pb", sb_), "cst"], [("eT", es)], bias=negoff)

        def emit_pv(i):
            h, kt = items[i]
            sl = h % 2
            jmin = max(0, kt - 8 - 4 * g)
            c0 = jmin * 128
            es = i % 3
            ob = OB[h % 2]
            vsl = (h // 4) % 2
            MM(pbs[ob][:, c0:GT], vS4[vsl][:, kt, (h % 4) * 128:(h % 4 + 1) * 128], eT[es][:, c0:GT],
               kt == 0, kt == nkmax - 1, [("vS4", vsl, kt // 4), ("eT", es)], [("pb", ob)])
            MM(pbs[7][:, c0:GT], ones_bf, eT[es][:, c0:GT], kt == 0, kt == nkmax - 1,
               ["onesb", ("eT", es)], [("pb", 7)])
            if kt == nkmax - 1:
                os_ = h % 2
                RCP(rcb[os_], pbs[7][:], [("pb", 7)], [("rcb", os_)])
                TT("dve", otmp[os_], pbs[ob][:], rcb[os_], ALU.mult, [("pb", ob), ("rcb", os_)], [("otmp", os_)])
                TT("pool", yin[:, h, :], otmp[os_], yin[:, h, :], ALU.mult,
                   [("otmp", os_), ("yin", h)], [("yin", h)])

        dma_k(0)
        dma_k(1)
        dma_v4(0)
        mm_k(0)
        mm_v4(0)
        for i in range(LA):
            emit_qk(i)
        for i in range(len(items)):
            h, kt = items[i]
            if kt == 0:
                if h + 2 < 16:
                    dma_k(h + 2)
                if h % 4 == 0 and h // 4 + 1 < 4:
                    dma_v4(h // 4 + 1)
                if h + 1 < 16:
                    mm_k(h + 1)
                if h % 4 == 1 and h // 4 + 1 < 4:
                    mm_v4(h // 4 + 1)
            if i + LA < len(items):
                emit_qk(i + LA)
            emit_pv(i)
        S.barrier(); chk("g%d_p5" % g)

        yin_all = [("yin", i) for i in range(32)]
        def ssq_mm(db):
            qs = db % 2
            for j in range(4):
                MM(pbs[6][:, j:j + 1], sqb[qs][:, j * 128:(j + 1) * 128], ones_bf[:, 0:1],
                   db == 0 and j == 0, db == 31 and j == 3, [("sqb", qs), "onesb"], [("pb", 6)])

        for db in range(32):
            sl = db % 3
            DMA("pool", wb6[sl], w_out_v[:, :, db * 128:(db + 1) * 128], [], [("wb6", sl)], K_WB[sl])
            b = next_bank()
            for ec in range(KC):
                MM(pbs[b][:], wb6[sl][:, ec, :], yin[:, ec, :], ec == 0, ec == KC - 1,
                   yin_all + [("wb6", sl)], [("pb", b)])
            if db > 0:
                ssq_mm(db - 1)
            qs = db % 2
            ACT(sqb[qs], pbs[b][:], AF.Square, [("pb", b)], [("sqb", qs)])
            TS("dve", yg[:, db, :], pbs[b][:], gpost[:, db:db + 1], None, ALU.mult, None,
               [("pb", b), "cst"], [("yg", db)])
        ssq_mm(31)
        ACT(sdo, pbs[6][:, 0:4], AF.Sqrt, [("pb", 6), "cst"], ["sdo"], scale=1.0 / D, bias=epsT)
        RCP(rstdo, sdo, ["sdo"], ["rstdo"])
        yg_all = [("yg", db) for db in range(32)]
        for j in range(4):
            sl = xt_ctr[0] % 2
            xt_ctr[0] += 1
            t0 = g * GT + j * 128
            DMA("sp", xt[sl], xo[t0:t0 + 128, :], [], [("xt", sl)], K_XT[sl])
            for d4 in range(8):
                b = tr_banks[tr_ctr[0] % 2]
                tr_ctr[0] += 1
                for i in range(4):
                    db = d4 * 4 + i
                    TR(pbs[b][:, i * 128:(i + 1) * 128], yg[:, db, j * 128:(j + 1) * 128], ident_f,
                       [("yg", db), "identf"], [("pb", b)])
                STT(xt[sl][:, d4 * 512:(d4 + 1) * 512], pbs[b][:], rstdo[:, j:j + 1],
                    xt[sl][:, d4 * 512:(d4 + 1) * 512], ALU.mult, ALU.add,
                    [("pb", b), "rstdo", ("xt", sl)], [("xt", sl)])
            DMA("sp", yout[t0:t0 + 128, :], xt[sl], [("xt", sl)], [("yout", g, j)], K_ST[sl])
        S.barrier(); chk("g%d_p6" % g)

    S.emit(st)
    st.close()
    return nc


_NC_CACHE = {}
_ONLY_MAPS = False


def _get_nc():
    if "nc" not in _NC_CACHE:
        _NC_CACHE["nc"] = build_nc()
    return _NC_CACHE["nc"]


def kernel(x, pre_norm, w_in, kv_norm, w_uk, w_uv, idx_k_norm_g, idx_k_norm_b, w_pool, pool_scale, w_out,
           post_norm):
    f32 = np.float32
    x = np.asarray(x, f32)
    B = x.shape[0]
    w_in0 = np.ascontiguousarray(np.asarray(w_in, f32)[0])
    w_out0 = np.ascontiguousarray(np.asarray(w_out, f32)[0])
    w_uk0 = np.ascontiguousarray(np.asarray(w_uk, f32)[0])
    w_uv0 = np.ascontiguousarray(np.asarray(w_uv, f32)[0])
    w_pool0 = np.ascontiguousarray(np.asarray(w_pool, f32)[0])
    gpre_b = np.ascontiguousarray(np.broadcast_to(np.asarray(pre_norm, f32)[0][None, :], (128, D)))
    kchunk_b = np.ascontiguousarray(np.broadcast_to((np.arange(SEQ) // 64).astype(f32)[None, :], (128, SEQ)))
    ident_f = np.eye(128, dtype=f32)
    ident_bf = np.eye(128, dtype=f32).astype(ml_dtypes.bfloat16)
    blk64 = np.zeros((128, 128), f32)
    blk64[0:64, 0:64] = 1.0 / 64
    blk64[64:128, 64:128] = 1.0 / 64
    cst0 = np.zeros((128, 512), f32)
    cst0[:, 0:4] = np.asarray(kv_norm, f32)[0].reshape(4, 128).T
    kg = np.asarray(idx_k_norm_g, f32)[0]
    kb = np.asarray(idx_k_norm_b, f32)[0]
    cst0[:, 4] = np.concatenate([kg, kg])
    cst0[:, 5] = np.concatenate([kb, kb])
    cst0[:, 6] = EPS
    cst0[:, 7] = -MASK_OFF
    cst0[:, 8:24] = np.asarray(pool_scale, f32)[0].reshape(16, 128).T
    cst0[:, 24:56] = np.asarray(post_norm, f32)[0].reshape(32, 128).T
    wins = [2, 4, 8, 16]
    in_maps = []
    for b in range(B):
        for half in range(2):
            cst = cst0.copy()
            tok = half * 1024 + np.arange(1024)
            cst[:, 56:64] = (tok // 64).astype(f32).reshape(8, 128).T
            pf = np.ones((2, 4, 16), f32)
            if half == 0:
                for gi, wdw in enumerate(wins):
                    t = np.arange(16)
                    pf[0, gi, :] = wdw / np.minimum(t + 1, wdw)
            cst[:, 64:192] = pf.reshape(1, 128)
            xo = np.ascontiguousarray(x[b, half * 1024:(half + 1) * 1024])
            xh = np.zeros((2, 128, D), f32)
            if half == 1:
                xh[0] = x[b, 896:1024]
            xh[1] = xo[384:512]
            in_maps.append({
                "xk": np.ascontiguousarray(x[b]), "xo": xo, "xh": xh,
                "w_in": w_in0, "w_out": w_out0, "w_uk": w_uk0, "w_uv": w_uv0, "w_pool": w_pool0,
                "gpre_b": gpre_b, "kchunk_b": kchunk_b, "cst": cst, "ident_f": ident_f, "blk64": blk64,
                "ident_bf": ident_bf,
            })
    if _ONLY_MAPS:
        return in_maps
    nc = _get_nc()
    res = run_bass_kernel_spmd(nc, in_maps, core_ids=list(range(2 * B)))
    out = np.zeros((B, SEQ, D), f32)
    for b in range(B):
        for half in range(2):
            out[b, half * 1024:(half + 1) * 1024] = res.results[2 * b + half]["y"]
    return out
```

```python
from contextlib import ExitStack
import numpy as np
import ml_dtypes
import concourse.bass as bass
import concourse.mybir as mybir
from concourse.bass_utils import run_bass_kernel_spmd

F32 = mybir.dt.float32
BF16 = mybir.dt.bfloat16
ALU = mybir.AluOpType
AF = mybir.ActivationFunctionType

ENGS = ("pe", "act", "dve", "pool", "sp")
N_DMA_SEMS = 22

D = 4096
DIN = 9808
SEQ = 2048
KC = 32
GT = 512
C_Q, C_C, C_QI, C_KX, C_WX, C_GA, C_PI, C_GB = 0, 2048, 2560, 3584, 3648, 3664, 5712, 7760
EPS = 1e-6
ATTN_SCALE = 128 ** -0.5
IDX_SCALE = 64 ** -0.5
WIX_SCALE = 16 ** -0.5
BIG = 1.0e30
BIS_R = 16.0
BIS_N = 21
TOPK = 256
MASK_OFF = 100.0


class Op:
    __slots__ = ("eng", "fn", "deps", "dma", "sig", "sem", "val", "idx")

    def __init__(self, eng, fn, dma):
        self.eng = eng
        self.fn = fn
        self.deps = set()
        self.dma = dma
        self.sig = False
        self.sem = None
        self.val = 0


class Sched:
    def __init__(self, nc):
        self.nc = nc
        self.ops = []
        self.lastw = {}
        self.readers = {}
        self.fence = set()
        self.last_eng = {}
        self.last_dma = {}
        self.frozen = False

    def add(self, eng, fn, reads=(), writes=(), dma=None):
        if self.frozen:
            return None
        op = Op(eng, fn, dma)
        idx = len(self.ops)
        op.idx = idx
        if eng != "pe":
            extra = [("pbx", r[1]) for r in reads if isinstance(r, tuple) and r[0] == "pb"]
            if extra:
                writes = list(writes) + extra
        for r in reads:
            w = self.lastw.get(r)
            if w is not None:
                op.deps.add(w)
        for w_ in writes:
            w = self.lastw.get(w_)
            if w is not None:
                op.deps.add(w)
            for rd in self.readers.get(w_, ()):
                op.deps.add(rd)
        for r in reads:
            self.readers.setdefault(r, []).append(idx)
        for w_ in writes:
            self.lastw[w_] = idx
            self.readers[w_] = []
        op.deps |= self.fence
        op.deps.discard(idx)
        self.ops.append(op)
        if dma is None:
            self.last_eng[eng] = idx
        else:
            self.last_dma[dma] = idx
        return idx

    def barrier(self):
        self.fence = set(self.last_eng.values()) | set(self.last_dma.values())

    def emit(self, stack):
        nc = self.nc
        ops = self.ops

        def skip(dop, op):
            return dop.eng == "pe" and op.eng == "pe" and dop.dma is None and op.dma is None

        for op in ops:
            for d in op.deps:
                if not skip(ops[d], op):
                    ops[d].sig = True
        for op in ops:
            if op.dma is not None:
                op.sig = True
        esem = {e: stack.enter_context(nc.semaphore("s_" + e)) for e in ENGS}
        dsem = [stack.enter_context(nc.semaphore("d_%d" % i)) for i in range(N_DMA_SEMS)]
        ecount = {e: 0 for e in ENGS}
        dcount = [0] * N_DMA_SEMS
        for op in ops:
            if not op.sig:
                continue
            if op.dma is not None:
                dcount[op.dma] += 16
                op.sem = dsem[op.dma]
                op.val = dcount[op.dma]
            else:
                ecount[op.eng] += 1
                op.sem = esem[op.eng]
                op.val = ecount[op.eng]
        per_eng = {e: [] for e in ENGS}
        for op in ops:
            per_eng[op.eng].append(op)
        dfinal = list(dcount)
        block = stack.enter_context(nc.Block())

        def make_body(e):
            def body(eng):
                waited = {}
                for op in per_eng[e]:
                    need = {}
                    for d in op.deps:
                        dop = ops[d]
                        if skip(dop, op):
                            continue
                        k = id(dop.sem)
                        if k not in need or need[k][1] < dop.val:
                            need[k] = (dop.sem, dop.val)
                    for k, (sem, val) in need.items():
                        if waited.get(k, 0) >= val:
                            continue
                        eng.wait_ge(sem, val)
                        waited[k] = val
                    ins = op.fn(eng)
                    if op.sig:
                        ins.then_inc(op.sem, 16 if op.dma is not None else 1)
                if e == "sp":
                    for i in range(N_DMA_SEMS):
                        if dfinal[i] > 0:
                            eng.wait_ge(dsem[i], dfinal[i])
            return body

        block.tensor(make_body("pe"))
        block.scalar(make_body("act"))
        block.vector(make_body("dve"))
        block.gpsimd(make_body("pool"))
        block.sync(make_body("sp"))


class _Stop(Exception):
    pass


STOP = None


def build_nc():
    nc = bass.Bass("TRN2", target_bir_lowering=False)

    def chk(label):
        if STOP == label:
            S.frozen = True

    def din(name, shape, dt=F32):
        return nc.dram_tensor(name, list(shape), dt, kind="ExternalInput").ap()

    xk = din("xk", [SEQ, D])
    xo = din("xo", [1024, D])
    xh = din("xh", [2, 128, D])
    w_in = din("w_in", [D, DIN])
    w_out = din("w_out", [D, D])
    w_uk = din("w_uk", [16, 512, 128])
    w_uv = din("w_uv", [16, 512, 128])
    w_pool = din("w_pool", [4, 512, 512])
    gpre_d = din("gpre_b", [128, D])
    kchunk_d = din("kchunk_b", [128, SEQ])
    cst_d = din("cst", [128, 512])
    identf_d = din("ident_f", [128, 128])
    blk64_d = din("blk64", [128, 128])
    identb_d = din("ident_bf", [128, 128], BF16)
    yout = nc.dram_tensor("y", [1024, D], F32, kind="ExternalOutput").ap()

    w_in_v = w_in.rearrange("(kc p) e -> p kc e", p=128)
    w_out_v = w_out.rearrange("(kc p) e -> p kc e", p=128)
    w_pool_v = w_pool.rearrange("g (cc p) d -> p (g cc) d", p=128)

    st = ExitStack()
    st.enter_context(nc.allow_low_precision("bf16 matmul operands, fp32 accumulation"))
    ACOLS = 49152
    arena = st.enter_context(nc.sbuf_tensor("arena", [128, ACOLS], F32))
    pbs = [st.enter_context(nc.psum_tensor("pb%d" % i, [128, 512], F32)) for i in range(8)]
    S = Sched(nc)

    def fv(off, n):
        assert off + n <= ACOLS
        return arena[:, off:off + n]

    def bvw(off, ncols):
        assert off + ncols <= ACOLS
        return arena[:, off:off + ncols].bitcast(BF16)

    def pbT(i):
        return pbs[i][:].bitcast(BF16).rearrange("p (a b) -> p a b", b=128)

    cTn = bvw(0, 4096).rearrange("p (a b) -> p a b", b=SEQ)
    kixT = bvw(4096, 1024)
    o = 5120
    cst = fv(o, 512); o += 512
    ident_f = fv(o, 128); o += 128
    blk64 = fv(o, 128); o += 128
    ones_f = fv(o, 128); o += 128
    ident_bf = bvw(o, 64); o += 64
    ones_bf = bvw(o, 64); o += 64
    assert o <= 6144
    gkv = cst[:, 0:4]
    kxg = cst[:, 4:5]
    kxb = cst[:, 5:6]
    epsT = cst[:, 6:7]
    negoff = cst[:, 7:8]
    pscale = cst[:, 8:24]
    gpost = cst[:, 24:56]
    qchunk = cst[:, 56:64]
    pfix = cst[:, 64:192].rearrange("p (g a b) -> p g a b", g=2, a=4)
    ss2 = cst[:, 192:194]
    sd2 = cst[:, 194:196]
    rstd2 = cst[:, 196:198]
    wix_sb = cst[:, 200:264].rearrange("p (j h) -> p j h", h=16)
    cand = cst[:, 264:268]
    cnt = cst[:, 268:272]
    tflag = cst[:, 272:276]
    sdo = cst[:, 276:280]
    rstdo = cst[:, 280:284]

    YIN0, QT0, QIT0, HT0, HTH0, R10 = 6144, 14336, 18432, 20480, 28672, 30720
    yin = bvw(YIN0, 8192).rearrange("p (a b) -> p a b", b=GT)
    qT = bvw(QT0, 4096).rearrange("p (a b) -> p a b", b=GT)
    qiT = bvw(QIT0, 2048).rearrange("p (a b) -> p a b", b=GT)
    hT = bvw(HT0, 8192).rearrange("p (a b) -> p a b", b=GT)
    hTh = bvw(HTH0, 2048).rearrange("p (a b) -> p a b", b=128)
    xt = [fv(R10 + i * 4096, 4096) for i in range(2)]
    xsb = [bvw(R10 + 8192 + i * 2048, 2048) for i in range(2)]
    gpre = fv(R10 + 12288, 4096)
    wb = [bvw(R10 + i * 2048, 2048).rearrange("p (a b) -> p a b", b=128) for i in range(3)]
    ptmp = [fv(R10 + 6144 + i * 528, 528) for i in range(3)]
    wbx = bvw(R10 + 7744, 256).rearrange("p (a b) -> p a b", b=16)
    mixT = bvw(R10 + 8192, 4096).rearrange("p (a b) -> p a b", b=GT)
    wck_c = bvw(6144, 8192).rearrange("p (a b) -> p a b", b=512)
    wck_k = bvw(14336, 2048).rearrange("p (a b) -> p a b", b=128)
    csb = fv(16384, 2048).rearrange("p (a b) -> p a b", b=512)
    sqc = fv(18432, 2048).rearrange("p (a b) -> p a b", b=512)
    P0M = R10 + 16384
    kxsb = fv(P0M, 512)
    xcb = fv(P0M + 512, 512)
    sq2b = fv(P0M + 1024, 512)
    rsb = fv(P0M + 1536, 512)
    sdb = sq2b
    knb = xcb
    wpool = bvw(R10 + 12288, 4096).rearrange("p (a b) -> p a b", b=512)
    acc = [fv(R10 + j * 2048, 2048) for j in range(4)]
    negb = [bvw(R10 + 8192 + j * 1024, 1024) for j in range(4)]
    rbuf = [bvw(R10 + 12288 + i * 256, 256) for i in range(4)]
    junk = bvw(R10 + 13312, 1024)
    maskbf = [bvw(R10 + 14336 + i * 1024, 1024) for i in range(2)]
    kixAB = [bvw(HTH0 + i * 1024, 1024) for i in range(2)]
    kchunk = fv(R10 + 16384, 2048)
    maskT = bvw(HT0, 4096).rearrange("p (k j t) -> p k j t", k=16, j=4)
    diag = [bvw(HT0 + 4096 + i * 1024, 1024).rearrange("p (a b) -> p a b", b=128) for i in range(2)]
    kTh = [bvw(R10 + i * 1024, 1024) for i in range(2)]
    vS4 = [bvw(R10 + 2048 + i * 4096, 4096).rearrange("p (a b) -> p a b", b=512) for i in range(2)]
    wuk = [bvw(R10 + 10240 + i * 256, 256).rearrange("p (a b) -> p a b", b=128) for i in range(2)]
    wuv4 = [bvw(R10 + 10752 + i * 1024, 1024).rearrange("p (a b) -> p a b", b=512) for i in range(2)]
    eT = [bvw(R10 + 12800 + i * 256, 256) for i in range(3)]
    pT = [bvw(R10 + 13568 + i * 256, 256) for i in range(3)]
    rcb = [fv(R10 + 14336 + i * 512, 512) for i in range(2)]
    otmp = [fv(R10 + 15360 + i * 512, 512) for i in range(2)]
    yg = fv(QT0, 16384).rearrange("p (a b) -> p a b", b=GT)
    wb6 = [bvw(R10 + 8192 + i * 2048, 2048).rearrange("p (a b) -> p a b", b=128) for i in range(3)]
    sqb = [bvw(R10 + 14336 + i * 256, 256) for i in range(2)]

    def MM(out, lhsT, rhs, start, stop, r, w):
        S.add("pe", lambda e: e.matmul(out, lhsT, rhs, start=start, stop=stop), r, w)

    def TR(out, in_, ident, r, w):
        S.add("pe", lambda e: e.transpose(out, in_, ident), r, w)

    def ACT(out, in_, func, r, w, scale=None, bias=None, accum=None):
        kw = {}
        if scale is not None:
            kw["scale"] = scale
        if bias is not None:
            kw["bias"] = bias
        if accum is not None:
            kw["accum_out"] = accum
        S.add("act", lambda e: e.activation(out, in_, func, **kw), r, w)

    def TS(eng, out, in0, s1, s2, op0, op1, r, w, accum=None):
        if op1 is None:
            if accum is None:
                S.add(eng, lambda e: e.tensor_scalar(out, in0, s1, None, op0), r, w)
            else:
                raise ValueError
        else:
            if accum is None:
                S.add(eng, lambda e: e.tensor_scalar(out, in0, s1, s2, op0, op1), r, w)
            else:
                S.add(eng, lambda e: e.tensor_scalar(out, in0, s1, s2, op0, op1, accum_out=accum), r, w)

    def TT(eng, out, in0, in1, op, r, w):
        S.add(eng, lambda e: e.tensor_tensor(out, in0, in1, op), r, w)

    def STT(out, in0, scalar, in1, op0, op1, r, w):
        S.add("dve", lambda e: e.scalar_tensor_tensor(out, in0, scalar, in1, op0, op1), r, w)

    def RCP(out, in_, r, w):
        S.add("dve", lambda e: e.reciprocal(out, in_), r, w)

    def CP(eng, out, in_, r, w):
        if eng == "act":
            S.add("act", lambda e: e.copy(out, in_), r, w)
        else:
            S.add(eng, lambda e: e.tensor_copy(out, in_), r, w)

    def DMA(eng, out, in_, r, w, key):
        S.add(eng, lambda e: e.dma_start(out=out, in_=in_), r, w, dma=key)

    K_XT = [0, 1]
    K_WB = [2, 3, 4]
    K_ST = [5, 6]
    K_UK = [7, 8]
    K_UV = [9, 10]
    K_MISC = 11
    K_MISC_SW = 12
    K_UV4 = [[13, 14, 15, 16], [17, 18, 19, 20]]

    DMA("sp", cst, cst_d, [], ["cst"], K_MISC)
    DMA("sp", ident_f, identf_d, [], ["identf"], K_MISC)
    DMA("sp", blk64, blk64_d, [], ["blk64"], K_MISC)
    DMA("sp", ident_bf, identb_d, [], ["identb"], K_MISC)
    S.add("dve", lambda e: e.memset(ones_f, 1.0), [], ["onesf"])
    S.add("dve", lambda e: e.memset(ones_bf, 1.0), [], ["onesb"])
    S.barrier(); chk("setup")

    tr_banks = [0, 1]
    tr_ctr = [0]
    xt_ctr = [0]

    def stage1(src, sl):
        xs = xsb[sl]
        xk_ = ("xs", sl)
        DMA("sp", xt[sl], src, [], [("xt", sl)], K_XT[sl])
        ACT(xs, xt[sl], AF.Square, [("xt", sl)], [xk_, ("ss", sl)], accum=ss2[:, sl:sl + 1])
        ACT(sd2[:, sl:sl + 1], ss2[:, sl:sl + 1], AF.Sqrt, [("ss", sl), "cst"], [("sd", sl)],
            scale=1.0 / D, bias=epsT)
        RCP(rstd2[:, sl:sl + 1], sd2[:, sl:sl + 1], [("sd", sl)], [("rstd", sl)])
        STT(xs, xt[sl], rstd2[:, sl:sl + 1], gpre, ALU.mult, ALU.mult,
            [("xt", sl), ("rstd", sl), "gpre"], [xk_])

    def stage2(sl, dst, dst_keys):
        xs = xsb[sl]
        xk_ = ("xs", sl)
        for kq in range(4):
            b = tr_banks[tr_ctr[0] % len(tr_banks)]
            tr_ctr[0] += 1
            pt = pbT(b)
            for i in range(8):
                kc = kq * 8 + i
                TR(pt[:, i, :], xs[:, kc * 128:(kc + 1) * 128], ident_bf, [xk_, "identb"], [("pb", b)])
            eng = "act" if kq % 2 == 0 else "dve"
            CP(eng, dst[:, kq * 8:(kq + 1) * 8, :], pt, [("pb", b)], [dst_keys[kq]])

    def run_tiles(tiles, after=None):
        n = len(tiles)
        sls = []
        for s_ in range(n + 1):
            if s_ < n:
                sl = xt_ctr[0] % 2
                xt_ctr[0] += 1
                sls.append(sl)
                stage1(tiles[s_][0], sl)
            if s_ >= 1:
                t = s_ - 1
                stage2(sls[t], tiles[t][1], tiles[t][2])
                if after is not None:
                    after(t)

    acc_banks = [2, 3, 4, 5]
    acc_ctr = [0]

    def next_bank():
        b = acc_banks[acc_ctr[0] % 4]
        acc_ctr[0] += 1
        return b

    DMA("sp", gpre, gpre_d, [], ["gpre"], K_MISC)
    DMA("pool", wck_c, w_in_v[:, :, C_C:C_C + 512], [], ["wckc"], K_WB[0])
    DMA("pool", wck_k[:, :, 0:64], w_in_v[:, :, C_KX:C_KX + 64], [], ["wckk0"], K_WB[1])
    DMA("pool", wck_k[:, :, 64:128], w_in_v[:, :, C_KX:C_KX + 64], [], ["wckk1"], K_WB[2])
    chk("p0a")
    hT_keys = [[("hT", kq, j) for kq in range(4)] for j in range(4)]
    hT_all = [k for ks in hT_keys for k in ks]
    HW = 256

    def hg_info(hg):
        hf = hg % 2
        hc = slice(hf * HW, (hf + 1) * HW)
        hkeys = hT_keys[hf * 2] + hT_keys[hf * 2 + 1]
        cols = slice(hg * HW, (hg + 1) * HW)
        return hc, hkeys, cols

    def mm_block(hg):
        hc, hkeys, cols = hg_info(hg)
        for blk in range(4):
            b = 2 + blk
            for kc in range(KC):
                MM(pbs[b][:, 0:HW], wck_c[:, kc, blk * 128:(blk + 1) * 128], hT[:, kc, hc], kc == 0, kc == KC - 1,
                   hkeys + ["wckc"], [("pb", b)])
        for kc in range(KC):
            MM(pbs[6][:, 0:HW], wck_k[:, kc, :], hT[:, kc, hc], kc == 0, kc == KC - 1,
               hkeys + ["wckk0", "wckk1"], [("pb", 6)])

    def norm_a(hg):
        for blk in range(4):
            b = 2 + blk
            CP("dve", csb[:, blk, 0:HW], pbs[b][:, 0:HW], [("pb", b)], [("csb", blk)])
            ACT(sqc[:, blk, 0:HW], pbs[b][:, 0:HW], AF.Square, [("pb", b)], [("sqc", blk)])
        CP("dve", kxsb[:, 0:HW], pbs[6][:, 0:HW], [("pb", 6)], ["kxsb"])
        for blk in range(4):
            MM(pbs[7][:, 0:HW], ones_f, sqc[:, blk, 0:HW], blk == 0, blk == 3, [("sqc", blk), "onesf"], [("pb", 7)])
        MM(pbs[6][:, 0:HW], blk64, kxsb[:, 0:HW], True, True, ["kxsb", "blk64"], [("pb", 6)])
        TT("dve", xcb[:, 0:HW], kxsb[:, 0:HW], pbs[6][:, 0:HW], ALU.subtract, ["kxsb", ("pb", 6)], ["xcb"])

    def norm_b(hg):
        hc, hkeys, cols = hg_info(hg)
        ACT(sdb[:, 0:HW], pbs[7][:, 0:HW], AF.Sqrt, [("pb", 7), "cst"], ["sq2b"], scale=1.0 / 512, bias=epsT)
        RCP(rsb[:, 0:HW], sdb[:, 0:HW], ["sq2b"], ["rsb"])
        for blk in range(4):
            STT(cTn[:, blk, cols], csb[:, blk, 0:HW], gkv[:, blk:blk + 1], rsb[:, 0:HW], ALU.mult, ALU.mult,
                [("csb", blk), "rsb", "cst"], [("cTn", hg // 2)])
        ACT(sq2b[:, 0:HW], xcb[:, 0:HW], AF.Square, ["xcb"], ["sq2b"])
        MM(pbs[7][:, 0:HW], blk64, sq2b[:, 0:HW], True, True, ["sq2b", "blk64"], [("pb", 7)])

    def norm_c(hg):
        hc, hkeys, cols = hg_info(hg)
        ACT(sdb[:, 0:HW], pbs[7][:, 0:HW], AF.Sqrt, [("pb", 7), "cst"], ["sq2b"], scale=1.0, bias=epsT)
        RCP(rsb[:, 0:HW], sdb[:, 0:HW], ["sq2b"], ["rsb"])
        TT("dve", knb[:, 0:HW], xcb[:, 0:HW], rsb[:, 0:HW], ALU.mult, ["xcb", "rsb"], ["xcb"])
        TS("dve", kixT[:, cols], knb[:, 0:HW], kxg, kxb, ALU.mult, ALU.add, ["xcb", "cst"], [("kixT", hg // 2)])

    def after0(t):
        if t % 2 == 1:
            hg = t // 2
            mm_block(hg)
            if hg >= 1:
                norm_b(hg - 1)
        elif t >= 2:
            hg = t // 2 - 1
            if hg >= 1:
                norm_c(hg - 1)
            norm_a(hg)

    ktiles = []
    for t in range(16):
        hf = (t // 2) % 2
        j = hf * 2 + (t % 2)
        ktiles.append((xk[t * 128:(t + 1) * 128, :], hT[:, :, j * 128:(j + 1) * 128], hT_keys[j]))
    run_tiles(ktiles, after0)
    norm_c(6)
    norm_a(7)
    norm_b(7)
    norm_c(7)
    S.barrier(); chk("p0")

    cTn_all = [("cTn", kg) for kg in range(4)]
    kix_all = [("kixT", kg) for kg in range(4)]

    for g in range(2):
        if g > 0:
            DMA("sp", gpre, gpre_d, [], ["gpre"], K_MISC)
        otiles = []
        for j in range(4):
            t0 = g * GT + j * 128
            otiles.append((xo[t0:t0 + 128, :], hT[:, :, j * 128:(j + 1) * 128], hT_keys[j]))
        otiles.append((xh[g], hTh, [("hTh", kq) for kq in range(4)]))
        tr_banks[:] = [0, 1, 2, 3, 4, 5]
        run_tiles(otiles)
        tr_banks[:] = [0, 1]
        hTh_all = [("hTh", kq) for kq in range(4)]
        S.barrier(); chk("g%d_p1" % g)

        blocks = []
        for b_ in range(8):
            blocks.append(("qi", b_, C_QI + b_ * 128))
        for h in range(16):
            blocks.append(("q", h, C_Q + h * 128))
        for h in range(16):
            blocks.append(("ga", h, C_GA + h * 128))
        for b_ in range(16):
            blocks.append(("pi", b_, C_PI + b_ * 128))
        for b_ in range(16):
            blocks.append(("gb", b_, C_GB + b_ * 128))
        DMA("pool", wbx, w_in_v[:, :, C_WX:C_WX + 16], [], ["wbx"], K_MISC_SW)
        DMA("pool", wpool, w_pool_v, ["wbx"], ["wpool"], K_MISC_SW)
        DMA("sp", kchunk, kchunk_d, [], ["kchunk"], K_MISC)
        for j in range(4):
            for kc in range(KC):
                MM(pbs[7][:, 0:16], hT[:, kc, j * 128:(j + 1) * 128], wbx[:, kc, :], kc == 0, kc == KC - 1,
                   hT_keys[j] + ["wbx"], [("pb", 7)])
            ACT(wix_sb[:, j, :], pbs[7][:, 0:16], AF.Copy, [("pb", 7)], [("wix", j)], scale=WIX_SCALE)
        for bi, (kind, idx, c0) in enumerate(blocks):
            sl = bi % 3
            DMA("pool", wb[sl], w_in_v[:, :, c0:c0 + 128], [], [("wb", sl)], K_WB[sl])
            b = next_bank()
            for kc in range(KC):
                MM(pbs[b][:], wb[sl][:, kc, :], hT[:, kc, :], kc == 0, kc == KC - 1,
                   hT_all + [("wb", sl)], [("pb", b)])
            if kind == "qi":
                ACT(qiT[:, idx, :], pbs[b][:], AF.Copy, [("pb", b)], [("qiT", idx)], scale=IDX_SCALE)
            elif kind == "q":
                ACT(qT[:, idx, :], pbs[b][:], AF.Copy, [("pb", b)], [("qT", idx)], scale=ATTN_SCALE)
            elif kind == "ga":
                ACT(yin[:, idx, :], pbs[b][:], AF.Silu, [("pb", b)], [("yin", idx)])
            elif kind == "gb":
                ACT(yin[:, 16 + idx, :], pbs[b][:], AF.Silu, [("pb", b)], [("yin", 16 + idx)])
            else:
                for kc in range(KC):
                    MM(pbs[6][:, 0:16], wb[sl][:, kc, :], hTh[:, kc, 112:128], kc == 0, kc == KC - 1,
                       hTh_all + [("wb", sl)], [("pb", 6)])
                ps = bi % 3
                p = ptmp[ps]
                ta = ptmp[(ps + 1) % 3]
                tb = ptmp[(ps + 2) % 3]
                pk = ("ptmp", ps)
                tak = ("ptmp", (ps + 1) % 3)
                tbk = ("ptmp", (ps + 2) % 3)
                CP("act", p[:, 0:16], pbs[6][:, 0:16], [("pb", 6)], [pk])
                CP("act", p[:, 16:528], pbs[b][:], [("pb", b)], [pk])
                gp = idx // 4
                nlev = gp + 1
                win = 2 ** nlev
                src, srck = p, pk
                dsts = [(ta, tak), (tb, tbk)]
                for lev in range(nlev):
                    sh = 2 ** lev
                    dst, dstk = dsts[lev % 2]
                    lo_ = 2 ** (lev + 1) - 1
                    TT("dve", dst[:, lo_:528], src[:, lo_:528], src[:, lo_ - sh:528 - sh], ALU.add,
                       [srck], [dstk])
                    src, srck = dst, dstk
                TS("dve", src[:, 16:528], src[:, 16:528], 1.0 / win, None, ALU.mult, None, [srck], [srck])
                TT("dve", src[:, 16:32], src[:, 16:32], pfix[:, g, gp, :], ALU.mult, [srck, "cst"], [srck])
                TT("dve", mixT[:, idx, :], src[:, 16:528], p[:, 16:528], ALU.subtract, [srck, pk],
                   [("mixT", idx)])
        S.barrier(); chk("g%d_p2" % g)

        for gp in range(4):
            for dj in range(4):
                b = next_bank()
                for cc in range(4):
                    MM(pbs[b][:], wpool[:, gp * 4 + cc, dj * 128:(dj + 1) * 128], mixT[:, gp * 4 + cc, :],
                       cc == 0, cc == 3, ["wpool", ("mixT", gp * 4 + cc)], [("pb", b)])
                blk = gp * 4 + dj
                STT(yin[:, 16 + blk, :], pbs[b][:], pscale[:, blk:blk + 1], yin[:, 16 + blk, :],
                    ALU.mult, ALU.mult, [("pb", b), ("yin", 16 + blk), "cst"], [("yin", 16 + blk)])
        S.barrier(); chk("g%d_p3" % g)

        nks = [8 + 4 * g + j + 1 for j in range(4)]
        S.add("pool", lambda e: e.memset(kixAB[0][64:128, :], 0.0), [], [("kixAB", 0, 1)])
        S.add("pool", lambda e: e.memset(kixAB[1][0:64, :], 0.0), [], [("kixAB", 1, 0)])
        CP("pool", kixAB[0][0:64, :], kixT[0:64, :], kix_all, [("kixAB", 0, 0)])
        CP("pool", kixAB[1][64:128, :], kixT[64:128, :], kix_all, [("kixAB", 1, 1)])
        kixAB_keys = [("kixAB", 0, 0), ("kixAB", 0, 1), ("kixAB", 1, 0), ("kixAB", 1, 1)]
        for j in range(4):
            ncol = nks[j] * 128
            qi_ = 4 * g + j
            TS("dve", negb[j][:, 0:ncol], kchunk[:, 0:ncol], qchunk[:, qi_:qi_ + 1], -BIG, ALU.is_gt, ALU.mult,
               ["kchunk", "cst"], [("negb", j)])
        rctr = [0]
        abctr = [0]

        def emit_scores(j):
            ncol = nks[j] * 128
            dg = diag[j % 2]
            for h in range(16):
                TS("pool", dg[:, h, :], ident_bf, wix_sb[:, j, h:h + 1], 1.0, ALU.mult, ALU.mult,
                   ["identb", ("wix", j)], [("diag", j % 2, h)])
            for s0 in range(0, ncol, 512):
                wd = min(512, ncol - s0)
                ab = 6 + abctr[0] % 2
                abctr[0] += 1
                slots = {}

                def dots(h):
                    b = next_bank()
                    MM(pbs[b][:, 0:wd], qiT[:, h // 2, j * 128:(j + 1) * 128], kixAB[h % 2][:, s0:s0 + wd],
                       True, True, [("qiT", h // 2)] + kixAB_keys, [("pb", b)])
                    rs_ = rctr[0] % 4
                    rctr[0] += 1
                    slots[h] = rs_
                    if j >= 2 and h % 2 == 1:
                        TS("dve", rbuf[rs_][:, 0:wd], pbs[b][:, 0:wd], 0.0, None, ALU.max, None,
                           [("pb", b)], [("rbuf", rs_)])
                    else:
                        ACT(rbuf[rs_][:, 0:wd], pbs[b][:, 0:wd], AF.Relu, [("pb", b)], [("rbuf", rs_)])

                dots(0)
                dots(1)
                for h in range(16):
                    if h + 2 < 16:
                        dots(h + 2)
                    rs_ = slots[h]
                    MM(pbs[ab][:, 0:wd], dg[:, h, :], rbuf[rs_][:, 0:wd], h == 0, False,
                       [("diag", j % 2, h), ("rbuf", rs_)], [("pb", ab)])
                MM(pbs[ab][:, 0:wd], ident_bf, negb[j][:, s0:s0 + wd], False, True,
                   ["identb", ("negb", j)], [("pb", ab)])
                CP("act", acc[j][:, s0:s0 + wd], pbs[ab][:, 0:wd], [("pb", ab)], [("acc", j)])

        def emit_bisect(js):
            for j in js:
                S.add("dve", lambda e, j=j: e.memset(cand[:, j:j + 1], 0.0), [], [("cand", j)])
            for it in range(BIS_N):
                step = BIS_R * (2.0 ** (-it))
                last = it == BIS_N - 1
                for j in js:
                    ncol = nks[j] * 128
                    TS("dve", junk[:, 0:ncol], acc[j][:, 0:ncol], cand[:, j:j + 1], 0.0, ALU.is_ge, ALU.add,
                       [("acc", j), ("cand", j)], ["junk", ("cnt", j)], accum=cnt[:, j:j + 1])
                    TS("dve", tflag[:, j:j + 1], cnt[:, j:j + 1], TOPK - 0.5, step, ALU.is_ge, ALU.mult,
                       [("cnt", j)], [("tflag", j)])
                    STT(cand[:, j:j + 1], tflag[:, j:j + 1], (-step if last else -0.5 * step), cand[:, j:j + 1],
                        ALU.add, ALU.add, [("tflag", j), ("cand", j)], [("cand", j)])

        def emit_masks(js):
            for j in js:
                ncol = nks[j] * 128
                ms = j % 2
                TS("dve", maskbf[ms][:, 0:ncol], acc[j][:, 0:ncol], cand[:, j:j + 1], MASK_OFF, ALU.is_ge, ALU.mult,
                   [("acc", j), ("cand", j)], [("maskbf", ms)])
                for k8 in range(0, nks[j], 8):
                    n8 = min(8, nks[j] - k8)
                    b = tr_banks[tr_ctr[0] % len(tr_banks)]
                    tr_ctr[0] += 1
                    pt = pbT(b)
                    for i in range(n8):
                        kt = k8 + i
                        TR(pt[:, i, :], maskbf[ms][:, kt * 128:(kt + 1) * 128], ident_bf,
                           [("maskbf", ms), "identb"], [("pb", b)])
                    CP("act", maskT[:, k8:k8 + n8, j, :], pt[:, 0:n8, :], [("pb", b)], [("maskT", j)])

        emit_scores(3)
        emit_scores(2)
        emit_bisect([3, 2])
        emit_scores(1)
        emit_scores(0)
        emit_masks([3, 2])
        emit_bisect([1, 0])
        emit_masks([1, 0])
        S.barrier(); chk("g%d_p4" % g)

        nkmax = 8 + 4 * g + 4
        mask_all = [("maskT", j) for j in range(4)]
        items = [(h, kt) for h in range(16) for kt in range(nkmax)]
        LA = 2
        SB = [0, 1, 2]
        OB = [3, 6]
        kvb = [0]

        def dma_k(h):
            sl = h % 2
            DMA("pool", wuk[sl], w_uk[h].rearrange("(cc p) d -> p cc d", p=128), [], [("wuk", sl)], K_UK[sl])

        def mm_k(h):
            sl = h % 2
            for s0 in range(0, nkmax * 128, 512):
                b = 4 + kvb[0] % 2
                kvb[0] += 1
                for cc in range(4):
                    MM(pbs[b][:], wuk[sl][:, cc, :], cTn[:, cc, s0:s0 + 512], cc == 0, cc == 3,
                       [("wuk", sl)] + cTn_all, [("pb", b)])
                CP("act", kTh[sl][:, s0:s0 + 512], pbs[b][:], [("pb", b)], [("kTh", sl, s0 // 512)])

        def dma_v4(hgp):
            sl = hgp % 2
            for hh in range(4):
                DMA("pool", wuv4[sl][:, :, hh * 128:(hh + 1) * 128],
                    w_uv[4 * hgp + hh].rearrange("(cc p) d -> p cc d", p=128),
                    [], [("wuv4", sl, hh)], K_UV4[sl][hh])

        def mm_v4(hgp):
            sl = hgp % 2
            for kt in range(nkmax):
                b = 4 + kvb[0] % 2
                kvb[0] += 1
                for cc in range(4):
                    MM(pbs[b][:], cTn[:, cc, kt * 128:(kt + 1) * 128], wuv4[sl][:, cc, :],
                       cc == 0, cc == 3, [("wuv4", sl, hh_) for hh_ in range(4)] + cTn_all, [("pb", b)])
                eng = "dve" if kt % 2 == 0 else "act"
                CP(eng, vS4[sl][:, kt, :], pbs[b][:], [("pb", b)], [("vS4", sl, kt // 4)])

        def emit_qk(i):
            h, kt = items[i]
            sl = h % 2
            jmin = max(0, kt - 8 - 4 * g)
            c0 = jmin * 128
            sb_ = SB[i % 3]
            es = i % 3
            MM(pbs[sb_][:, c0:GT], kTh[sl][:, kt * 128:(kt + 1) * 128], qT[:, h, c0:GT], True, False,
               [("kTh", sl, kt // 4), ("qT", h)], [("pb", sb_)])
            MM(pbs[sb_][:, c0:GT], ident_bf, maskT[:, kt, jmin:4, :].rearrange("p j t -> p (j t)"), False, True,
               ["identb"] + mask_all, [("pb", sb_)])
            ACT(eT[es][:, c0:GT], pbs[sb_][:, c0:GT], AF.Exp, [("pb", sb_), "cst"], [("eT", es)], bias=negoff)

        def emit_pv(i):
            h, kt = items[i]
            sl = h % 2
            jmin = max(0, kt - 8 - 4 * g)
            c0 = jmin * 128
            es = i % 3
            ob = OB[h % 2]
            vsl = (h // 4) % 2
            MM(pbs[ob][:, c0:GT], vS4[vsl][:, kt, (h % 4) * 128:(h % 4 + 1) * 128], eT[es][:, c0:GT],
               kt == 0, kt == nkmax - 1, [("vS4", vsl, kt // 4), ("eT", es)], [("pb", ob)])
            MM(pbs[7][:, c0:GT], ones_bf, eT[es][:, c0:GT], kt == 0, kt == nkmax - 1,
               ["onesb", ("eT", es)], [("pb", 7)])
            if kt == nkmax - 1:
                os_ = h % 2
                RCP(rcb[os_], pbs[7][:], [("pb", 7)], [("rcb", os_)])
                TT("dve", otmp[os_], pbs[ob][:], rcb[os_], ALU.mult, [("pb", ob), ("rcb", os_)], [("otmp", os_)])
                TT("pool", yin[:, h, :], otmp[os_], yin[:, h, :], ALU.mult,
                   [("otmp", os_), ("yin", h)], [("yin", h)])

        dma_k(0)
        dma_k(1)
        dma_v4(0)
        mm_k(0)
        mm_v4(0)
        for i in range(LA):
            emit_qk(i)
        for i in range(len(items)):
            h, kt = items[i]
            if kt == 0:
                if h + 2 < 16:
                    dma_k(h + 2)
                if h % 4 == 0 and h // 4 + 1 < 4:
                    dma_v4(h // 4 + 1)
                if h + 1 < 16:
                    mm_k(h + 1)
                if h % 4 == 1 and h // 4 + 1 < 4:
                    mm_v4(h // 4 + 1)
            if i + LA < len(items):
                emit_qk(i + LA)
            emit_pv(i)
        S.barrier(); chk("g%d_p5" % g)

        yin_all = [("yin", i) for i in range(32)]
        def ssq_mm(db):
            qs = db % 2
            for j in range(4):
                MM(pbs[6][:, j:j + 1], sqb[qs][:, j * 128:(j + 1) * 128], ones_bf[:, 0:1],
                   db == 0 and j == 0, db == 31 and j == 3, [("sqb", qs), "onesb"], [("pb", 6)])

        for db in range(32):
            sl = db % 3
            DMA("pool", wb6[sl], w_out_v[:, :, db * 128:(db + 1) * 128], [], [("wb6", sl)], K_WB[sl])
            b = next_bank()
            for ec in range(KC):
                MM(pbs[b][:], wb6[sl][:, ec, :], yin[:, ec, :], ec == 0, ec == KC - 1,
                   yin_all + [("wb6", sl)], [("pb", b)])
            if db > 0:
                ssq_mm(db - 1)
            qs = db % 2
            ACT(sqb[qs], pbs[b][:], AF.Square, [("pb", b)], [("sqb", qs)])
            TS("dve", yg[:, db, :], pbs[b][:], gpost[:, db:db + 1], None, ALU.mult, None,
               [("pb", b), "cst"], [("yg", db)])
        ssq_mm(31)
        ACT(sdo, pbs[6][:, 0:4], AF.Sqrt, [("pb", 6), "cst"], ["sdo"], scale=1.0 / D, bias=epsT)
        RCP(rstdo, sdo, ["sdo"], ["rstdo"])
        yg_all = [("yg", db) for db in range(32)]
        tr_banks[:] = [0, 1, 2, 3, 4, 5]
        for j in range(4):
            sl = xt_ctr[0] % 2
            xt_ctr[0] += 1
            t0 = g * GT + j * 128
            DMA("sp", xt[sl], xo[t0:t0 + 128, :], [], [("xt", sl)], K_XT[sl])
            for d4 in range(8):
                b = tr_banks[tr_ctr[0] % len(tr_banks)]
                tr_ctr[0] += 1
                for i in range(4):
                    db = d4 * 4 + i
                    TR(pbs[b][:, i * 128:(i + 1) * 128], yg[:, db, j * 128:(j + 1) * 128], ident_f,
                       [("yg", db), "identf"], [("pb", b)])
                STT(xt[sl][:, d4 * 512:(d4 + 1) * 512], pbs[b][:], rstdo[:, j:j + 1],
                    xt[sl][:, d4 * 512:(d4 + 1) * 512], ALU.mult, ALU.add,
                    [("pb", b), "rstdo", ("xt", sl)], [("xt", sl)])
            DMA("sp", yout[t0:t0 + 128, :], xt[sl], [("xt", sl)], [("yout", g, j)], K_ST[sl])
        tr_banks[:] = [0, 1]
        S.barrier(); chk("g%d_p6" % g)

    S.emit(st)
    st.close()
    return nc


_NC_CACHE = {}
_ONLY_MAPS = False


def _get_nc():
    if "nc" not in _NC_CACHE:
        _NC_CACHE["nc"] = build_nc()
    return _NC_CACHE["nc"]


def kernel(x, pre_norm, w_in, kv_norm, w_uk, w_uv, idx_k_norm_g, idx_k_norm_b, w_pool, pool_scale, w_out,
           post_norm):
    f32 = np.float32
    x = np.asarray(x, f32)
    B = x.shape[0]
    w_in0 = np.ascontiguousarray(np.asarray(w_in, f32)[0])
    w_out0 = np.ascontiguousarray(np.asarray(w_out, f32)[0])
    w_uk0 = np.ascontiguousarray(np.asarray(w_uk, f32)[0])
    w_uv0 = np.ascontiguousarray(np.asarray(w_uv, f32)[0])
    w_pool0 = np.ascontiguousarray(np.asarray(w_pool, f32)[0])
    gpre_b = np.ascontiguousarray(np.broadcast_to(np.asarray(pre_norm, f32)[0][None, :], (128, D)))
    kchunk_b = np.ascontiguousarray(np.broadcast_to((np.arange(SEQ) // 64).astype(f32)[None, :], (128, SEQ)))
    ident_f = np.eye(128, dtype=f32)
    ident_bf = np.eye(128, dtype=f32).astype(ml_dtypes.bfloat16)
    blk64 = np.zeros((128, 128), f32)
    blk64[0:64, 0:64] = 1.0 / 64
    blk64[64:128, 64:128] = 1.0 / 64
    cst0 = np.zeros((128, 512), f32)
    cst0[:, 0:4] = np.asarray(kv_norm, f32)[0].reshape(4, 128).T
    kg = np.asarray(idx_k_norm_g, f32)[0]
    kb = np.asarray(idx_k_norm_b, f32)[0]
    cst0[:, 4] = np.concatenate([kg, kg])
    cst0[:, 5] = np.concatenate([kb, kb])
    cst0[:, 6] = EPS
    cst0[:, 7] = -MASK_OFF
    cst0[:, 8:24] = np.asarray(pool_scale, f32)[0].reshape(16, 128).T
    cst0[:, 24:56] = np.asarray(post_norm, f32)[0].reshape(32, 128).T
    wins = [2, 4, 8, 16]
    in_maps = []
    for b in range(B):
        for half in range(2):
            cst = cst0.copy()
            tok = half * 1024 + np.arange(1024)
            cst[:, 56:64] = (tok // 64).astype(f32).reshape(8, 128).T
            pf = np.ones((2, 4, 16), f32)
            if half == 0:
                for gi, wdw in enumerate(wins):
                    t = np.arange(16)
                    pf[0, gi, :] = wdw / np.minimum(t + 1, wdw)
            cst[:, 64:192] = pf.reshape(1, 128)
            xo = np.ascontiguousarray(x[b, half * 1024:(half + 1) * 1024])
            xh = np.zeros((2, 128, D), f32)
            if half == 1:
                xh[0] = x[b, 896:1024]
            xh[1] = xo[384:512]
            in_maps.append({
                "xk": np.ascontiguousarray(x[b]), "xo": xo, "xh": xh,
                "w_in": w_in0, "w_out": w_out0, "w_uk": w_uk0, "w_uv": w_uv0, "w_pool": w_pool0,
                "gpre_b": gpre_b, "kchunk_b": kchunk_b, "cst": cst, "ident_f": ident_f, "blk64": blk64,
                "ident_bf": ident_bf,
            })
    if _ONLY_MAPS:
        return in_maps
    nc = _get_nc()
    res = run_bass_kernel_spmd(nc, in_maps, core_ids=list(range(2 * B)))
    out = np.zeros((B, SEQ, D), f32)
    for b in range(B):
        for half in range(2):
            out[b, half * 1024:(half + 1) * 1024] = res.results[2 * b + half]["y"]
    return out
```
